# Optimizing a Trainium2 kernel written in Bass

```python
import math
import jax
import jax.numpy as jnp
from jax import lax
import numpy as np

D_MODEL = 1024
BATCH = 2
SEQ = 8192
DEPTH = 2

GRID_W = 64
CTX_LEN = 256
DN_HEADS = 8
DN_DK = 128
DN_DV = 128
DN_CONV = 5
DN_CHUNK = 64
NA_HEADS = 8
NA_DH = 64
NA_WIN_R = 8
NA_WIN_W = 16
ROPE_BASE = 10000.0
D_FF_DENSE = 2816
N_EXPERTS = 8
TOP_K = 2
D_FF_EXPERT = 3584
EPS = 1e-6

DN_QK = DN_HEADS * DN_DK
DN_V = DN_HEADS * DN_DV
NA_W = NA_HEADS * NA_DH
IN_SIZES = (DN_QK, DN_QK, DN_V, DN_V, 4 * DN_HEADS, NA_W, NA_W, NA_W, D_MODEL, D_MODEL)
D_IN = sum(IN_SIZES)
N_DENSE = (DEPTH + 1) // 2
N_MOE = DEPTH // 2

kernel_name = 'hybrid_deltanet_natten_moe_block'

F32 = jnp.float32


def split_cols(p, sizes):
    out, start = [], 0
    for s in sizes:
        out.append(p[..., start:start + s])
        start += s
    return out


def rmsnorm(x, g):
    xf = x.astype(F32)
    y = xf * lax.rsqrt(jnp.mean(xf * xf, axis=-1, keepdims=True) + EPS)
    return (y * g.astype(F32)).astype(x.dtype)


def l2norm(x):
    return x * lax.rsqrt(jnp.sum(x * x, axis=-1, keepdims=True) + EPS)


def short_conv(x, w):
    k = w.shape[-1]
    pad = k // 2
    t = x.shape[1]
    xp = jnp.pad(x, ((0, 0), (pad, pad), (0, 0)))
    out = xp[:, 0:t] * w[:, 0]
    for i in range(1, k):
        out = out + xp[:, i:i + t] * w[:, i]
    return out


def axial_rope(t_len, dh):
    t = jnp.arange(t_len)
    row = (t // GRID_W).astype(F32)
    col = (t % GRID_W).astype(F32)
    n_freq = dh // 4
    inv = ROPE_BASE ** (-jnp.arange(n_freq, dtype=F32) / n_freq)
    ang = jnp.concatenate([row[:, None] * inv, col[:, None] * inv], axis=-1)
    return jnp.cos(ang), jnp.sin(ang)


def apply_rope(x, cos, sin):
    x1, x2 = jnp.split(x, 2, axis=-1)
    c = cos[:, None, :]
    s = sin[:, None, :]
    return jnp.concatenate([x1 * c - x2 * s, x1 * s + x2 * c], axis=-1)


def gated_delta_chunked(q, k, v, g, beta, s0):
    b_, h_, t_, _ = q.shape
    dv = v.shape[-1]
    c_ = DN_CHUNK
    n_ = t_ // c_
    rs = lambda a: a.reshape((b_, h_, n_, c_) + a.shape[3:])
    q, k, v, g, beta = rs(q), rs(k), rs(v), rs(g), rs(beta)
    gc = jnp.cumsum(g, axis=-1)
    tril = jnp.tril(jnp.ones((c_, c_), dtype=bool))
    strict = jnp.tril(jnp.ones((c_, c_), dtype=bool), -1)
    diff = gc[..., :, None] - gc[..., None, :]
    decay_mat = jnp.where(tril, jnp.exp(jnp.where(tril, diff, 0.0)), 0.0)
    kb = k * beta[..., None]
    vb = v * beta[..., None]
    t_mat = jnp.where(strict, jnp.einsum('bhncd,bhnjd->bhncj', kb, k) * decay_mat, 0.0)
    a_mat = t_mat + jnp.eye(c_, dtype=q.dtype)
    u = lax.linalg.triangular_solve(a_mat, vb, left_side=True, lower=True, unit_diagonal=True)
    w = lax.linalg.triangular_solve(a_mat, kb * jnp.exp(gc)[..., None], left_side=True, lower=True,
                                    unit_diagonal=True)
    attn = jnp.einsum('bhncd,bhnjd->bhncj', q, k) * decay_mat
    g_last = gc[..., -1]
    k_dec = k * jnp.exp(g_last[..., None] - gc)[..., None]
    q_dec = q * jnp.exp(gc)[..., None]

    def step(state, xs):
        u_c, w_c, attn_c, q_c, k_c, gl = xs
        v_new = u_c - jnp.einsum('bhcd,bhde->bhce', w_c, state)
        o_c = jnp.einsum('bhcd,bhde->bhce', q_c, state) + jnp.einsum('bhcj,bhje->bhce', attn_c, v_new)
        state = state * jnp.exp(gl)[..., None, None] + jnp.einsum('bhcd,bhce->bhde', k_c, v_new)
        return state, o_c

    mv = lambda a: jnp.moveaxis(a, 2, 0)
    s_fin, o = lax.scan(step, s0, (mv(u), mv(w), mv(attn), mv(q_dec), mv(k_dec), mv(g_last)))
    o = jnp.moveaxis(o, 0, 2).reshape(b_, h_, t_, dv)
    return o, s_fin


def dn_stream(q, k, v, ab, conv_w, a_log, dt_bias, rope):
    b_, t_, _ = q.shape
    qkv = jax.nn.silu(short_conv(jnp.concatenate([q, k, v], axis=-1), conv_w)).astype(F32)
    q, k, v = split_cols(qkv, (DN_QK, DN_QK, DN_V))
    q = l2norm(q.reshape(b_, t_, DN_HEADS, DN_DK))
    k = l2norm(k.reshape(b_, t_, DN_HEADS, DN_DK))
    if rope is not None:
        q = apply_rope(q, rope[0], rope[1])
        k = apply_rope(k, rope[0], rope[1])
    q = q * (DN_DK ** -0.5)
    v = v.reshape(b_, t_, DN_HEADS, DN_DV)
    ab = ab.astype(F32)
    a = ab[..., :2 * DN_HEADS].reshape(b_, t_, 2, DN_HEADS)
    bl = ab[..., 2 * DN_HEADS:].reshape(b_, t_, 2, DN_HEADS)
    g = -jnp.exp(a_log.astype(F32)) * jax.nn.softplus(a + dt_bias.astype(F32))
    beta = jax.nn.sigmoid(bl)
    to_bht = lambda t: jnp.moveaxis(t, 1, 2)
    to_dbht = lambda t: jnp.transpose(t, (2, 0, 3, 1))
    return to_bht(q), to_bht(k), to_bht(v), to_dbht(g), to_dbht(beta)


def dn_output(o, z, norm_w):
    b_, h_, t_, dv = o.shape
    o = jnp.moveaxis(o, 1, 2)
    o = o * lax.rsqrt(jnp.mean(o * o, axis=-1, keepdims=True) + EPS) * norm_w.astype(F32)
    o = o.reshape(b_, t_, h_ * dv) * jax.nn.silu(z.astype(F32))
    return o.astype(z.dtype)


def neighbourhood_attention(q, k, v, kc, vc, rpb):
    b_, s_, h_, dh = q.shape
    rows = s_ // GRID_W
    kr = min(NA_WIN_R, rows)
    kw = NA_WIN_W
    scale = dh ** -0.5
    r = jnp.arange(rows)
    col = jnp.arange(GRID_W)
    r0 = jnp.clip(r - kr // 2, 0, rows - kr)
    key_rows = r0[:, None] + jnp.arange(kr)[None, :]
    c0 = jnp.clip(col - kw // 2, 0, GRID_W - kw)
    col_in = (col[None, :] >= c0[:, None]) & (col[None, :] < c0[:, None] + kw)
    qg = q.reshape(b_, rows, GRID_W, h_, dh)
    kg = k.reshape(b_, rows, GRID_W, h_, dh)[:, key_rows]
    vg = v.reshape(b_, rows, GRID_W, h_, dh)[:, key_rows].reshape(b_, rows, kr * GRID_W, h_, dh)
    roff = key_rows - r[:, None] + (NA_WIN_R - 1)
    coff = jnp.clip(col[None, :] - col[:, None] + (NA_WIN_W - 1), 0, 2 * NA_WIN_W - 2)
    bias = rpb[:, roff[:, None, :, None], coff[None, :, None, :]].astype(F32)
    s_loc = jnp.einsum('brqhd,brikhd->bhrqik', qg, kg).astype(F32) * scale + bias[None]
    s_loc = jnp.where(col_in[:, None, :], s_loc, -jnp.inf).reshape(b_, h_, rows, GRID_W, kr * GRID_W)
    s_ctx = jnp.einsum('brqhd,bchd->bhrqc', qg, kc).astype(F32) * scale
    p = jax.nn.softmax(jnp.concatenate([s_loc, s_ctx], axis=-1), axis=-1).astype(v.dtype)
    p_loc = p[..., :kr * GRID_W]
    p_ctx = p[..., kr * GRID_W:]
    o = (jnp.einsum('bhrqk,brkhd->brqhd', p_loc, vg)
         + jnp.einsum('bhrqc,bchd->brqhd', p_ctx, vc))
    return o.reshape(b_, s_, h_ * dh)


def context_attention(q, k, v):
    b_, c_, h_, dh = q.shape
    s = jnp.einsum('bqhd,bkhd->bhqk', q, k).astype(F32) * (dh ** -0.5)
    p = jax.nn.softmax(s, axis=-1).astype(v.dtype)
    return jnp.einsum('bhqk,bkhd->bqhd', p, v).reshape(b_, c_, h_ * dh)


def merge_branches(dn_o, na_o, gate_dn, gate_na, w_pa, w_pb, w_out):
    y = jax.nn.sigmoid(gate_dn) * (dn_o @ w_pa) + jax.nn.sigmoid(gate_na) * (na_o @ w_pb)
    return y @ w_out


def hybrid_mixer(h, hc, w_in, conv_w, a_log, dt_bias, dn_norm_w, rpb, w_pa, w_pb, w_out, cos, sin,
                 need_ctx):
    b_ = h.shape[0]
    p = split_cols(h @ w_in, IN_SIZES)
    pc = split_cols(hc @ w_in, IN_SIZES)
    ql, kl, vl, gl, bl = dn_stream(p[0], p[1], p[2], p[4], conv_w, a_log, dt_bias, (cos, sin))
    qc, kc, vc, gc, bc = dn_stream(pc[0], pc[1], pc[2], pc[4], conv_w, a_log, dt_bias, None)
    s0 = jnp.zeros((b_, DN_HEADS, DN_DK, DN_DV), F32)
    flip = lambda t: jnp.flip(t, axis=2)
    oc_f, s_f = gated_delta_chunked(qc, kc, vc, gc[0], bc[0], s0)
    oc_b, s_b = gated_delta_chunked(flip(qc), flip(kc), flip(vc), flip(gc[1]), flip(bc[1]), s0)
    ol_f, _ = gated_delta_chunked(ql, kl, vl, gl[0], bl[0], s_f)
    ol_b, _ = gated_delta_chunked(flip(ql), flip(kl), flip(vl), flip(gl[1]), flip(bl[1]), s_b)
    dn_lat = dn_output(ol_f + flip(ol_b), p[3], dn_norm_w)
    heads = lambda t: t.reshape(t.shape[0], t.shape[1], NA_HEADS, NA_DH)
    kcn, vcn = heads(pc[6]), heads(pc[7])
    na_lat = neighbourhood_attention(heads(p[5]), heads(p[6]), heads(p[7]), kcn, vcn, rpb)
    y = merge_branches(dn_lat, na_lat, p[8], p[9], w_pa, w_pb, w_out)
    if not need_ctx:
        return y, None
    dn_ctx = dn_output(oc_f + flip(oc_b), pc[3], dn_norm_w)
    na_ctx = context_attention(heads(pc[5]), kcn, vcn)
    yc = merge_branches(dn_ctx, na_ctx, pc[8], pc[9], w_pa, w_pb, w_out)
    return y, yc


def swiglu(h, w1, w3, w2):
    return (jax.nn.silu(h @ w1) * (h @ w3)) @ w2


def moe_swiglu(h, router, w1, w3, w2):
    logits = (h @ router).astype(F32)
    top_v, top_i = lax.top_k(logits, TOP_K)
    top_w = jax.nn.softmax(top_v, axis=-1)
    gate = jnp.sum(jax.nn.one_hot(top_i, N_EXPERTS, dtype=F32) * top_w[..., None], axis=-2)
    out = jnp.zeros_like(h)
    for e in range(N_EXPERTS):
        out = out + gate[..., e:e + 1].astype(h.dtype) * swiglu(h, w1[e], w3[e], w2[e])
    return out


def setup_inputs(seed: int = 0) -> dict:
    key = jax.random.key(seed)
    ks = iter(jax.random.split(key, 32))
    nrm = lambda shape, scale: jax.random.normal(next(ks), shape, F32) * scale
    gain = lambda shape: 1.0 + nrm(shape, 0.02)
    d = D_MODEL
    inp = {}
    inp['x'] = nrm((BATCH, SEQ, d), 1.0)
    inp['c'] = nrm((BATCH, d), 1.0)
    inp['ctx'] = nrm((BATCH, CTX_LEN, d), 1.0)
    inp['c_ctx'] = nrm((d,), 1.0)
    inp['ada_w'] = nrm((DEPTH, d, 6 * d), 0.5 * d ** -0.5)
    inp['ada_b'] = nrm((DEPTH, 6 * d), 0.02)
    inp['norm_mix_pre'] = gain((DEPTH, d))
    inp['norm_mix_post'] = gain((DEPTH, d))
    inp['norm_ffn_pre'] = gain((DEPTH, d))
    inp['norm_ffn_post'] = gain((DEPTH, d))
    inp['w_in'] = nrm((DEPTH, d, D_IN), d ** -0.5)
    inp['dn_conv'] = nrm((DEPTH, 2 * DN_QK + DN_V, DN_CONV), DN_CONV ** -0.5)
    inp['dn_a_log'] = jnp.log(jax.random.uniform(next(ks), (DEPTH, 2, DN_HEADS), F32, 1.0, 16.0))
    dt = jnp.exp(jax.random.uniform(next(ks), (DEPTH, 2, DN_HEADS), F32, math.log(1e-3), math.log(1e-1)))
    inp['dn_dt_bias'] = dt + jnp.log(-jnp.expm1(-dt))
    inp['dn_norm'] = gain((DEPTH, DN_DV))
    inp['na_rpb'] = nrm((DEPTH, NA_HEADS, 2 * NA_WIN_R - 1, 2 * NA_WIN_W - 1), 0.05)
    inp['w_branch_dn'] = nrm((DEPTH, DN_V, d), DN_V ** -0.5)
    inp['w_branch_na'] = nrm((DEPTH, NA_W, d), NA_W ** -0.5)
    inp['w_out'] = nrm((DEPTH, d, d), d ** -0.5)
    inp['ffn_w1'] = nrm((N_DENSE, d, D_FF_DENSE), d ** -0.5)
    inp['ffn_w3'] = nrm((N_DENSE, d, D_FF_DENSE), d ** -0.5)
    inp['ffn_w2'] = nrm((N_DENSE, D_FF_DENSE, d), D_FF_DENSE ** -0.5)
    inp['moe_router'] = nrm((N_MOE, d, N_EXPERTS), d ** -0.5)
    inp['moe_w1'] = nrm((N_MOE, N_EXPERTS, d, D_FF_EXPERT), d ** -0.5)
    inp['moe_w3'] = nrm((N_MOE, N_EXPERTS, d, D_FF_EXPERT), d ** -0.5)
    inp['moe_w2'] = nrm((N_MOE, N_EXPERTS, D_FF_EXPERT, d), D_FF_EXPERT ** -0.5)
    return inp


def reference(x, c, ctx, c_ctx, ada_w, ada_b, norm_mix_pre, norm_mix_post, norm_ffn_pre, norm_ffn_post,
              w_in, dn_conv, dn_a_log, dn_dt_bias, dn_norm, na_rpb, w_branch_dn, w_branch_na, w_out,
              ffn_w1, ffn_w3, ffn_w2, moe_router, moe_w1, moe_w3, moe_w2):
    seq_len = x.shape[1]
    cos, sin = axial_rope(seq_len, DN_DK)
    xc = ctx
    for l in range(DEPTH):
        last = l == DEPTH - 1
        mod = jax.nn.silu(c) @ ada_w[l] + ada_b[l]
        sh1, sc1, g1, sh2, sc2, g2 = jnp.split(mod[:, None, :], 6, axis=-1)
        cmod = jax.nn.silu(c_ctx) @ ada_w[l] + ada_b[l]
        csh1, csc1, cg1, csh2, csc2, cg2 = jnp.split(cmod, 6)

        h = rmsnorm(x, norm_mix_pre[l]) * (1 + sc1) + sh1
        hc = rmsnorm(xc, norm_mix_pre[l]) * (1 + csc1) + csh1
        y, yc = hybrid_mixer(h, hc, w_in[l], dn_conv[l], dn_a_log[l], dn_dt_bias[l], dn_norm[l], na_rpb[l],
                             w_branch_dn[l], w_branch_na[l], w_out[l], cos, sin, not last)
        x = x + g1 * rmsnorm(y, norm_mix_post[l])

        def channel_mixer(t):
            if l % 2 == 0:
                return swiglu(t, ffn_w1[l // 2], ffn_w3[l // 2], ffn_w2[l // 2])
            return moe_swiglu(t, moe_router[l // 2], moe_w1[l // 2], moe_w3[l // 2], moe_w2[l // 2])

        h = rmsnorm(x, norm_ffn_pre[l]) * (1 + sc2) + sh2
        x = x + g2 * rmsnorm(channel_mixer(h), norm_ffn_post[l])
        if not last:
            xc = xc + cg1 * rmsnorm(yc, norm_mix_post[l])
            hc = rmsnorm(xc, norm_ffn_pre[l]) * (1 + csc2) + csh2
            xc = xc + cg2 * rmsnorm(channel_mixer(hc), norm_ffn_post[l])
    return x
```

```python
import contextlib
import os
import numpy as np
import ml_dtypes
import concourse.bass as bass
import concourse.mybir as mybir
from concourse.bass_utils import run_bass_kernel_spmd

F32 = mybir.dt.float32
BF16 = mybir.dt.bfloat16
AF = mybir.ActivationFunctionType
ALU = mybir.AluOpType
AX = mybir.AxisListType
NPBF = ml_dtypes.bfloat16

D = 1024
NB = 2
SEQ = 8192
CTX = 256
TB = CTX + SEQ
NT_B = TB // 128
OWN = 2048 + 64
EPS = 1e-6
DFF = 2816
NE = 8
DFE = 3584
D_IN = 7712
NEG = -30000.0

ENGINES = ("sync", "tensor", "vector", "scalar", "gpsimd")
EPOCH = 30000
DMA_K = 8


class Prog:
    def __init__(self, nc, n_sems=140):
        self.nc = nc
        self.es = contextlib.ExitStack()
        self.sem_next = 0
        self.streams = {e: [] for e in ENGINES}
        self.cnt = {e: 0 for e in ENGINES}
        self.sem = {e: self._new_sem() for e in ENGINES}
        self.known = {e: {} for e in ENGINES}
        self.last_write = {}
        self.readers = {}
        self.dma_sems = {}
        self.dma_cnt = {}
        self.ninst = 0

    def _new_sem(self):
        s = self.es.enter_context(self.nc.semaphore("s%d" % self.sem_next))
        self.sem_next += 1
        return s

    def _wait(self, eng, tok):
        s, v = tok
        k = self.known[eng]
        if k.get(id(s), 0) >= v:
            return
        k[id(s)] = v
        self.streams[eng].append(lambda e, s=s, v=v: e.wait_ge(s, v))

    def _deps(self, eng, reads, writes):
        toks = []
        for r in reads:
            t = self.last_write.get(r)
            if t is not None:
                toks.append(t)
        for w in writes:
            t = self.last_write.get(w)
            if t is not None:
                toks.append(t)
            toks.extend(self.readers.get(w, {}).values())
        own = self.sem[eng]
        for t in toks:
            if eng == "tensor" and t[0] is own:
                continue
            self._wait(eng, t)

    def _record(self, tok, reads, writes):
        for w in writes:
            self.last_write[w] = tok
            self.readers[w] = {}
        for r in reads:
            if r in writes:
                continue
            self.readers.setdefault(r, {})[id(tok[0])] = tok

    def op(self, eng, fn, reads=(), writes=()):
        reads = tuple(reads)
        writes = tuple(writes) + tuple(r for r in reads if r.startswith("psum"))
        self._deps(eng, reads, writes)
        if self.cnt[eng] >= EPOCH:
            self.sem[eng] = self._new_sem()
            self.cnt[eng] = 0
        self.cnt[eng] += 1
        s, v = self.sem[eng], self.cnt[eng]
        self.streams[eng].append(lambda e, s=s: fn(e).then_inc(s, 1))
        self._record((s, v), reads, writes)
        self.ninst += 1
        return (s, v)

    def dma(self, eng, fn, reads=(), writes=()):
        reads = tuple(reads)
        writes = tuple(writes)
        self._deps(eng, reads, writes)
        if eng not in self.dma_sems:
            self.dma_sems[eng] = [self._new_sem() for _ in range(DMA_K)]
            self.dma_cnt[eng] = 0
        i = self.dma_cnt[eng]
        self.dma_cnt[eng] += 1
        s = self.dma_sems[eng][i % DMA_K]
        tgt = 16 * (i // DMA_K + 1)
        if tgt > 16:
            self._wait(eng, (s, tgt - 16))
        self.streams[eng].append(lambda e, s=s: fn(e).then_inc(s, 16))
        self._record((s, tgt), reads, writes)
        self.ninst += 1
        return (s, tgt)

    def finish(self, eng="sync"):
        for t in list(self.last_write.values()):
            self._wait(eng, t)

    def build(self):
        nc = self.nc
        with nc.Block() as block:
            for e in ENGINES:
                stream = self.streams[e]
                if not stream:
                    continue

                def body(eng, stream=stream):
                    for f in stream:
                        f(eng)
                getattr(block, e)(body)
        self.streams = {e: [] for e in ENGINES}


class Ctx:
    def __init__(self, nc):
        self.nc = nc
        self.P = Prog(nc)
        self.es = contextlib.ExitStack()
        self.nps = 0
        self.dmaq = 0
        self.uid = 0

    def sb(self, name, shape, dt, es=None):
        self.uid += 1
        return (es or self.es).enter_context(self.nc.sbuf_tensor("%s_%d" % (name, self.uid), list(shape), dt))

    def psum(self, name, shape, dt, es=None):
        self.uid += 1
        return (es or self.es).enter_context(self.nc.psum_tensor("%s_%d" % (name, self.uid), list(shape), dt))

    def flush(self):
        if self.P.ninst == getattr(self, "_flushed_at", -1):
            return
        self.P.finish()
        self.P.build()
        self._flushed_at = self.P.ninst

    def dram(self, name, shape, dt, kind):
        return self.nc.dram_tensor(name, list(shape), dt, kind=kind).ap()

    def mm(self, out, lhsT, rhs, start, stop, r, w):
        return self.P.op("tensor", lambda e: e.matmul(out, lhsT=lhsT, rhs=rhs, start=start, stop=stop), r, w)

    def tr(self, out, in_, ident, r, w):
        return self.P.op("tensor", lambda e: e.transpose(out, in_, ident), r, w)

    def act(self, out, in_, func, r, w, bias=None, scale=None, accum_out=None, eng="scalar"):
        kw = {}
        if bias is not None:
            kw["bias"] = bias
        if scale is not None:
            kw["scale"] = scale
        if accum_out is not None:
            kw["accum_out"] = accum_out
        return self.P.op("scalar", lambda e: e.activation(out=out, in_=in_, func=func, **kw), r, w)

    def tt(self, out, in0, in1, op, r, w, eng="vector"):
        return self.P.op(eng, lambda e: e.tensor_tensor(out=out, in0=in0, in1=in1, op=op), r, w)

    def ts(self, out, in0, s1, op0, r, w, s2=None, op1=None, eng="vector", accum_out=None):
        kw = {}
        if op1 is not None:
            kw["op1"] = op1
        if accum_out is not None:
            kw["accum_out"] = accum_out
        return self.P.op(eng, lambda e: e.tensor_scalar(out=out, in0=in0, scalar1=s1, scalar2=s2, op0=op0, **kw), r, w)

    def stt(self, out, in0, scalar, in1, op0, op1, r, w):
        return self.P.op("vector", lambda e: e.scalar_tensor_tensor(out=out, in0=in0, scalar=scalar, in1=in1,
                                                                   op0=op0, op1=op1), r, w)

    def cp(self, out, in_, r, w, eng="vector"):
        if eng == "scalar":
            return self.P.op("scalar", lambda e: e.copy(out=out, in_=in_), r, w)
        return self.P.op(eng, lambda e: e.tensor_copy(out=out, in_=in_), r, w)

    def memset(self, ap, val, w, eng="vector"):
        return self.P.op(eng, lambda e: e.memset(ap, val), (), w)

    def recip(self, out, in_, r, w):
        return self.P.op("vector", lambda e: e.reciprocal(out=out, in_=in_), r, w)

    def red(self, out, in_, op, r, w, axis=AX.X):
        return self.P.op("vector", lambda e: e.tensor_reduce(out=out, in_=in_, axis=axis, op=op), r, w)

    def dma(self, out, in_, r, w, eng=None):
        if eng is None:
            eng = ("sync", "gpsimd")[self.dmaq % 2]
            self.dmaq += 1
        return self.P.dma(eng, lambda e: e.dma_start(out=out, in_=in_), r, w)


class PsumPool:
    def __init__(self, C, n=8):
        self.C = C
        self.t = [C.psum("psb", [128, 512], F32) for _ in range(n)]
        self.i = 0
        self.n = n

    def get(self):
        i = self.i
        self.i = (self.i + 1) % self.n
        return self.t[i], "psum%d" % i


def rstd_from_ss(C, ss, rs, n, key, inv_n):
    C.ts(rs, ss, inv_n, ALU.mult, [key], [key + "_r"], s2=EPS, op1=ALU.add)
    C.act(rs, rs, AF.Sqrt, [key + "_r"], [key + "_r"])
    C.recip(rs, rs, [key + "_r"], [key + "_r"])


def compute_mods(C, pp, l, blocks, cvT_d, ada_w_d, ada_b_d, ones_f, es):
    out = {}
    for blk in blocks:
        out[blk] = C.sb("mod%d" % blk, [128, 2, 1024], F32, es)
    with contextlib.ExitStack() as tes:
        cv = C.sb("cv", [128, 2, 8], F32, tes)
        C.dma(cv[:], cvT_d, [], ["cv"], eng="sync")
        C.act(cv[:], cv[:], AF.Silu, ["cv"], ["cv"])
        rep = C.sb("rep", [128, 2, 8, 128], F32, tes)
        for s in range(2):
            for k in range(8):
                C.ts(rep[:, s, k, :], ones_f[:], cv[:, s, k:k + 1], ALU.mult, ["cv", "ones_f"], ["rep"])
        wt = [C.sb("adaw", [128, 8, 512], F32, tes) for _ in range(2)]
        bt = [C.sb("adab", [128, 512], F32, tes) for _ in range(2)]
        it = 0
        for blk in blocks:
            m = out[blk]
            for half in range(2):
                col = blk * 1024 + half * 512
                w = wt[it % 2]
                bb = bt[it % 2]
                wk = "adaw%d" % (it % 2)
                it += 1
                C.dma(w[:], ada_w_d[l].rearrange("(k p) n -> p k n", p=128)[:, :, col:col + 512], [], [wk], eng="sync")
                C.dma(bb[:], ada_b_d[l][:, col:col + 512], [], [wk + "b"], eng="sync")
                for s in range(2):
                    ps, pk = pp.get()
                    for k in range(8):
                        C.mm(ps[:], rep[:, s, k, :], w[:, k, :], k == 0, k == 7, ["rep", wk], [pk])
                    C.tt(m[:, s, half * 512:(half + 1) * 512], ps[:], bb[:], ALU.add, [pk, wk + "b"], ["mod%d" % blk])
        C.flush()
    return out


def phase_A(C, pp, xs, hT, mods, gpre, ident, es):
    M1 = C.sb("M1", [128, 2, 1024], F32, es)
    for s in range(2):
        C.stt(M1[:, s, :], mods[1][:, s, :], 1.0, gpre[:], ALU.add, ALU.mult, ["mod1", "gpre"], ["M1"])
    SH = mods[0]
    ss = C.sb("ssA", [128, 17], F32, es)
    rs = C.sb("rsA", [128, 17], F32, es)
    junk = C.sb("junkA", [128, 1024], F32, es)
    C.memset(ss[:], 1.0, ["ssA"])
    for t in range(17):
        rows = 128 if t < 16 else 64
        C.act(junk[:rows, :], xs[:rows, t, :], AF.Square, ["xs"], ["junkA", "ssA"], accum_out=ss[:rows, t:t + 1])
    C.ts(rs[:], ss[:], 1.0 / D, ALU.mult, ["ssA"], ["rsA"], s2=EPS, op1=ALU.add)
    C.act(rs[:], rs[:], AF.Sqrt, ["rsA"], ["rsA"])
    C.recip(rs[:], rs[:], ["rsA"], ["rsA"])
    tmp = [C.sb("tmpA", [128, 1024], F32, es) for _ in range(2)]
    hb = [C.sb("hbA", [128, 1024], BF16, es) for _ in range(2)]
    for t in range(17):
        rows = 128 if t < 16 else 64
        s = 0 if t < 16 else 1
        i = t % 2
        C.stt(tmp[i][:rows, :], xs[:rows, t, :], rs[:rows, t:t + 1], M1[:rows, s, :], ALU.mult, ALU.mult,
              ["xs", "rsA", "M1"], ["tmpA%d" % i])
        C.tt(hb[i][:rows, :], tmp[i][:rows, :], SH[:rows, s, :], ALU.add, ["tmpA%d" % i, "mod0"], ["hbA%d" % i],
             eng="gpsimd")
        ps, pk = pp.get()
        psb = ps[:].bitcast(BF16)
        for k in range(8):
            C.tr(psb[:, k * 128:k * 128 + rows], hb[i][:rows, k * 128:(k + 1) * 128], ident[:rows, :rows],
                 ["hbA%d" % i, "ident"], [pk])
        C.cp(hT[:, :, t * 128:t * 128 + rows],
             psb.rearrange("p (k t) -> p k t", k=8)[:, :, 0:rows], [pk], ["hT"], eng="scalar")


def phase_C1(C, pp, hT_d, dnT_d, naT_d, wg_d, wpa_d, wpb_d, yT_all, es0, colmap=None):
    with contextlib.ExitStack() as es:
        wg = C.sb("wg", [128, 8, 2048], BF16, es)
        wpa = C.sb("wpa", [128, 8, 1024], BF16, es)
        wpb = C.sb("wpb", [128, 4, 1024], BF16, es)
        for k in range(8):
            C.dma(wg[:, k, :], wg_d[k * 128:(k + 1) * 128, :], [], ["wg"], eng="gpsimd")
            C.dma(wpa[:, k, :], wpa_d[k * 128:(k + 1) * 128, :], [], ["wpa"], eng="gpsimd")
        for k in range(4):
            C.dma(wpb[:, k, :], wpb_d[k * 128:(k + 1) * 128, :], [], ["wpb"], eng="gpsimd")
        hTt = [C.sb("hTt", [128, 8, 512], BF16, es) for _ in range(2)]
        dnt = [C.sb("dnt", [128, 8, 512], BF16, es) for _ in range(2)]
        nat = [C.sb("nat", [128, 4, 512], BF16, es) for _ in range(2)]
        s1 = [C.sb("s1", [128, 512], F32, es) for _ in range(2)]
        s2 = [C.sb("s2", [128, 512], F32, es) for _ in range(2)]
        for tt in range(5):
            n = 512 if tt < 4 else 64
            t0 = tt * 512
            i = tt % 2
            C.dma(hTt[i][:, :, :n], hT_d.rearrange("(k p) t -> p k t", p=128)[:, :, t0:t0 + n], [], ["hTt%d" % i], eng="sync")
            if colmap is None:
                C.dma(dnt[i][:, :, :n], dnT_d.rearrange("h p t -> p h t")[:, :, t0:t0 + n], [], ["dnt%d" % i], eng="sync")
                C.dma(nat[i][:, :, :n], naT_d.rearrange("h p t -> p h t")[:, :, t0:t0 + n], [], ["nat%d" % i], eng="sync")
            else:
                m0 = colmap(tt)
                C.dma(dnt[i][:, :, :n], dnT_d.rearrange("h p t -> p h t")[:, :, m0:m0 + n], [], ["dnt%d" % i], eng="sync")
                nav_ = naT_d.rearrange("(j two) d t -> two d j t", two=2)
                for tw in range(2):
                    C.dma(nat[i][tw * 64:(tw + 1) * 64, :, :n], nav_[tw][:, :, m0:m0 + n], [], ["nat%d" % i], eng="sync")
            for oc in range(8):
                j = oc % 2
                p1, k1 = pp.get()
                for k in range(8):
                    C.mm(p1[:, :n], wg[:, k, oc * 128:(oc + 1) * 128], hTt[i][:, k, :n], k == 0, k == 7,
                         ["wg", "hTt%d" % i], [k1])
                p2, k2 = pp.get()
                for k in range(8):
                    C.mm(p2[:, :n], wg[:, k, 1024 + oc * 128:1024 + (oc + 1) * 128], hTt[i][:, k, :n], k == 0, k == 7,
                         ["wg", "hTt%d" % i], [k2])
                p3, k3 = pp.get()
                for k in range(8):
                    C.mm(p3[:, :n], wpa[:, k, oc * 128:(oc + 1) * 128], dnt[i][:, k, :n], k == 0, k == 7,
                         ["wpa", "dnt%d" % i], [k3])
                p4, k4 = pp.get()
                for k in range(4):
                    C.mm(p4[:, :n], wpb[:, k, oc * 128:(oc + 1) * 128], nat[i][:, k, :n], k == 0, k == 3,
                         ["wpb", "nat%d" % i], [k4])
                C.act(s1[j][:, :n], p1[:, :n], AF.Sigmoid, [k1], ["s1%d" % j])
                C.act(s2[j][:, :n], p2[:, :n], AF.Sigmoid, [k2], ["s2%d" % j])
                C.tt(s1[j][:, :n], s1[j][:, :n], p3[:, :n], ALU.mult, ["s1%d" % j, k3], ["s1%d" % j])
                C.tt(s2[j][:, :n], s2[j][:, :n], p4[:, :n], ALU.mult, ["s2%d" % j, k4], ["s2%d" % j])
                C.tt(yT_all[:, oc, t0:t0 + n], s1[j][:, :n], s2[j][:, :n], ALU.add, ["s1%d" % j, "s2%d" % j], ["yT"],
                     eng="gpsimd")
        C.flush()


def small_rstd(C, ssum, rs, rows, inv_n, kin, kout):
    C.ts(rs[:rows, :], ssum[:rows, :], inv_n, ALU.mult, [kin], [kout], s2=EPS, op1=ALU.add)
    C.act(rs[:rows, :], rs[:rows, :], AF.Sqrt, [kout], [kout])
    C.recip(rs[:rows, :], rs[:rows, :], [kout], [kout])


def phase_C2(C, pp, yT_all, wout_d, xs_in_d, xs_mid_d, mods, gpost, gpre2, h2T_all, identb, identf,
             router_d, gates, es0):
    with contextlib.ExitStack() as es:
        wout = C.sb("wout", [128, 8, 1024], BF16, es)
        for k in range(8):
            C.dma(wout[:, k, :], wout_d[k * 128:(k + 1) * 128, :], [], ["wout"], eng="gpsimd")
        G1P = C.sb("G1P", [128, 2, 1024], F32, es)
        M2 = C.sb("M2", [128, 2, 1024], F32, es)
        for s in range(2):
            C.tt(G1P[:, s, :], mods[2][:, s, :], gpost[:], ALU.mult, ["mod2", "gpost"], ["G1P"])
            C.stt(M2[:, s, :], mods[4][:, s, :], 1.0, gpre2[:], ALU.add, ALU.mult, ["mod4", "gpre2"], ["M2"])
        SH2 = mods[3]
        moe = router_d is not None
        if moe:
            rt = C.sb("router", [128, 8, 8], F32, es)
            C.dma(rt[:], router_d.rearrange("(k p) e -> p k e", p=128), [], ["router"], eng="sync")
            lg = C.sb("logits", [128, 17, 8], F32, es)
            C.memset(lg[:], 0.0, ["logits"])
            hf = [C.sb("h2f", [128, 8, 128], F32, es) for _ in range(2)]
            for i_ in range(2):
                C.memset(hf[i_][:], 0.0, ["h2f%d" % i_])
        xt = [C.sb("xt", [128, 1024], F32, es) for _ in range(2)]
        tmp = [C.sb("tmpC", [128, 1024], F32, es) for _ in range(2)]
        hb = [C.sb("hbC", [128, 1024], BF16, es) for _ in range(2)]
        junk = C.sb("junkC", [128, 1024], F32, es)
        ssy = [C.sb("ssy", [128, 4], F32, es) for _ in range(2)]
        for t in range(17):
            rows = 128 if t < 16 else 64
            s = 0 if t < 16 else 1
            i = t % 2
            ki = "%d" % i
            C.dma(xt[i][:rows, :], xs_in_d[t * 128:t * 128 + rows, :], [], ["xt" + ki], eng="sync")
            ph = []
            for half in range(2):
                p, pk = pp.get()
                for oc in range(8):
                    C.mm(p[:rows, :], yT_all[:, oc, t * 128:t * 128 + rows], wout[:, oc, half * 512:(half + 1) * 512],
                         oc == 0, oc == 7, ["yT", "wout"], [pk])
                C.act(junk[:rows, half * 512:(half + 1) * 512], p[:rows, :], AF.Square, [pk], ["junkC", "ssy" + ki],
                      accum_out=ssy[i][:rows, half:half + 1])
                ph.append((p, pk))
            C.tt(ssy[i][:rows, 2:3], ssy[i][:rows, 0:1], ssy[i][:rows, 1:2], ALU.add, ["ssy" + ki], ["ssy" + ki])
            small_rstd(C, ssy[i][:, 2:3], ssy[i][:, 3:4], rows, 1.0 / D, "ssy" + ki, "ssy" + ki)
            for half in range(2):
                p, pk = ph[half]
                sl = slice(half * 512, (half + 1) * 512)
                C.stt(tmp[i][:rows, sl], p[:rows, :], ssy[i][:rows, 3:4], G1P[:rows, s, sl], ALU.mult, ALU.mult,
                      [pk, "ssy" + ki, "G1P"], ["tmpC" + ki])
            C.tt(xt[i][:rows, :], xt[i][:rows, :], tmp[i][:rows, :], ALU.add, ["xt" + ki, "tmpC" + ki], ["xt" + ki],
                 eng="gpsimd")
            C.dma(xs_mid_d[t * 128:t * 128 + rows, :], xt[i][:rows, :], ["xt" + ki], ["xs_mid"], eng="sync")
            C.act(junk[:rows, :], xt[i][:rows, :], AF.Square, ["xt" + ki], ["junkC", "ssy" + ki],
                  accum_out=ssy[i][:rows, 0:1])
            small_rstd(C, ssy[i][:, 0:1], ssy[i][:, 1:2], rows, 1.0 / D, "ssy" + ki, "ssy" + ki)
            C.stt(tmp[i][:rows, :], xt[i][:rows, :], ssy[i][:rows, 1:2], M2[:rows, s, :], ALU.mult, ALU.mult,
                  ["xt" + ki, "ssy" + ki, "M2"], ["tmpC" + ki])
            if not moe:
                C.tt(hb[i][:rows, :], tmp[i][:rows, :], SH2[:rows, s, :], ALU.add, ["tmpC" + ki, "mod3"], ["hbC" + ki],
                     eng="gpsimd")
                p, pk = pp.get()
                pb = p[:].bitcast(BF16)
                for k in range(8):
                    C.tr(pb[:, k * 128:k * 128 + rows], hb[i][:rows, k * 128:(k + 1) * 128], identb[:rows, :rows],
                         ["hbC" + ki, "identb"], [pk])
                C.cp(h2T_all[:, :, t * 128:t * 128 + rows], pb.rearrange("p (k t) -> p k t", k=8)[:, :, 0:rows],
                     [pk], ["h2T"], eng="scalar")
            else:
                C.tt(tmp[i][:rows, :], tmp[i][:rows, :], SH2[:rows, s, :], ALU.add, ["tmpC" + ki, "mod3"], ["tmpC" + ki],
                     eng="gpsimd")
                dbx = os.environ.get("DBGX", "")
                pa, pka = pp.get()
                pb_, pkb = pp.get()
                if "t" not in dbx:
                    for k in range(8):
                        p, pk = (pa, pka) if k < 4 else (pb_, pkb)
                        kk = k % 4
                        C.tr(p[:, kk * 128:kk * 128 + rows], tmp[i][:rows, k * 128:(k + 1) * 128], identf[:rows, :rows],
                             ["tmpC" + ki, "identf"], [pk])
                if "e" not in dbx:
                    for hh, (p, pk) in enumerate(((pa, pka), (pb_, pkb))):
                        src = p[:].rearrange("p (k t) -> p k t", k=4)[:, :, 0:rows]
                        C.cp(h2T_all[:, hh * 4:(hh + 1) * 4, t * 128:t * 128 + rows], src, [pk], ["h2T"], eng="scalar")
                        C.cp(hf[i][:, hh * 4:(hh + 1) * 4, 0:rows], src, [pk], ["h2f" + ki], eng="vector")
                if "r" not in dbx:
                    p, pk = pp.get()
                    for k in range(8):
                        C.mm(p[:, 0:8], hf[i][:, k, :], rt[:, k, :], k == 0, k == 7, ["h2f" + ki, "router"], [pk])
                    C.cp(lg[:rows, t, :], p[:rows, 0:8], [pk], ["logits"], eng="vector")
        if moe and os.environ.get("DBG", "") != "2":
            srt = C.sb("srt", [128, 8], F32, es)
            nm1 = C.sb("nm1", [128, 1], F32, es)
            msk = C.sb("msk", [128, 8], F32, es)
            ex = C.sb("ex", [128, 8], F32, es)
            den = C.sb("den", [128, 1], F32, es)
            for t in range(17):
                rows = 128 if t < 16 else 64
                C.P.op("vector", lambda e, t=t, rows=rows: e.max(out=srt[:rows, :], in_=lg[:rows, t, :]), ["logits"], ["srt"])
                C.ts(nm1[:rows, :], srt[:rows, 0:1], -1.0, ALU.mult, ["srt"], ["nm1"])
                C.ts(msk[:rows, :], lg[:rows, t, :], srt[:rows, 1:2], ALU.is_ge, ["logits", "srt"], ["msk"])
                C.act(ex[:rows, :], lg[:rows, t, :], AF.Exp, ["logits", "nm1"], ["ex"], bias=nm1[:rows, :])
                C.tt(ex[:rows, :], ex[:rows, :], msk[:rows, :], ALU.mult, ["ex", "msk"], ["ex"])
                C.red(den[:rows, :], ex[:rows, :], ALU.add, ["ex"], ["den"])
                C.recip(den[:rows, :], den[:rows, :], ["den"], ["den"])
                C.ts(gates[:rows, t, :], ex[:rows, :], den[:rows, 0:1], ALU.mult, ["ex", "den"], ["gates"])
        C.flush()


def phase_C3(C, pp, h2T_all, w1_d, w3_d, w2_d, n_exp, dff, gates, acc, es0):
    nchunks = dff // 128
    groups = []
    c0 = 0
    while c0 < nchunks:
        nch = min(4, nchunks - c0)
        groups.append((c0, nch))
        c0 += nch
    with contextlib.ExitStack() as es:
        w1g = [C.sb("w1g", [128, 8, 512], BF16, es) for _ in range(2)]
        w3g = [C.sb("w3g", [128, 8, 512], BF16, es) for _ in range(2)]
        w2g = [C.sb("w2g", [128, 4, 1024], BF16, es) for _ in range(2)]
        actT = [C.sb("actT", [128, 4, 512], BF16, es) for _ in range(2)]
        sa = [C.sb("sa", [128, 512], F32, es) for _ in range(2)]
        C.memset(acc[:], 0.0, ["acc"])
        it = 0
        for e in range(n_exp):
            for (c0, nch) in groups:
                wi = it % 2
                it += 1
                kw = "wffn%d" % wi
                cs = slice(c0 * 128, (c0 + nch) * 128)
                w1v = w1_d[e].rearrange("(k p) n -> p k n", p=128)
                w3v = w3_d[e].rearrange("(k p) n -> p k n", p=128)
                for k in range(8):
                    C.dma(w1g[wi][:, k, 0:nch * 128], w1v[:, k, cs], [], [kw + "a"], eng="gpsimd")
                    C.dma(w3g[wi][:, k, 0:nch * 128], w3v[:, k, cs], [], [kw + "b"], eng="gpsimd")
                for j in range(nch):
                    C.dma(w2g[wi][:, j, :], w2_d[e][(c0 + j) * 128:(c0 + j + 1) * 128, :], [], [kw + "c"], eng="gpsimd")
                for tt in range(5):
                    n = 512 if tt < 4 else 64
                    t0 = tt * 512
                    ai = tt % 2
                    ka = "actT%d" % ai
                    for j in range(nch):
                        pa, pka = pp.get()
                        for k in range(8):
                            C.mm(pa[:, :n], w1g[wi][:, k, j * 128:(j + 1) * 128], h2T_all[:, k, t0:t0 + n], k == 0, k == 7,
                                 [kw + "a", "h2T"], [pka])
                        pb, pkb = pp.get()
                        for k in range(8):
                            C.mm(pb[:, :n], w3g[wi][:, k, j * 128:(j + 1) * 128], h2T_all[:, k, t0:t0 + n], k == 0, k == 7,
                                 [kw + "b", "h2T"], [pkb])
                        sj = j % 2
                        C.act(sa[sj][:, :n], pa[:, :n], AF.Silu, [pka], ["sa%d" % sj])
                        C.tt(actT[ai][:, j, :n], sa[sj][:, :n], pb[:, :n], ALU.mult, ["sa%d" % sj, pkb], [ka])
                    nsub = 4 if tt < 4 else 1
                    for sub in range(nsub):
                        t = tt * 4 + sub
                        rows = 128 if tt < 4 else 64
                        for half in range(2):
                            p, pk = pp.get()
                            for j in range(nch):
                                C.mm(p[:rows, :], actT[ai][:, j, sub * 128:sub * 128 + rows],
                                     w2g[wi][:, j, half * 512:(half + 1) * 512], j == 0, j == nch - 1, [ka, kw + "c"], [pk])
                            sl = slice(half * 512, (half + 1) * 512)
                            if gates is None:
                                C.tt(acc[:rows, t, sl], acc[:rows, t, sl], p[:rows, :], ALU.add, ["acc", pk], ["acc"])
                            else:
                                C.stt(acc[:rows, t, sl], p[:rows, :], gates[:rows, t, e:e + 1], acc[:rows, t, sl],
                                      ALU.mult, ALU.add, [pk, "gates", "acc"], ["acc"])
        C.flush()


def phase_C4(C, pp, acc, xs_mid_d, xs_out_d, mods, gpost2, n_tiles, es0):
    with contextlib.ExitStack() as es:
        G2P = C.sb("G2P", [128, 2, 1024], F32, es)
        for s in range(2):
            C.tt(G2P[:, s, :], mods[5][:, s, :], gpost2[:], ALU.mult, ["mod5", "gpost2"], ["G2P"])
        xt = [C.sb("xt4", [128, 1024], F32, es) for _ in range(2)]
        junk = C.sb("junk4", [128, 1024], F32, es)
        ss = [C.sb("ss4", [128, 2], F32, es) for _ in range(2)]
        for t in range(n_tiles):
            rows = 128 if t < 16 else 64
            s = 0 if t < 16 else 1
            i = t % 2
            ki = "%d" % i
            C.dma(xt[i][:rows, :], xs_mid_d[t * 128:t * 128 + rows, :], ["xs_mid"], ["xt4" + ki], eng="sync")
            C.act(junk[:rows, :], acc[:rows, t, :], AF.Square, ["acc"], ["junk4", "ss4" + ki], accum_out=ss[i][:rows, 0:1])
            small_rstd(C, ss[i][:, 0:1], ss[i][:, 1:2], rows, 1.0 / D, "ss4" + ki, "ss4" + ki)
            C.stt(junk[:rows, :], acc[:rows, t, :], ss[i][:rows, 1:2], G2P[:rows, s, :], ALU.mult, ALU.mult,
                  ["acc", "ss4" + ki, "G2P"], ["junk4"])
            C.tt(xt[i][:rows, :], xt[i][:rows, :], junk[:rows, :], ALU.add, ["xt4" + ki, "junk4"], ["xt4" + ki], eng="gpsimd")
            C.dma(xs_out_d[t * 128:t * 128 + rows, :], xt[i][:rows, :], ["xt4" + ki], ["xs_out"], eng="sync")
        C.flush()


def _finish(C):
    C.P.finish()
    C.P.build()
    C.es.close()
    C.P.es.close()


def load_consts(C, identb_d, identf_d=None):
    identb = C.sb("identb", [128, 128], BF16)
    C.dma(identb[:], identb_d, [], ["identb"], eng="sync")
    identf = None
    if identf_d is not None:
        identf = C.sb("identf", [128, 128], F32)
        C.dma(identf[:], identf_d, [], ["identf"], eng="sync")
    ones_f = C.sb("ones_f", [128, 128], F32)
    C.memset(ones_f[:], 1.0, ["ones_f"])
    return identb, identf, ones_f


def run_A(C, pp, xs_d, cvT_d, ada_w_d, ada_b_d, gpre_d, hT_d, identb, ones_f):
    with contextlib.ExitStack() as es:
        xs = C.sb("xs", [128, 17, D], F32, es)
        hT = C.sb("hT", [128, 8, OWN], BF16, es)
        gpre = C.sb("gpre", [128, D], F32, es)
        C.dma(xs[:, 0:16, :], xs_d[0:2048, :].rearrange("(t p) d -> p t d", p=128), ["xs_out"], ["xs"], eng="sync")
        C.dma(xs[0:64, 16, :], xs_d[2048:2112, :], ["xs_out"], ["xs"], eng="sync")
        C.dma(gpre[:], gpre_d, [], ["gpre"], eng="sync")
        mods = compute_mods(C, pp, 0, [0, 1], cvT_d, ada_w_d, ada_b_d, ones_f, es)
        phase_A(C, pp, xs, hT, mods, gpre, identb, es)
        C.dma(hT_d.rearrange("(k p) t -> p k t", p=128), hT[:], ["hT"], ["hT_d"], eng="sync")
        C.flush()


def build_A():
    nc = bass.Bass("TRN2", target_bir_lowering=False)
    C = Ctx(nc)
    xs_d = C.dram("xs", [OWN, D], F32, "ExternalInput")
    cvT_d = C.dram("cvT", [128, 2, 8], F32, "ExternalInput")
    ada_w_d = C.dram("ada_w", [1, D, 6 * D], F32, "ExternalInput")
    ada_b_d = C.dram("ada_b", [1, 128, 6 * D], F32, "ExternalInput")
    gpre_d = C.dram("gpre", [128, D], F32, "ExternalInput")
    identb_d = C.dram("identb", [128, 128], BF16, "ExternalInput")
    hT_d = C.dram("hT", [D, OWN], BF16, "ExternalOutput")
    pp = PsumPool(C)
    identb, _, ones_f = load_consts(C, identb_d)
    run_A(C, pp, xs_d, cvT_d, ada_w_d, ada_b_d, gpre_d, hT_d, identb, ones_f)
    _finish(C)
    return nc


def build_C(moe, with_next_A, n_exp_dbg=None):
    nc = bass.Bass("TRN2", target_bir_lowering=False)
    C = Ctx(nc)
    xs_d = C.dram("xs", [OWN, D], F32, "ExternalInput")
    hT_d = C.dram("hT", [D, OWN], BF16, "ExternalInput")
    dnT_d = C.dram("dnT", [8, 128, OWN], BF16, "ExternalInput")
    naT_d = C.dram("naT", [4, 128, OWN], BF16, "ExternalInput")
    cvT_d = C.dram("cvT", [128, 2, 8], F32, "ExternalInput")
    ada_w_d = C.dram("ada_w", [1, D, 6 * D], F32, "ExternalInput")
    ada_b_d = C.dram("ada_b", [1, 128, 6 * D], F32, "ExternalInput")
    gpost_d = C.dram("gpost", [128, D], F32, "ExternalInput")
    gpre2_d = C.dram("gpre2", [128, D], F32, "ExternalInput")
    gpost2_d = C.dram("gpost2", [128, D], F32, "ExternalInput")
    wg_d = C.dram("wg", [D, 2048], F32, "ExternalInput")
    wpa_d = C.dram("wpa", [D, D], F32, "ExternalInput")
    wpb_d = C.dram("wpb", [512, D], F32, "ExternalInput")
    wout_d = C.dram("wout", [D, D], F32, "ExternalInput")
    identb_d = C.dram("identb", [128, 128], BF16, "ExternalInput")
    identf_d = C.dram("identf", [128, 128], F32, "ExternalInput")
    if moe:
        n_exp, dff = (n_exp_dbg or NE), DFE
        router_d = C.dram("router", [D, NE], F32, "ExternalInput")
    else:
        n_exp, dff = 1, DFF
        router_d = None
    w1_d = C.dram("w1", [n_exp, D, dff], F32, "ExternalInput")
    w3_d = C.dram("w3", [n_exp, D, dff], F32, "ExternalInput")
    w2_d = C.dram("w2", [n_exp, dff, D], F32, "ExternalInput")
    xs_mid_d = C.dram("xs_mid", [OWN, D], F32, "Internal")
    xs_out_d = C.dram("xs_out", [OWN, D], F32, "ExternalOutput")
    if with_next_A:
        ada_w2_d = C.dram("ada_w_n", [1, D, 6 * D], F32, "ExternalInput")
        ada_b2_d = C.dram("ada_b_n", [1, 128, 6 * D], F32, "ExternalInput")
        gpre_n_d = C.dram("gpre_n", [128, D], F32, "ExternalInput")
        hTn_d = C.dram("hT_n", [D, OWN], BF16, "ExternalOutput")
    pp = PsumPool(C)
    identb, identf, ones_f = load_consts(C, identb_d, identf_d)
    with contextlib.ExitStack() as es1:
        h2T_all = C.sb("h2T", [128, 8, OWN], BF16, es1)
        gates = C.sb("gates", [128, 17, 8], F32, es1) if moe else None
        gpost2 = C.sb("gpost2", [128, D], F32, es1)
        C.dma(gpost2[:], gpost2_d, [], ["gpost2"], eng="sync")
        with contextlib.ExitStack() as es2:
            mods = compute_mods(C, pp, 0, [2, 3, 4], cvT_d, ada_w_d, ada_b_d, ones_f, es2)
            gpost = C.sb("gpost", [128, D], F32, es2)
            gpre2 = C.sb("gpre2", [128, D], F32, es2)
            C.dma(gpost[:], gpost_d, [], ["gpost"], eng="sync")
            C.dma(gpre2[:], gpre2_d, [], ["gpre2"], eng="sync")
            yT_all = C.sb("yT", [128, 8, OWN], BF16, es2)
            phase_C1(C, pp, hT_d, dnT_d, naT_d, wg_d, wpa_d, wpb_d, yT_all, es2)
            phase_C2(C, pp, yT_all, wout_d, xs_d, xs_mid_d, mods, gpost, gpre2, h2T_all, identb, identf,
                     router_d, gates, es2)
            C.flush()
        with contextlib.ExitStack() as es3:
            acc = C.sb("acc", [128, 17, D], F32, es3)
            if os.environ.get("DBG", "") not in ("2", "3"):
                phase_C3(C, pp, h2T_all, w1_d, w3_d, w2_d, n_exp, dff, gates, acc, es3)
            else:
                C.memset(acc[:], 0.0, ["acc"])
            mods5 = compute_mods(C, pp, 0, [5], cvT_d, ada_w_d, ada_b_d, ones_f, es3)
            phase_C4(C, pp, acc, xs_mid_d, xs_out_d, mods5, gpost2, 17, es3)
            C.flush()
        C.flush()
    if with_next_A:
        run_A(C, pp, xs_out_d, cvT_d, ada_w2_d, ada_b2_d, gpre_n_d, hTn_d, identb, ones_f)
    _finish(C)
    return nc


TT_B = [(i * 512, 512) for i in range(16)] + [(8192, 256)]


def na_pattern(p):
    r = 2 * p
    ws = min(max(r - 4, 0), 118)
    pat = {0: 0, 2: 1, 124: 3, 126: 4}.get(r, 2)
    return ws, pat


def phase_NA(C, pp, b, hT_full_d, wnaq_d, wnak_d, wnav_d, nabias_d, namask_d, naT_d, identb):
    with contextlib.ExitStack() as es:
        wq = C.sb("wnq", [128, 8, 64], BF16, es)
        wk = C.sb("wnk", [128, 8, 64], BF16, es)
        wv = C.sb("wnv", [128, 8, 64], BF16, es)
        for w, d, kk in ((wq, wnaq_d, "wnq"), (wk, wnak_d, "wnk"), (wv, wnav_d, "wnv")):
            C.dma(w[:], d.rearrange("(k p) n -> p k n", p=128), [], [kk], eng="gpsimd")
        bias = C.sb("nabias", [128, 5, 640], F32, es)
        mask = C.sb("namask", [128, 5, 640], F32, es)
        C.dma(bias[:], nabias_d.rearrange("a p n -> p a n"), [], ["nabias"], eng="sync")
        C.dma(mask[:], namask_d.rearrange("a p n -> p a n"), [], ["namask"], eng="sync")
        C.tt(bias[:], bias[:], mask[:], ALU.add, ["nabias", "namask"], ["nabias"])
        qT = C.sb("naqT", [64, TB], BF16, es)
        kT = C.sb("nakT", [64, TB], BF16, es)
        vt = C.sb("nav", [128, NT_B, 64], BF16, es)
        oT = C.sb("naoT", [64, TB], BF16, es)
        hTt = [C.sb("hTtn", [128, 8, 512], BF16, es) for _ in range(2)]
        hv = hT_full_d[b].rearrange("(k p) t -> p k t", p=128)
        for ti, (t0, n) in enumerate(TT_B):
            i = ti % 2
            kh = "hTtn%d" % i
            C.dma(hTt[i][:, :, :n], hv[:, :, t0:t0 + n], [], [kh], eng="sync")
            p, pk = pp.get()
            for k in range(8):
                C.mm(p[0:64, :n], wq[:, k, :], hTt[i][:, k, :n], k == 0, k == 7, ["wnq", kh], [pk])
            C.act(qT[:, t0:t0 + n], p[0:64, :n], AF.Copy, [pk], ["naqT"], scale=0.125)
            p, pk = pp.get()
            for k in range(8):
                C.mm(p[0:64, :n], wk[:, k, :], hTt[i][:, k, :n], k == 0, k == 7, ["wnk", kh], [pk])
            C.cp(kT[:, t0:t0 + n], p[0:64, :n], [pk], ["nakT"], eng="vector")
            p, pk = pp.get()
            for sub in range(n // 128):
                for k in range(8):
                    C.mm(p[:, sub * 64:(sub + 1) * 64], hTt[i][:, k, sub * 128:(sub + 1) * 128], wv[:, k, :], k == 0, k == 7,
                         ["wnv", kh], [pk])
            C.cp(vt[:, t0 // 128:t0 // 128 + n // 128, :], p[:, 0:(n // 128) * 64].rearrange("p (s d) -> p s d", d=64),
                 [pk], ["nav"], eng="scalar")
        NS = 4
        Sb = [C.sb("naS", [128, 896], F32, es) for _ in range(NS)]
        Pb = [C.sb("naP", [128, 896], BF16, es) for _ in range(NS)]
        PT = [C.sb("naPT", [128, 7, 128], BF16, es) for _ in range(NS)]
        st = [C.sb("nast", [128, 4], F32, es) for _ in range(NS)]
        ob = [C.sb("naob", [128, 64], BF16, es) for _ in range(NS)]

        def na_tile(qi, i):
            ki = "%d" % i
            bX, kX = pp.t[2 * i], "psum%d" % (2 * i)
            bY, kY = pp.t[2 * i + 1], "psum%d" % (2 * i + 1)
            bXb = bX[:].bitcast(BF16)
            bYb = bY[:].bitcast(BF16)
            if qi < 2:
                q0 = qi * 128
                nk, nblk, vtiles = 256, 2, [0, 1]
                C.mm(bY[:, 0:256], qT[:, q0:q0 + 128], kT[:, 0:256], True, True, ["naqT", "nakT"], [kY])
                yield
                C.cp(Sb[i][:, 0:256], bY[:, 0:256], [kY], ["naS" + ki], eng="scalar")
                yield
            else:
                p_ = qi - 2
                ws, pat = na_pattern(p_)
                q0 = CTX + 128 * p_
                k0 = CTX + ws * 64
                nk, nblk = 896, 7
                vtiles = [2 + ws // 2 + j for j in range(5)] + [0, 1]
                C.mm(bX[:, 0:512], qT[:, q0:q0 + 128], kT[:, k0:k0 + 512], True, True, ["naqT", "nakT"], [kX])
                C.mm(bY[:, 0:128], qT[:, q0:q0 + 128], kT[:, k0 + 512:k0 + 640], True, True, ["naqT", "nakT"], [kY])
                C.mm(bY[:, 128:384], qT[:, q0:q0 + 128], kT[:, 0:256], True, True, ["naqT", "nakT"], [kY])
                yield
                C.tt(Sb[i][:, 0:512], bX[:, 0:512], bias[:, pat, 0:512], ALU.add, [kX, "nabias"], ["naS" + ki])
                C.cp(Sb[i][:, 640:896], bY[:, 128:384], [kY], ["naS" + ki], eng="scalar")
                yield
                C.tt(Sb[i][:, 512:640], bY[:, 0:128], bias[:, pat, 512:640], ALU.add, [kY, "nabias"], ["naS" + ki])
                yield
            C.red(st[i][:, 0:1], Sb[i][:, 0:nk], ALU.max, ["naS" + ki], ["nast" + ki])
            yield
            C.ts(st[i][:, 1:2], st[i][:, 0:1], -1.0, ALU.mult, ["nast" + ki], ["nast" + ki])
            yield
            C.act(Pb[i][:, 0:nk], Sb[i][:, 0:nk], AF.Exp, ["naS" + ki, "nast" + ki], ["naP" + ki, "nast" + ki],
                  bias=st[i][:, 1:2], accum_out=st[i][:, 2:3])
            yield
            C.recip(st[i][:, 3:4], st[i][:, 2:3], ["nast" + ki], ["nast" + ki])
            for j in range(nblk):
                C.tr(bXb[:, j * 128:(j + 1) * 128], Pb[i][:, j * 128:(j + 1) * 128], identb[:], ["naP" + ki, "identb"], [kX])
            yield
            C.cp(PT[i][:, 0:nblk, :], bXb[:, 0:nblk * 128].rearrange("p (j t) -> p j t", t=128), [kX], ["naPT" + ki],
                 eng="scalar")
            yield
            for j in range(nblk):
                C.mm(bY[:, 384:448], PT[i][:, j, :], vt[:, vtiles[j], :], j == 0, j == nblk - 1, ["naPT" + ki, "nav"], [kY])
            yield
            C.ts(ob[i][:], bY[:, 384:448], st[i][:, 3:4], ALU.mult, [kY, "nast" + ki], ["naob" + ki])
            yield
            C.tr(bYb[0:64, 896:1024], ob[i][:], identb[:], ["naob" + ki, "identb"], [kY])
            yield
            C.cp(oT[:, q0:q0 + 128], bYb[0:64, 896:1024], [kY], ["naoT"], eng="scalar")
            yield

        def na_interleave(gens):
            gens = list(gens)
            while gens:
                for g_ in list(gens):
                    try:
                        next(g_)
                    except StopIteration:
                        gens.remove(g_)

        tiles_all = list(range(2 + 64))
        for g0 in range(0, len(tiles_all), NS):
            na_interleave([na_tile(qi, qi % NS) for qi in tiles_all[g0:g0 + NS]])
        C.dma(naT_d[b], oT[:], ["naoT"], ["naT_d"], eng="sync")
        C.flush()


def phase_DN(C, pp, b, hT_full_d, wq_d, wk_d, wv_d, wzab_d, conv_d, dnpar_d, normw_d, cos_d, sin_d,
             cm_d, cmb_d, dnT_d, identb, identf, ones_f):
    with contextlib.ExitStack() as es:
        cm = C.sb("cm", [128, 5, 128], F32, es)
        cmb = C.sb("cmb", [128, 6, 128], BF16, es)
        C.dma(cm[:], cm_d.rearrange("a p n -> p a n"), [], ["cm"], eng="sync")
        C.dma(cmb[:], cmb_d.rearrange("a p n -> p a n"), [], ["cmb"], eng="sync")
        PM = [cmb[:, 0, :], cmb[:, 2, :]]
        NMt = [cmb[:, 1, :], cmb[:, 3, :]]
        rotT = cmb[:, 4, :]
        ones_b = cmb[:, 5, :]
        wqkv = C.sb("wqkv", [128, 3, 8, 128], BF16, es)
        for x, d in enumerate((wq_d, wk_d, wv_d)):
            C.dma(wqkv[:, x, :, :], d.rearrange("(k p) n -> p k n", p=128), [], ["wqkv"], eng="gpsimd")
        wzab = C.sb("wzab", [128, 8, 132], BF16, es)
        if isinstance(wzab_d, tuple):
            C.dma(wzab[:, :, 0:128], wzab_d[0].rearrange("(k p) n -> p k n", p=128), [], ["wzab"], eng="gpsimd")
            abblk = C.sb("abblk", [128, 8, 32], BF16, es)
            C.dma(abblk[:], wzab_d[1].rearrange("(k p) n -> p k n", p=128), [], ["abblk"], eng="gpsimd")
            C.cp(wzab[:, :, 128:132], abblk[:, :, wzab_d[2]:32:8], ["abblk"], ["wzab"], eng="vector")
        else:
            C.dma(wzab[:], wzab_d.rearrange("(k p) n -> p k n", p=128), [], ["wzab"], eng="gpsimd")
        convw = C.sb("convw", [128, 3, 5], F32, es)
        C.dma(convw[:], conv_d.rearrange("x p k -> p x k"), [], ["convw"], eng="sync")
        par = C.sb("dnpar", [128, 4], F32, es)
        C.dma(par[:], dnpar_d, [], ["dnpar"], eng="sync")
        normw = C.sb("normw", [128, 128], F32, es)
        C.dma(normw[:], normw_d, [], ["normw"], eng="sync")
        qT = C.sb("dqT", [128, TB], BF16, es)
        kT = C.sb("dkT", [128, TB], BF16, es)
        ktok = C.sb("dktok", [128, NT_B, 128], BF16, es)
        vtok = C.sb("dvtok", [128, NT_B, 128], BF16, es)
        sz = C.sb("dsz", [128, NT_B, 128], BF16, es)
        abt = C.sb("dab", [128, NT_B, 4], F32, es)
        RW = 2 + CTX + 2 + 2 + SEQ + 2
        OFFC, OFFL = 2, 2 + CTX + 2 + 2
        with contextlib.ExitStack() as es1:
            raw = C.sb("draw", [128, 3, RW], BF16, es1)
            for x in range(3):
                C.memset(raw[:, x, 0:2], 0.0, ["draw"], eng="gpsimd")
                C.memset(raw[:, x, OFFC + CTX:OFFL], 0.0, ["draw"], eng="gpsimd")
                C.memset(raw[:, x, OFFL + SEQ:RW], 0.0, ["draw"], eng="gpsimd")
            hTt = [C.sb("hTtd", [128, 8, 512], BF16, es1) for _ in range(2)]
            ztmp = [C.sb("ztmp", [128, 128], F32, es1) for _ in range(2)]
            hv = hT_full_d[b].rearrange("(k p) t -> p k t", p=128)
            tiles = [(0, 256)] + [(CTX + i * 512, 512) for i in range(16)]
            for ti, (t0, n) in enumerate(tiles):
                i = ti % 2
                kh = "hTtd%d" % i
                C.dma(hTt[i][:, :, :n], hv[:, :, t0:t0 + n], [], [kh], eng="sync")
                ro = OFFC + t0 if t0 < CTX else OFFL + (t0 - CTX)
                for x in range(3):
                    p, pk = pp.get()
                    for k in range(8):
                        C.mm(p[:, :n], wqkv[:, x, k, :], hTt[i][:, k, :n], k == 0, k == 7, ["wqkv", kh], [pk])
                    C.cp(raw[:, x, ro:ro + n], p[:, :n], [pk], ["draw"], eng=("scalar" if x != 1 else "vector"))
                for sub in range(n // 128):
                    tl = t0 // 128 + sub
                    p, pk = pp.get()
                    for k in range(8):
                        C.mm(p[:, 0:132], hTt[i][:, k, sub * 128:(sub + 1) * 128], wzab[:, k, :], k == 0, k == 7,
                             ["wzab", kh], [pk])
                    zi = tl % 2
                    C.act(ztmp[zi][:], p[:, 0:128], AF.Silu, [pk], ["ztmp%d" % zi])
                    C.tt(sz[:, tl, :], ztmp[zi][:], normw[:], ALU.mult, ["ztmp%d" % zi, "normw"], ["dsz"], eng="gpsimd")
                    C.cp(abt[:, tl, :], p[:, 128:132], [pk], ["dab"], eng="vector")
            acc = [C.sb("cacc", [128, 512], F32, es1) for _ in range(2)]
            sq = [C.sb("csq", [128, 512], BF16, es1) for _ in range(2)]
            rn = [C.sb("crn", [128, 512], F32, es1) for _ in range(2)]
            t1 = [C.sb("ct1", [128, 512], F32, es1) for _ in range(2)]
            t2 = [C.sb("ct2", [128, 512], F32, es1) for _ in range(2)]
            cs = [C.sb("ccs", [128, 2, 512], F32, es1) for _ in range(2)]
            yb3 = [[C.sb("cyb3", [128, 512], BF16, es1) for _ in range(3)] for _ in range(2)]
            for ti, (t0, n) in enumerate(tiles):
                lat = t0 >= CTX
                ro = OFFC + t0 if not lat else OFFL + (t0 - CTX)
                ci = ti % 2
                if lat:
                    C.dma(cs[ci][:, 0, :n], cos_d[:, t0 - CTX:t0 - CTX + n], [], ["ccs%d" % ci], eng="sync")
                    C.dma(cs[ci][:, 1, :n], sin_d[:, t0 - CTX:t0 - CTX + n], [], ["ccs%d" % ci], eng="sync")
                for x in range(3):
                    i = x % 2
                    ki = "%d" % i
                    ybx = yb3[ci][x]
                    ky = "cyb3_%d_%d" % (ci, x)
                    C.ts(acc[i][:, :n], raw[:, x, ro - 2:ro - 2 + n], convw[:, x, 0:1], ALU.mult, ["draw", "convw"], ["cacc" + ki])
                    for tap in range(1, 5):
                        C.stt(acc[i][:, :n], raw[:, x, ro - 2 + tap:ro - 2 + tap + n], convw[:, x, tap:tap + 1], acc[i][:, :n],
                              ALU.mult, ALU.add, ["draw", "convw", "cacc" + ki], ["cacc" + ki])
                    C.act(ybx[:, :n], acc[i][:, :n], AF.Silu, ["cacc" + ki], [ky])
                for x in (2, 0, 1):
                    i = x % 2
                    ki = "%d" % i
                    ybx = yb3[ci][x]
                    ky = "cyb3_%d_%d" % (ci, x)
                    if x == 2:
                        for sub in range(n // 128):
                            tl = t0 // 128 + sub
                            p, pk = pp.get()
                            pb = p[:].bitcast(BF16)
                            C.tr(pb[:, 0:128], ybx[:, sub * 128:(sub + 1) * 128], identb[:], [ky, "identb"], [pk])
                            C.cp(vtok[:, tl, :], pb[:, 0:128], [pk], ["dvtok"], eng="scalar")
                        continue
                    dst = qT if x == 0 else kT
                    dk = "dqT" if x == 0 else "dkT"
                    scale = 128.0 ** -0.5 if x == 0 else 1.0
                    C.tt(sq[i][:, :n], ybx[:, :n], ybx[:, :n], ALU.mult, [ky], ["csq" + ki], eng="gpsimd")
                    p, pk = pp.get()
                    C.mm(p[:, :n], ones_b, sq[i][:, :n], True, True, ["cmb", "csq" + ki], [pk])
                    C.act(rn[i][:, :n], p[:, :n], AF.Ln, [pk], ["crn" + ki], bias=EPS)
                    C.act(rn[i][:, :n], rn[i][:, :n], AF.Exp, ["crn" + ki], ["crn" + ki], scale=-0.5)
                    if lat:
                        p2, pk2 = pp.get()
                        C.mm(p2[:, :n], rotT, ybx[:, :n], True, True, ["cmb", ky], [pk2])
                        C.tt(t1[i][:, :n], ybx[:, :n], cs[ci][:, 0, :n], ALU.mult, [ky, "ccs%d" % ci], ["ct1" + ki],
                             eng="gpsimd")
                        C.tt(t2[i][:, :n], p2[:, :n], cs[ci][:, 1, :n], ALU.mult, [pk2, "ccs%d" % ci], ["ct2" + ki])
                        C.tt(t1[i][:, :n], t1[i][:, :n], t2[i][:, :n], ALU.add, ["ct1" + ki, "ct2" + ki], ["ct1" + ki], eng="gpsimd")
                        C.stt(dst[:, t0:t0 + n], t1[i][:, :n], scale, rn[i][:, :n], ALU.mult, ALU.mult,
                              ["ct1" + ki, "crn" + ki], [dk])
                    else:
                        C.stt(dst[:, t0:t0 + n], ybx[:, :n], scale, rn[i][:, :n], ALU.mult, ALU.mult,
                              [ky, "crn" + ki], [dk])
                    if x == 1:
                        for sub in range(n // 128):
                            tl = t0 // 128 + sub
                            p, pk = pp.get()
                            pb = p[:].bitcast(BF16)
                            C.tr(pb[:, 0:128], kT[:, t0 + sub * 128:t0 + (sub + 1) * 128], identb[:], ["dkT", "identb"], [pk])
                            C.cp(ktok[:, tl, :], pb[:, 0:128], [pk], ["dktok"], eng="scalar")
            C.flush()
        NT = NT_B
        G = C.sb("dG", [128, 2, NT], F32, es)
        BETA = C.sb("dBETA", [128, 2, NT], F32, es)
        GC = C.sb("dGC", [128, 2, NT], F32, es)
        NGC = C.sb("dNGC", [128, 2, NT], F32, es)
        NBT = C.sb("dNB", [128, 2, NT], F32, es)
        BEG = C.sb("dBEG", [128, 2, NT], F32, es)
        EDK = C.sb("dEDK", [128, 2, NT], F32, es)
        EGL = C.sb("dEGL", [128, 2, 2, NT], F32, es)
        nea = C.sb("dnea", [128, 2], F32, es)
        C.act(nea[:], par[:, 0:2], AF.Exp, ["dnpar"], ["dnea"])
        C.ts(nea[:], nea[:], -1.0, ALU.mult, ["dnea"], ["dnea"])
        for d in range(2):
            C.act(G[:, d, :], abt[:, :, d], AF.Exp, ["dab", "dnpar"], ["dG"], bias=par[:, 2 + d:3 + d])
            C.act(G[:, d, :], G[:, d, :], AF.Ln, ["dG"], ["dG"], bias=1.0)
            C.ts(G[:, d, :], G[:, d, :], nea[:, d:d + 1], ALU.mult, ["dG", "dnea"], ["dG"])
            C.act(BETA[:, d, :], abt[:, :, 2 + d], AF.Sigmoid, ["dab"], ["dBETA"])
            p, pk = pp.get()
            C.mm(p[:, 0:NT], cm[:, d, :], G[:, d, :], True, True, ["cm", "dG"], [pk])
            C.cp(GC[:, d, :], p[:, 0:NT], [pk], ["dGC"], eng="vector")
            p, pk = pp.get()
            C.mm(p[:, 0:NT], cm[:, 2, :], G[:, d, :], True, True, ["cm", "dG"], [pk])
            C.tt(EDK[:, d, :], p[:, 0:NT], GC[:, d, :], ALU.subtract, [pk, "dGC"], ["dEDK"])
            C.act(EDK[:, d, :], EDK[:, d, :], AF.Exp, ["dEDK"], ["dEDK"])
            for hf in range(2):
                p, pk = pp.get()
                C.mm(p[:, 0:NT], cm[:, 3 + hf, :], G[:, d, :], True, True, ["cm", "dG"], [pk])
                C.act(EGL[:, d, hf, :], p[:, 0:NT], AF.Exp, [pk], ["dEGL"])
        C.ts(NGC[:], GC[:], -1.0, ALU.mult, ["dGC"], ["dNGC"])
        C.ts(NBT[:], BETA[:], -1.0, ALU.mult, ["dBETA"], ["dNB"])
        C.act(BEG[:], GC[:], AF.Exp, ["dGC"], ["dBEG"])
        C.tt(BEG[:], BEG[:], BETA[:], ALU.mult, ["dBEG", "dBETA"], ["dBEG"])
        obuf = C.sb("dobuf", [128, NT, 128], F32, es)
        C.memset(obuf[:], 0.0, ["ob%d" % t for t in range(NT)], eng="gpsimd")
        S32 = [C.sb("dS32", [128, 128], F32, es) for _ in range(2)]
        S16 = [C.sb("dS16", [128, 128], BF16, es) for _ in range(2)]
        for d in range(2):
            C.memset(S32[d][:], 0.0, ["S32_%d" % d])
            C.memset(S16[d][:], 0.0, ["S16_%d" % d])
        NPAR = 3
        NSLOT = 2 * NPAR
        ring = []
        for d in range(2):
            ring.append([dict(u=C.sb("pu", [128, 128], F32, es), wT=C.sb("pw", [128, 128], BF16, es),
                              at=C.sb("pat", [128, 128], BF16, es), qg=C.sb("pqg", [128, 128], BF16, es),
                              kd=C.sb("pkd", [128, 128], BF16, es), vn=C.sb("pvn", [128, 128], BF16, es))
                         for _ in range(NSLOT)])
        tmpf = {n_: [C.sb("pt" + n_, [128, 128], F32, es) for _ in range(2 * NPAR)] for n_ in ("dg", "E", "ET", "EG")}
        tmpb = {n_: [C.sb("pb" + n_, [128, 128], BF16, es) for _ in range(4 * NPAR)] for n_ in ("N", "X", "PT")}
        tmpc = {n_: [C.sb("pc" + n_, [128, 128], BF16, es) for _ in range(2 * NPAR)] for n_ in ("kbg", "vb", "dgh", "dgl")}
        pmf = C.sb("pmf", [128, 4, 128], F32, es)
        C.cp(pmf[:], cmb[:, 0:4, :], ["cmb"], ["pmf"], eng="vector")
        PMf = [pmf[:, 0, :], pmf[:, 2, :]]
        NMf = [pmf[:, 1, :], pmf[:, 3, :]]
        GCHb = C.sb("dGCHb", [128, 2, NT], BF16, es)
        GCH = C.sb("dGCH", [128, 2, NT], F32, es)
        GCL = C.sb("dGCL", [128, 2, NT], F32, es)
        C.cp(GCHb[:], GC[:], ["dGC"], ["dGCHb"], eng="vector")
        C.cp(GCH[:], GCHb[:], ["dGCHb"], ["dGCH"], eng="vector")
        C.tt(GCL[:], GC[:], GCH[:], ALU.subtract, ["dGC", "dGCH"], ["dGCL"])

        def bankq(bi, qi):
            b_ = pp.t[bi]
            return b_[:, qi * 128:(qi + 1) * 128], b_[:].bitcast(BF16)[:, qi * 256:qi * 256 + 128], "psum%d" % bi

        def prep(t, d, slot, sk, par):
            R = ring[d][slot]
            c0 = t * 128
            q_ = NPAR * d + par
            kd = "%d" % q_
            Q0, _, kB = bankq(q_, 0)
            Q1, _, _ = bankq(q_, 1)
            Q2, _, _ = bankq(q_, 2)
            Q3, Q3b, _ = bankq(q_, 3)
            C.mm(Q0, kT[:, c0:c0 + 128], kT[:, c0:c0 + 128], True, True, ["dkT"], [kB])
            C.mm(Q1, kT[:, c0:c0 + 128], qT[:, c0:c0 + 128], True, True, ["dkT", "dqT"], [kB])
            dgh, dgl = tmpc["dgh"][q_], tmpc["dgl"][q_]
            C.act(dgh[:], identf[:], AF.Copy, ["identf", "dGCH"], ["dgh" + kd], scale=GCH[:, d, t:t + 1])
            C.act(dgl[:], identf[:], AF.Copy, ["identf", "dGCL"], ["dgl" + kd], scale=GCL[:, d, t:t + 1])
            kbg, vb = tmpc["kbg"][q_], tmpc["vb"][q_]
            C.act(kbg[:], ktok[:, t, :], AF.Copy, ["dktok", "dBEG"], ["kbg" + kd], scale=BEG[:, d, t:t + 1])
            C.ts(vb[:], vtok[:, t, :], BETA[:, d, t:t + 1], ALU.mult, ["dvtok", "dBETA"], ["vb" + kd], eng="vector")
            C.act(R["kd"][:], ktok[:, t, :], AF.Copy, ["dktok", "dEDK"], [sk + "kd"], scale=EDK[:, d, t:t + 1])
            yield
            C.mm(Q2, ones_b, dgh[:], True, False, ["cmb", "dgh" + kd], [kB])
            C.mm(Q2, ones_b, dgl[:], False, True, ["cmb", "dgl" + kd], [kB])
            yield
            E, ET, EG = tmpf["E"][q_], tmpf["ET"][q_], tmpf["EG"][q_]
            bs = tmpf["dg"][q_]
            C.cp(bs[:], Q2, [kB], ["bs" + kd], eng="scalar")
            yield
            C.tt(E[:], bs[:], PMf[d], ALU.add, ["bs" + kd, "pmf"], ["E" + kd], eng="gpsimd")
            C.tt(ET[:], bs[:], NMf[d], ALU.add, ["bs" + kd, "pmf"], ["ET" + kd], eng="gpsimd")
            yield
            C.act(E[:], E[:], AF.Exp, ["E" + kd, "dGC"], ["E" + kd], bias=GC[:, d, t:t + 1], scale=-1.0)
            C.act(ET[:], ET[:], AF.Exp, ["ET" + kd, "dNGC"], ["ET" + kd], bias=NGC[:, d, t:t + 1], scale=1.0)
            C.act(EG[:], bs[:], AF.Exp, ["bs" + kd], ["EG" + kd])
            yield
            N = tmpb["N"]
            X = tmpb["X"]
            PTb = tmpb["PT"]
            o = 2 * q_
            C.stt(N[o][:], Q0, NBT[:, d, t:t + 1], E[:], ALU.mult, ALU.mult, [kB, "dNB", "E" + kd], ["N%d" % o])
            C.tt(R["at"][:], Q1, ET[:], ALU.mult, [kB, "ET" + kd], [sk + "at"])
            yield
            C.tr(Q3b, N[o][:], identb[:], ["N%d" % o, "identb"], [kB])
            yield
            C.cp(X[o][:], Q3b, [kB], ["X%d" % o], eng="scalar")
            C.tt(PTb[o][:], Q3b, identb[:], ALU.add, [kB, "identb"], ["PT%d" % o])
            yield
            C.tt(R["qg"][:], qT[:, c0:c0 + 128], EG[:], ALU.mult, ["dqT", "EG" + kd], [sk + "qg"], eng="gpsimd")
            cur = 0
            for lev in range(1, 6):
                a, bb = o + cur, o + 1 - cur
                C.mm(Q0, X[a][:], N[a][:], True, True, ["X%d" % a, "N%d" % a], [kB])
                if lev < 5:
                    C.mm(Q1, N[a][:], X[a][:], True, True, ["X%d" % a, "N%d" % a], [kB])
                yield
                C.cp(N[bb][:], Q0, [kB], ["N%d" % bb], eng="scalar")
                if lev < 5:
                    C.cp(X[bb][:], Q1, [kB], ["X%d" % bb], eng="vector")
                yield
                C.mm(Q2, N[bb][:], PTb[a][:], True, True, ["N%d" % bb, "PT%d" % a], [kB])
                yield
                C.tt(PTb[bb][:], PTb[a][:], Q2, ALU.add, ["PT%d" % a, kB], ["PT%d" % bb])
                yield
                cur = 1 - cur
            fin = o + cur
            C.mm(Q0, PTb[fin][:], vb[:], True, True, ["PT%d" % fin, "vb" + kd], [kB])
            C.mm(Q1, kbg[:], PTb[fin][:], True, True, ["PT%d" % fin, "kbg" + kd], [kB])
            yield
            C.cp(R["u"][:], Q0, [kB], [sk + "u"], eng="scalar")
            C.cp(R["wT"][:], Q1, [kB], [sk + "wT"], eng="vector")
            yield

        def step(t, hf, d, slot, sk):
            R = ring[d][slot]
            sl = slice(64 * hf, 64 * hf + 64)
            kd = "%d" % d
            bS = pp.t[2 * NPAR + d]
            kS = "psum%d" % (2 * NPAR + d)
            pS, pO, pD = bS[:, 0:128], bS[:, 128:256], bS[:, 256:384]
            C.mm(pS[sl, :], R["wT"][:, sl], S16[d][:], True, True, [sk + "wT", "S16_" + kd], [kS])
            yield
            C.tt(R["vn"][sl, :], R["u"][sl, :], pS[sl, :], ALU.subtract, [sk + "u", kS], [sk + "vn"])
            yield
            C.mm(pO[sl, :], R["qg"][:, sl], S16[d][:], True, False, [sk + "qg", "S16_" + kd], [kS])
            C.mm(pO[sl, :], R["at"][sl, sl], R["vn"][sl, :], False, True, [sk + "at", sk + "vn"], [kS])
            C.mm(pD, R["kd"][sl, :], R["vn"][sl, :], True, True, [sk + "kd", sk + "vn"], [kS])
            yield
            C.stt(S32[d][:], S32[d][:], EGL[:, d, hf, t:t + 1], pD, ALU.mult, ALU.add,
                  ["S32_" + kd, "dEGL", kS], ["S32_" + kd])
            yield
            C.cp(S16[d][:], S32[d][:], ["S32_" + kd], ["S16_" + kd], eng="scalar")
            C.tt(obuf[sl, t, :], obuf[sl, t, :], pO[sl, :], ALU.add, ["ob%d" % t, kS], ["ob%d" % t])
            yield

        def steps(d, idxs, order):
            for i_ in idxs:
                t = order[i_]
                slot = i_ % NSLOT
                sk = "r%d_%d_" % (d, slot)
                for hf in ((0, 1) if d == 0 else (1, 0)):
                    yield from step(t, hf, d, slot, sk)

        def interleave(gens):
            gens = list(gens)
            while gens:
                for g_ in list(gens):
                    try:
                        next(g_)
                    except StopIteration:
                        gens.remove(g_)

        order_f = list(range(NT))
        order_b = [1, 0] + list(range(NT - 1, 1, -1))
        orders = (order_f, order_b)

        def preps_for(j):
            gl_ = []
            for i_ in range(NPAR * j, NPAR * j + NPAR):
                for d in range(2):
                    slot = i_ % NSLOT
                    gl_.append(prep(orders[d][i_], d, slot, "r%d_%d_" % (d, slot), i_ % NPAR))
            return gl_

        assert NT % NPAR == 0
        NP = NT // NPAR
        interleave(preps_for(0))
        for j in range(NP):
            gl_ = preps_for(j + 1) if j + 1 < NP else []
            gl_.append(steps(0, range(NPAR * j, NPAR * j + NPAR), order_f))
            gl_.append(steps(1, range(NPAR * j, NPAR * j + NPAR), order_b))
            interleave(gl_)
        with contextlib.ExitStack() as es2:
            sqb = vtok
            ssq = C.sb("dssq", [128, NT], F32, es2)
            oT = qT
            on = [C.sb("don", [128, 128], BF16, es2) for _ in range(2)]
            allob = ["ob%d" % t for t in range(NT)]
            C.tt(sqb[:], obuf[:], obuf[:], ALU.mult, allob, ["dvtok"])
            C.red(ssq[:], sqb[:], ALU.add, ["dvtok"], ["dssq"])
            C.ts(ssq[:], ssq[:], 1.0 / 128, ALU.mult, ["dssq"], ["dssq"], s2=EPS, op1=ALU.add)
            C.act(ssq[:], ssq[:], AF.Sqrt, ["dssq"], ["dssq"])
            C.recip(ssq[:], ssq[:], ["dssq"], ["dssq"])
            for t in range(NT):
                i = t % 2
                C.stt(on[i][:], obuf[:, t, :], ssq[:, t:t + 1], sz[:, t, :], ALU.mult, ALU.mult,
                      ["ob%d" % t, "dssq", "dsz"], ["don%d" % i])
                p, pk = pp.get()
                pb = p[:].bitcast(BF16)
                C.tr(pb[:, 0:128], on[i][:], identb[:], ["don%d" % i, "identb"], [pk])
                C.cp(oT[:, t * 128:(t + 1) * 128], pb[:, 0:128], [pk], ["dqT"], eng="scalar")
            C.dma(dnT_d[b], oT[:], ["dqT"], ["dnT_d"], eng="sync")
            C.flush()
        C.flush()


OFF_Q, OFF_K, OFF_V, OFF_Z, OFF_AB, OFF_NQ, OFF_NK, OFF_NV, OFF_GD, OFF_GN = (
    0, 1024, 2048, 3072, 4096, 4128, 4640, 5152, 5664, 6688)

_CONST_CACHE = {}


def b_consts():
    if "b" in _CONST_CACHE:
        return _CONST_CACHE["b"]
    idx = np.arange(128)
    same = (idx[:, None] // 64) == (idx[None, :] // 64)
    a, bq = idx[:, None], idx[None, :]
    cm = np.stack([
        same & (a <= bq),
        same & (a >= bq),
        same,
        np.broadcast_to(a < 64, (128, 128)),
        np.broadcast_to(a >= 64, (128, 128)),
    ]).astype(np.float32)
    big = 30000.0
    cmb = np.stack([
        np.where(same & (a > bq), 0.0, big),
        np.where(same & (bq >= a), 0.0, -big),
        np.where(same & (a < bq), 0.0, big),
        np.where(same & (bq <= a), 0.0, -big),
        np.where(a == bq + 64, -1.0, 0.0) + np.where(a == bq - 64, 1.0, 0.0),
        np.ones((128, 128)),
    ]).astype(NPBF)
    t = np.arange(SEQ)
    row = (t // 64).astype(np.float32)
    col = (t % 64).astype(np.float32)
    inv = (np.float32(10000.0) ** (-np.arange(32, dtype=np.float32) / np.float32(32))).astype(np.float32)
    ang = np.concatenate([row[None, :] * inv[:, None], col[None, :] * inv[:, None]], 0)
    ang = np.concatenate([ang, ang], 0).astype(np.float32)
    cosT = np.cos(ang).astype(np.float32)
    sinT = np.sin(ang).astype(np.float32)
    reps = [0, 2, 10, 124, 126]
    ri = np.zeros((5, 128, 640), np.int64)
    ci = np.zeros((5, 128, 640), np.int64)
    mk = np.zeros((5, 128, 640), np.float32)
    qq = np.arange(128)
    kk = np.arange(640)
    dr, cq = qq // 64, qq % 64
    kr, ck = kk // 64, kk % 64
    for pi, r in enumerate(reps):
        ws = min(max(r - 4, 0), 118)
        R_ = r + dr
        r0 = np.clip(R_ - 4, 0, 120)
        krow = ws + kr
        okr = (krow[None, :] >= r0[:, None]) & (krow[None, :] < r0[:, None] + 8)
        c0 = np.clip(cq - 8, 0, 48)
        okc = (ck[None, :] >= c0[:, None]) & (ck[None, :] < c0[:, None] + 16)
        ok = okr & okc
        ri[pi] = np.clip(krow[None, :] - R_[:, None] + 7, 0, 14)
        ci[pi] = np.clip(ck[None, :] - cq[:, None] + 15, 0, 30)
        mk[pi] = np.where(ok, 0.0, NEG)
    out = dict(cm=cm, cmb=cmb, cosT=cosT, sinT=sinT, ri=ri, ci=ci, mk=mk,
               identb=np.eye(128).astype(NPBF), identf=np.eye(128, dtype=np.float32))
    _CONST_CACHE["b"] = out
    return out


def host_inputs_B(inp, l, hd, hT_full):
    k = b_consts()
    w_in = inp["w_in"][l]
    sl = lambda o, n: np.ascontiguousarray(w_in[:, o + hd * n:o + (hd + 1) * n])
    ab_cols = [OFF_AB + hd, OFF_AB + 8 + hd, OFF_AB + 16 + hd, OFF_AB + 24 + hd]
    rpb = inp["na_rpb"][l, hd]
    conv = inp["dn_conv"][l]
    im = dict(
        hT_full=hT_full,
        wnaq=sl(OFF_NQ, 64), wnak=sl(OFF_NK, 64), wnav=sl(OFF_NV, 64),
        nabias=np.ascontiguousarray(rpb[k["ri"], k["ci"]]).astype(np.float32), namask=k["mk"],
        wq=sl(OFF_Q, 128), wk=sl(OFF_K, 128), wv=sl(OFF_V, 128),
        wzab=np.ascontiguousarray(np.concatenate([w_in[:, OFF_Z + hd * 128:OFF_Z + (hd + 1) * 128], w_in[:, ab_cols]], 1)),
        conv=np.ascontiguousarray(np.stack([conv[x * 1024 + hd * 128:x * 1024 + (hd + 1) * 128] for x in range(3)])),
        dnpar=np.ascontiguousarray(np.broadcast_to(np.array([inp["dn_a_log"][l, 0, hd], inp["dn_a_log"][l, 1, hd],
                                                             inp["dn_dt_bias"][l, 0, hd], inp["dn_dt_bias"][l, 1, hd]],
                                                            np.float32)[None], (128, 4))),
        normw=np.ascontiguousarray(np.broadcast_to(inp["dn_norm"][l][None], (128, 128))).astype(np.float32),
        cosT=k["cosT"], sinT=k["sinT"], cm=k["cm"], cmb=k["cmb"], identb=k["identb"], identf=k["identf"],
    )
    return im, None


def build_B(do_na=True, do_dn=True, nbatch=NB):
    nc = bass.Bass("TRN2", target_bir_lowering=False)
    C = Ctx(nc)
    hT_full_d = C.dram("hT_full", [NB, D, TB], BF16, "ExternalInput")
    wnaq_d = C.dram("wnaq", [D, 64], F32, "ExternalInput")
    wnak_d = C.dram("wnak", [D, 64], F32, "ExternalInput")
    wnav_d = C.dram("wnav", [D, 64], F32, "ExternalInput")
    nabias_d = C.dram("nabias", [5, 128, 640], F32, "ExternalInput")
    namask_d = C.dram("namask", [5, 128, 640], F32, "ExternalInput")
    wq_d = C.dram("wq", [D, 128], F32, "ExternalInput")
    wk_d = C.dram("wk", [D, 128], F32, "ExternalInput")
    wv_d = C.dram("wv", [D, 128], F32, "ExternalInput")
    wzab_d = C.dram("wzab", [D, 132], F32, "ExternalInput")
    conv_d = C.dram("conv", [3, 128, 5], F32, "ExternalInput")
    dnpar_d = C.dram("dnpar", [128, 4], F32, "ExternalInput")
    normw_d = C.dram("normw", [128, 128], F32, "ExternalInput")
    cos_d = C.dram("cosT", [128, SEQ], F32, "ExternalInput")
    sin_d = C.dram("sinT", [128, SEQ], F32, "ExternalInput")
    cm_d = C.dram("cm", [5, 128, 128], F32, "ExternalInput")
    cmb_d = C.dram("cmb", [6, 128, 128], BF16, "ExternalInput")
    identb_d = C.dram("identb", [128, 128], BF16, "ExternalInput")
    identf_d = C.dram("identf", [128, 128], F32, "ExternalInput")
    naT_d = C.dram("naT", [NB, 64, TB], BF16, "ExternalOutput")
    dnT_d = C.dram("dnT", [NB, 128, TB], BF16, "ExternalOutput")
    pp = PsumPool(C)
    identb, identf, ones_f = load_consts(C, identb_d, identf_d)
    for b in range(nbatch):
        if do_na:
            phase_NA(C, pp, b, hT_full_d, wnaq_d, wnak_d, wnav_d, nabias_d, namask_d, naT_d, identb)
        if do_dn:
            phase_DN(C, pp, b, hT_full_d, wq_d, wk_d, wv_d, wzab_d, conv_d, dnpar_d, normw_d, cos_d, sin_d,
                     cm_d, cmb_d, dnT_d, identb, identf, ones_f)
    _finish(C)
    return nc


def _rep(v, n=128):
    return np.ascontiguousarray(np.broadcast_to(np.asarray(v, np.float32)[None], (n,) + tuple(v.shape)))


def _run(nc, in_maps):
    res = run_bass_kernel_spmd(nc, in_maps, core_ids=list(range(len(in_maps))))
    return res.results


def _assemble_hT(hT_sh):
    out = np.empty((NB, D, TB), NPBF)
    for c in range(8):
        b, q = c // 4, c % 4
        out[b, :, CTX + 2048 * q:CTX + 2048 * (q + 1)] = hT_sh[c][:, :2048]
        out[b, :, 64 * q:64 * (q + 1)] = hT_sh[c][:, 2048:]
    return out


def _reshard_mixer(dnT, naT):
    dn_all, na_all = [], []
    for c in range(8):
        b, q = c // 4, c % 4
        cols = np.r_[CTX + 2048 * q:CTX + 2048 * (q + 1), 64 * q:64 * (q + 1)]
        dn_all.append(np.ascontiguousarray(np.stack([dnT[hd][b][:, cols] for hd in range(8)])))
        na = np.stack([naT[hd][b][:, cols] for hd in range(8)])
        na_all.append(np.ascontiguousarray(na.reshape(4, 128, OWN)))
    return dn_all, na_all


def kernel_unfused(**inp):
    inp = {k: np.asarray(v) for k, v in inp.items()}
    K = b_consts()
    x, ctx = inp["x"], inp["ctx"]
    xs, cvT = [], []
    for c in range(8):
        b, q = c // 4, c % 4
        xs.append(np.ascontiguousarray(np.concatenate([x[b, 2048 * q:2048 * (q + 1)], ctx[b, 64 * q:64 * (q + 1)]], 0)))
        cvT.append(np.ascontiguousarray(np.stack([inp["c"][b].reshape(8, 128).T, inp["c_ctx"].reshape(8, 128).T], 1)
                                        .astype(np.float32)))
    ada_w = [np.ascontiguousarray(inp["ada_w"][l:l + 1]) for l in range(2)]
    ada_b = [_rep(inp["ada_b"][l])[None] for l in range(2)]

    ncA = build_A()
    res = _run(ncA, [dict(xs=xs[c], cvT=cvT[c], ada_w=ada_w[0], ada_b=ada_b[0], gpre=_rep(inp["norm_mix_pre"][0]),
                          identb=K["identb"]) for c in range(8)])
    hT_sh = [r["hT"] for r in res]
    ncB = build_B()
    for l in range(2):
        hT_full = _assemble_hT(hT_sh)
        res = _run(ncB, [host_inputs_B(inp, l, hd, hT_full)[0] for hd in range(8)])
        dn_all, na_all = _reshard_mixer([r["dnT"] for r in res], [r["naT"] for r in res])
        moe = (l % 2 == 1)
        last = (l == 1)
        ncC = build_C(moe, not last)
        w_in = inp["w_in"][l]
        common = dict(ada_w=ada_w[l], ada_b=ada_b[l], gpost=_rep(inp["norm_mix_post"][l]), gpre2=_rep(inp["norm_ffn_pre"][l]),
                      gpost2=_rep(inp["norm_ffn_post"][l]), wg=np.ascontiguousarray(w_in[:, OFF_GD:OFF_GD + 2048]),
                      wpa=inp["w_branch_dn"][l], wpb=inp["w_branch_na"][l], wout=inp["w_out"][l],
                      identb=K["identb"], identf=K["identf"])
        if moe:
            common.update(router=inp["moe_router"][l // 2], w1=inp["moe_w1"][l // 2], w3=inp["moe_w3"][l // 2],
                          w2=inp["moe_w2"][l // 2])
        else:
            common.update(w1=inp["ffn_w1"][l // 2:l // 2 + 1], w3=inp["ffn_w3"][l // 2:l // 2 + 1],
                          w2=inp["ffn_w2"][l // 2:l // 2 + 1])
        if not last:
            common.update(ada_w_n=ada_w[l + 1], ada_b_n=ada_b[l + 1], gpre_n=_rep(inp["norm_mix_pre"][l + 1]))
        res = _run(ncC, [dict(common, xs=xs[c], hT=hT_sh[c], dnT=dn_all[c], naT=na_all[c], cvT=cvT[c]) for c in range(8)])
        xs = [r["xs_out"] for r in res]
        if not last:
            hT_sh = [r["hT_n"] for r in res]
    out = np.empty((NB, SEQ, D), np.float32)
    for c in range(8):
        b, q = c // 4, c % 4
        out[b, 2048 * q:2048 * (q + 1)] = xs[c][:2048]
    return out


def mods_to_dram(C, pp, l, cvT_d, ada_w_d, ada_b_d, ones_f, mods_d):
    for grp in ([0, 1, 2], [3, 4, 5]):
        with contextlib.ExitStack() as es:
            m = compute_mods(C, pp, l, grp, cvT_d, ada_w_d, ada_b_d, ones_f, es)
            for blk in grp:
                C.dma(mods_d[l, blk], m[blk][:], ["mod%d" % blk], ["modsd"], eng="sync")
            C.flush()


def load_mods(C, mods_dl, blocks, es):
    out = {}
    for blk in blocks:
        t = C.sb("mod%d" % blk, [128, 2, 1024], F32, es)
        C.dma(t[:], mods_dl[blk], [], ["mod%d" % blk], eng="sync")
        out[blk] = t
    return out


def run_A2(C, pp, xs_d, mods_dl, gpre_dl, outs, identb):
    with contextlib.ExitStack() as es:
        xs = C.sb("xs", [128, 17, D], F32, es)
        hT = C.sb("hT", [128, 8, OWN], BF16, es)
        gpre = C.sb("gpre", [128, D], F32, es)
        C.dma(xs[:, 0:16, :], xs_d[0:2048, :].rearrange("(t p) d -> p t d", p=128), [], ["xs"], eng="sync")
        C.dma(xs[0:64, 16, :], xs_d[2048:2112, :], [], ["xs"], eng="sync")
        C.dma(gpre[:], gpre_dl, [], ["gpre"], eng="sync")
        mods = load_mods(C, mods_dl, [0, 1], es)
        phase_A(C, pp, xs, hT, mods, gpre, identb, es)
        for (dst, c0, c1) in outs:
            C.dma(dst, hT[:, :, c0:c1], ["hT"], ["hT_out"], eng="sync")
        C.flush()


def stage_C(C, pp, moe, xs_in_d, xs_mid_d, xs_out_d, hT_d, dnT_d, naT_d, colmap, mods_dl, G, W, identb, identf):
    with contextlib.ExitStack() as es1:
        h2T_all = C.sb("h2T", [128, 8, OWN], BF16, es1)
        gates = C.sb("gates", [128, 17, 8], F32, es1) if moe else None
        gpost2 = C.sb("gpost2", [128, D], F32, es1)
        C.dma(gpost2[:], G["gpost2"], [], ["gpost2"], eng="sync")
        with contextlib.ExitStack() as es2:
            mods = load_mods(C, mods_dl, [2, 3, 4], es2)
            gpost = C.sb("gpost", [128, D], F32, es2)
            gpre2 = C.sb("gpre2", [128, D], F32, es2)
            C.dma(gpost[:], G["gpost"], [], ["gpost"], eng="sync")
            C.dma(gpre2[:], G["gpre2"], [], ["gpre2"], eng="sync")
            yT_all = C.sb("yT", [128, 8, OWN], BF16, es2)
            phase_C1(C, pp, hT_d, dnT_d, naT_d, W["wg"], W["wpa"], W["wpb"], yT_all, es2, colmap=colmap)
            phase_C2(C, pp, yT_all, W["wout"], xs_in_d, xs_mid_d, mods, gpost, gpre2, h2T_all, identb, identf,
                     W.get("router"), gates, es2)
            C.flush()
        with contextlib.ExitStack() as es3:
            acc = C.sb("acc", [128, 17, D], F32, es3)
            phase_C3(C, pp, h2T_all, W["w1"], W["w3"], W["w2"], (NE if moe else 1), (DFE if moe else DFF), gates, acc, es3)
            mods5 = load_mods(C, mods_dl, [5], es3)
            phase_C4(C, pp, acc, xs_mid_d, xs_out_d, mods5, gpost2, 17, es3)
            C.flush()
        C.flush()


def select4(C, sel, jobs, width, dt):
    with contextlib.ExitStack() as es:
        src = [[C.sb("selsrc", [128, width], dt, es) for _ in range(4)] for _ in range(2)]
        acc = [C.sb("selacc", [128, width], dt, es) for _ in range(2)]
        for ji, (dst, rows, slots) in enumerate(jobs):
            i = ji % 2
            for s in range(4):
                for (ap, c0, c1, r0, r1) in slots[s]:
                    C.dma(src[i][s][r0:r1, c0:c1], ap, [], ["selsrc%d_%d" % (i, s)], eng="sync")
            C.ts(acc[i][:rows, :], src[i][0][:rows, :], sel[:rows, 0:1], ALU.mult, ["selsrc%d_0" % i, "sel"], ["selacc%d" % i])
            for s in range(1, 4):
                C.stt(acc[i][:rows, :], src[i][s][:rows, :], sel[:rows, s:s + 1], acc[i][:rows, :], ALU.mult, ALU.add,
                      ["selsrc%d_%d" % (i, s), "sel", "selacc%d" % i], ["selacc%d" % i])
            C.dma(dst, acc[i][:rows, :], ["selacc%d" % i], ["seldst"], eng="sync")
        C.flush()


def build_fused(n_heads=8):
    nc = bass.Bass("TRN2", target_bir_lowering=False)
    C = Ctx(nc)
    I = "ExternalInput"
    xb_d = C.dram("xb", [SEQ, D], F32, I)
    ctxb_d = C.dram("ctxb", [CTX, D], F32, I)
    cvT_d = C.dram("cvT", [128, 2, 8], F32, I)
    sel_d = C.dram("sel", [128, 4], F32, I)
    ada_w_d = C.dram("ada_w", [2, D, 6 * D], F32, I)
    ada_b_d = C.dram("ada_b", [2, 128, 6 * D], F32, I)
    gains_d = C.dram("gains", [2, 4, 128, D], F32, I)
    w_in_d = C.dram("w_in", [2, D, D_IN], F32, I)
    conv_d = C.dram("dn_conv", [2, 3072, 5], F32, I)
    dnpar_d = C.dram("dnpar", [2, 8, 128, 4], F32, I)
    normw_d = C.dram("normw", [2, 128, 128], F32, I)
    nabias_d = C.dram("nabias", [2, 8, 5, 128, 640], F32, I)
    namask_d = C.dram("namask", [5, 128, 640], F32, I)
    cos_d = C.dram("cosT", [128, SEQ], F32, I)
    sin_d = C.dram("sinT", [128, SEQ], F32, I)
    cm_d = C.dram("cm", [5, 128, 128], F32, I)
    cmb_d = C.dram("cmb", [6, 128, 128], BF16, I)
    identb_d = C.dram("identb", [128, 128], BF16, I)
    identf_d = C.dram("identf", [128, 128], F32, I)
    wbd_d = C.dram("w_branch_dn", [2, D, D], F32, I)
    wbn_d = C.dram("w_branch_na", [2, 512, D], F32, I)
    wout_d = C.dram("w_out", [2, D, D], F32, I)
    f1_d = C.dram("ffn_w1", [1, D, DFF], F32, I)
    f3_d = C.dram("ffn_w3", [1, D, DFF], F32, I)
    f2_d = C.dram("ffn_w2", [1, DFF, D], F32, I)
    rt_d = C.dram("moe_router", [1, D, NE], F32, I)
    m1_d = C.dram("moe_w1", [1, NE, D, DFE], F32, I)
    m3_d = C.dram("moe_w3", [1, NE, D, DFE], F32, I)
    m2_d = C.dram("moe_w2", [1, NE, DFE, D], F32, I)
    out_d = C.dram("xs_out", [OWN, D], F32, "ExternalOutput")
    N = "Internal"
    xwork = C.dram("xwork", [4, OWN, D], F32, N)
    xmid = C.dram("xmid", [OWN, D], F32, N)
    hT_full = C.dram("hT_full", [1, D, TB], BF16, N)
    hT_q = C.dram("hT_q", [4, D, OWN], BF16, N)
    dnT_all = C.dram("dnT_all", [8, 128, TB], BF16, N)
    naT_all = C.dram("naT_all", [8, 64, TB], BF16, N)
    mods_d = C.dram("mods", [2, 6, 128, 2, 1024], F32, N)
    xs_sel = C.dram("xs_sel", [OWN, D], F32, N)
    hT_sel = C.dram("hT_sel", [D, OWN], BF16, N)
    dn_sel = C.dram("dn_sel", [8, 128, OWN], BF16, N)
    na_sel = C.dram("na_sel", [4, 128, OWN], BF16, N)

    pp = PsumPool(C)
    identb, identf, ones_f = load_consts(C, identb_d, identf_d)
    sel = C.sb("sel", [128, 4], F32)
    C.dma(sel[:], sel_d, [], ["sel"], eng="sync")
    for s in range(4):
        C.dma(xwork[s][0:2048, :], xb_d[2048 * s:2048 * (s + 1), :], [], ["xw%d" % s], eng="sync")
        C.dma(xwork[s][2048:2112, :], ctxb_d[64 * s:64 * (s + 1), :], [], ["xw%d" % s], eng="sync")
    C.flush()
    hfv = hT_full[0].rearrange("(k p) t -> p k t", p=128)
    for l in range(2):
        moe = (l % 2 == 1)
        mods_to_dram(C, pp, l, cvT_d, ada_w_d, ada_b_d, ones_f, mods_d)
        for s in range(4):
            outs = [(hT_q[s].rearrange("(k p) t -> p k t", p=128), 0, OWN),
                    (hfv[:, :, CTX + 2048 * s:CTX + 2048 * (s + 1)], 0, 2048),
                    (hfv[:, :, 64 * s:64 * (s + 1)], 2048, OWN)]
            run_A2(C, pp, xwork[s], mods_d[l], gains_d[l, 0], outs, identb)
        wl = w_in_d[l]
        for hd in range(n_heads):
            cs = lambda o, n: wl[:, o + hd * n:o + (hd + 1) * n]
            phase_NA(C, pp, 0, hT_full, cs(OFF_NQ, 64), cs(OFF_NK, 64), cs(OFF_NV, 64), nabias_d[l, hd], namask_d,
                     naT_all[hd:hd + 1], identb)
            convv = conv_d[l].rearrange("(x c) k -> x c k", x=3)[:, hd * 128:(hd + 1) * 128, :]
            phase_DN(C, pp, 0, hT_full, cs(OFF_Q, 128), cs(OFF_K, 128), cs(OFF_V, 128), (cs(OFF_Z, 128), wl[:, OFF_AB:OFF_AB + 32], hd), convv,
                     dnpar_d[l, hd], normw_d[l], cos_d, sin_d, cm_d, cmb_d, dnT_all[hd:hd + 1], identb, identf, ones_f)
        G = dict(gpost=gains_d[l, 1], gpre2=gains_d[l, 2], gpost2=gains_d[l, 3])
        W = dict(wg=wl[:, OFF_GD:OFF_GD + 2048], wpa=wbd_d[l], wpb=wbn_d[l], wout=wout_d[l])
        if not moe:
            W.update(w1=f1_d, w3=f3_d, w2=f2_d)
            for s in range(4):
                colmap = (lambda tt, s=s: (CTX + 2048 * s + tt * 512) if tt < 4 else 64 * s)
                stage_C(C, pp, False, xwork[s], xmid, xwork[s], hT_q[s], dnT_all, naT_all, colmap, mods_d[l], G, W,
                        identb, identf)
        else:
            W.update(w1=m1_d[0], w3=m3_d[0], w2=m2_d[0], router=rt_d[0])
            jobs = []
            for t in range(17):
                rows = 128 if t < 16 else 64
                jobs.append((xs_sel[t * 128:t * 128 + rows, :], rows,
                             [[(xwork[s][t * 128:t * 128 + rows, :], 0, D, 0, rows)] for s in range(4)]))
            select4(C, sel, jobs, D, F32)
            jobs = []
            for k in range(8):
                jobs.append((hT_sel[k * 128:(k + 1) * 128, :], 128,
                             [[(hT_q[s][k * 128:(k + 1) * 128, :], 0, OWN, 0, 128)] for s in range(4)]))
            for h in range(8):
                jobs.append((dn_sel[h], 128,
                             [[(dnT_all[h][:, CTX + 2048 * s:CTX + 2048 * (s + 1)], 0, 2048, 0, 128),
                               (dnT_all[h][:, 64 * s:64 * (s + 1)], 2048, OWN, 0, 128)] for s in range(4)]))
            for j in range(4):
                slots = []
                for s in range(4):
                    sl_ = []
                    for tw in range(2):
                        src_h = naT_all[2 * j + tw]
                        sl_.append((src_h[:, CTX + 2048 * s:CTX + 2048 * (s + 1)], 0, 2048, tw * 64, tw * 64 + 64))
                        sl_.append((src_h[:, 64 * s:64 * (s + 1)], 2048, OWN, tw * 64, tw * 64 + 64))
                    slots.append(sl_)
                jobs.append((na_sel[j], 128, slots))
            select4(C, sel, jobs, OWN, BF16)
            stage_C(C, pp, True, xs_sel, xmid, out_d, hT_sel, dn_sel, na_sel, None, mods_d[l], G, W, identb, identf)
    _finish(C)
    return nc


def fused_inputs(inp):
    K = b_consts()
    ri, ci = K["ri"], K["ci"]
    nabias = np.ascontiguousarray(np.stack([np.stack([inp["na_rpb"][l, hd][ri, ci] for hd in range(8)]) for l in range(2)])
                                  ).astype(np.float32)
    dnpar = np.empty((2, 8, 128, 4), np.float32)
    for l in range(2):
        for hd in range(8):
            dnpar[l, hd] = np.array([inp["dn_a_log"][l, 0, hd], inp["dn_a_log"][l, 1, hd],
                                     inp["dn_dt_bias"][l, 0, hd], inp["dn_dt_bias"][l, 1, hd]], np.float32)[None]
    gains = np.stack([np.stack([_rep(inp[k][l]) for k in ("norm_mix_pre", "norm_mix_post", "norm_ffn_pre", "norm_ffn_post")])
                      for l in range(2)])
    shared = dict(
        ada_w=np.ascontiguousarray(inp["ada_w"]), ada_b=np.stack([_rep(inp["ada_b"][l]) for l in range(2)]),
        gains=np.ascontiguousarray(gains), w_in=np.ascontiguousarray(inp["w_in"]), dn_conv=np.ascontiguousarray(inp["dn_conv"]),
        dnpar=dnpar, normw=np.stack([_rep(inp["dn_norm"][l]) for l in range(2)]), nabias=nabias, namask=K["mk"],
        cosT=K["cosT"], sinT=K["sinT"], cm=K["cm"], cmb=K["cmb"], identb=K["identb"], identf=K["identf"],
        w_branch_dn=inp["w_branch_dn"], w_branch_na=inp["w_branch_na"], w_out=inp["w_out"],
        ffn_w1=inp["ffn_w1"], ffn_w3=inp["ffn_w3"], ffn_w2=inp["ffn_w2"], moe_router=inp["moe_router"],
        moe_w1=inp["moe_w1"], moe_w3=inp["moe_w3"], moe_w2=inp["moe_w2"])
    shared = {k: np.ascontiguousarray(v) for k, v in shared.items()}
    maps = []
    for c in range(8):
        b, q = c // 4, c % 4
        selv = np.zeros((128, 4), np.float32)
        selv[:, q] = 1.0
        m = dict(shared)
        m.update(xb=np.ascontiguousarray(inp["x"][b]), ctxb=np.ascontiguousarray(inp["ctx"][b]),
                 cvT=np.ascontiguousarray(np.stack([inp["c"][b].reshape(8, 128).T, inp["c_ctx"].reshape(8, 128).T], 1)
                                          .astype(np.float32)), sel=selv)
        maps.append(m)
    return maps


def kernel_fused(**inp):
    inp = {k: np.asarray(v) for k, v in inp.items()}
    nc = build_fused()
    res = _run(nc, fused_inputs(inp))
    out = np.empty((NB, SEQ, D), np.float32)
    for c in range(8):
        b, q = c // 4, c % 4
        out[b, 2048 * q:2048 * (q + 1)] = res[c]["xs_out"][:2048]
    return out


def kernel(**inp):
    return kernel_fused(**inp)
```

```python
import contextlib
import os
import numpy as np
import ml_dtypes
import concourse.bass as bass
import concourse.mybir as mybir
from concourse.bass_utils import run_bass_kernel_spmd

F32 = mybir.dt.float32
BF16 = mybir.dt.bfloat16
AF = mybir.ActivationFunctionType
ALU = mybir.AluOpType
AX = mybir.AxisListType
NPBF = ml_dtypes.bfloat16

D = 1024
NB = 2
SEQ = 8192
CTX = 256
TB = CTX + SEQ
NT_B = TB // 128
OWN = 2048 + 64
EPS = 1e-6
DFF = 2816
NE = 8
DFE = 3584
D_IN = 7712
NEG = -30000.0

ENGINES = ("sync", "tensor", "vector", "scalar", "gpsimd")
EPOCH = 30000
DMA_K = 8


class Prog:
    def __init__(self, nc, n_sems=140):
        self.nc = nc
        self.es = contextlib.ExitStack()
        self.sem_next = 0
        self.streams = {e: [] for e in ENGINES}
        self.cnt = {e: 0 for e in ENGINES}
        self.sem = {e: self._new_sem() for e in ENGINES}
        self.known = {e: {} for e in ENGINES}
        self.last_write = {}
        self.readers = {}
        self.dma_sems = {}
        self.dma_cnt = {}
        self.ninst = 0

    def _new_sem(self):
        s = self.es.enter_context(self.nc.semaphore("s%d" % self.sem_next))
        self.sem_next += 1
        return s

    def _wait(self, eng, tok):
        s, v = tok
        k = self.known[eng]
        if k.get(id(s), 0) >= v:
            return
        k[id(s)] = v
        self.streams[eng].append(lambda e, s=s, v=v: e.wait_ge(s, v))

    def _deps(self, eng, reads, writes):
        toks = []
        for r in reads:
            t = self.last_write.get(r)
            if t is not None:
                toks.append(t)
        for w in writes:
            t = self.last_write.get(w)
            if t is not None:
                toks.append(t)
            toks.extend(self.readers.get(w, {}).values())
        own = self.sem[eng]
        for t in toks:
            if eng == "tensor" and t[0] is own:
                continue
            self._wait(eng, t)

    def _record(self, tok, reads, writes):
        for w in writes:
            self.last_write[w] = tok
            self.readers[w] = {}
        for r in reads:
            if r in writes:
                continue
            self.readers.setdefault(r, {})[id(tok[0])] = tok

    def op(self, eng, fn, reads=(), writes=()):
        reads = tuple(reads)
        writes = tuple(writes) + tuple(r for r in reads if r.startswith("psum"))
        self._deps(eng, reads, writes)
        if self.cnt[eng] >= EPOCH:
            self.sem[eng] = self._new_sem()
            self.cnt[eng] = 0
        self.cnt[eng] += 1
        s, v = self.sem[eng], self.cnt[eng]
        self.streams[eng].append(lambda e, s=s: fn(e).then_inc(s, 1))
        self._record((s, v), reads, writes)
        self.ninst += 1
        return (s, v)

    def dma(self, eng, fn, reads=(), writes=()):
        reads = tuple(reads)
        writes = tuple(writes)
        self._deps(eng, reads, writes)
        if eng not in self.dma_sems:
            self.dma_sems[eng] = [self._new_sem() for _ in range(DMA_K)]
            self.dma_cnt[eng] = 0
        i = self.dma_cnt[eng]
        self.dma_cnt[eng] += 1
        s = self.dma_sems[eng][i % DMA_K]
        tgt = 16 * (i // DMA_K + 1)
        if tgt > 16:
            self._wait(eng, (s, tgt - 16))
        self.streams[eng].append(lambda e, s=s: fn(e).then_inc(s, 16))
        self._record((s, tgt), reads, writes)
        self.ninst += 1
        return (s, tgt)

    def finish(self, eng="sync"):
        for t in list(self.last_write.values()):
            self._wait(eng, t)

    def build(self):
        nc = self.nc
        with nc.Block() as block:
            for e in ENGINES:
                stream = self.streams[e]
                if not stream:
                    continue

                def body(eng, stream=stream):
                    for f in stream:
                        f(eng)
                getattr(block, e)(body)
        self.streams = {e: [] for e in ENGINES}


class Ctx:
    def __init__(self, nc):
        self.nc = nc
        self.P = Prog(nc)
        self.es = contextlib.ExitStack()
        self.nps = 0
        self.dmaq = 0
        self.uid = 0

    def sb(self, name, shape, dt, es=None):
        self.uid += 1
        return (es or self.es).enter_context(self.nc.sbuf_tensor("%s_%d" % (name, self.uid), list(shape), dt))

    def psum(self, name, shape, dt, es=None):
        self.uid += 1
        return (es or self.es).enter_context(self.nc.psum_tensor("%s_%d" % (name, self.uid), list(shape), dt))

    def flush(self):
        if self.P.ninst == getattr(self, "_flushed_at", -1):
            return
        self.P.finish()
        self.P.build()
        self._flushed_at = self.P.ninst

    def dram(self, name, shape, dt, kind):
        return self.nc.dram_tensor(name, list(shape), dt, kind=kind).ap()

    def mm(self, out, lhsT, rhs, start, stop, r, w):
        return self.P.op("tensor", lambda e: e.matmul(out, lhsT=lhsT, rhs=rhs, start=start, stop=stop), r, w)

    def tr(self, out, in_, ident, r, w):
        return self.P.op("tensor", lambda e: e.transpose(out, in_, ident), r, w)

    def act(self, out, in_, func, r, w, bias=None, scale=None, accum_out=None, eng="scalar"):
        kw = {}
        if bias is not None:
            kw["bias"] = bias
        if scale is not None:
            kw["scale"] = scale
        if accum_out is not None:
            kw["accum_out"] = accum_out
        return self.P.op("scalar", lambda e: e.activation(out=out, in_=in_, func=func, **kw), r, w)

    def tt(self, out, in0, in1, op, r, w, eng="vector"):
        return self.P.op(eng, lambda e: e.tensor_tensor(out=out, in0=in0, in1=in1, op=op), r, w)

    def ts(self, out, in0, s1, op0, r, w, s2=None, op1=None, eng="vector", accum_out=None):
        kw = {}
        if op1 is not None:
            kw["op1"] = op1
        if accum_out is not None:
            kw["accum_out"] = accum_out
        return self.P.op(eng, lambda e: e.tensor_scalar(out=out, in0=in0, scalar1=s1, scalar2=s2, op0=op0, **kw), r, w)

    def stt(self, out, in0, scalar, in1, op0, op1, r, w):
        return self.P.op("vector", lambda e: e.scalar_tensor_tensor(out=out, in0=in0, scalar=scalar, in1=in1,
                                                                   op0=op0, op1=op1), r, w)

    def cp(self, out, in_, r, w, eng="vector"):
        if eng == "scalar":
            return self.P.op("scalar", lambda e: e.copy(out=out, in_=in_), r, w)
        return self.P.op(eng, lambda e: e.tensor_copy(out=out, in_=in_), r, w)

    def memset(self, ap, val, w, eng="vector"):
        return self.P.op(eng, lambda e: e.memset(ap, val), (), w)

    def recip(self, out, in_, r, w):
        return self.P.op("vector", lambda e: e.reciprocal(out=out, in_=in_), r, w)

    def red(self, out, in_, op, r, w, axis=AX.X):
        return self.P.op("vector", lambda e: e.tensor_reduce(out=out, in_=in_, axis=axis, op=op), r, w)

    def dma(self, out, in_, r, w, eng=None):
        if eng is None:
            eng = ("sync", "gpsimd")[self.dmaq % 2]
            self.dmaq += 1
        return self.P.dma(eng, lambda e: e.dma_start(out=out, in_=in_), r, w)


class PsumPool:
    def __init__(self, C, n=8):
        self.C = C
        self.t = [C.psum("psb", [128, 512], F32) for _ in range(n)]
        self.i = 0
        self.n = n

    def get(self):
        i = self.i
        self.i = (self.i + 1) % self.n
        return self.t[i], "psum%d" % i


def rstd_from_ss(C, ss, rs, n, key, inv_n):
    C.ts(rs, ss, inv_n, ALU.mult, [key], [key + "_r"], s2=EPS, op1=ALU.add)
    C.act(rs, rs, AF.Sqrt, [key + "_r"], [key + "_r"])
    C.recip(rs, rs, [key + "_r"], [key + "_r"])


def compute_mods(C, pp, l, blocks, cvT_d, ada_w_d, ada_b_d, ones_f, es):
    out = {}
    for blk in blocks:
        out[blk] = C.sb("mod%d" % blk, [128, 2, 1024], F32, es)
    with contextlib.ExitStack() as tes:
        cv = C.sb("cv", [128, 2, 8], F32, tes)
        C.dma(cv[:], cvT_d, [], ["cv"], eng="sync")
        C.act(cv[:], cv[:], AF.Silu, ["cv"], ["cv"])
        rep = C.sb("rep", [128, 2, 8, 128], F32, tes)
        for s in range(2):
            for k in range(8):
                C.ts(rep[:, s, k, :], ones_f[:], cv[:, s, k:k + 1], ALU.mult, ["cv", "ones_f"], ["rep"])
        wt = [C.sb("adaw", [128, 8, 512], F32, tes) for _ in range(2)]
        bt = [C.sb("adab", [128, 512], F32, tes) for _ in range(2)]
        it = 0
        for blk in blocks:
            m = out[blk]
            for half in range(2):
                col = blk * 1024 + half * 512
                w = wt[it % 2]
                bb = bt[it % 2]
                wk = "adaw%d" % (it % 2)
                it += 1
                C.dma(w[:], ada_w_d[l].rearrange("(k p) n -> p k n", p=128)[:, :, col:col + 512], [], [wk], eng="sync")
                C.dma(bb[:], ada_b_d[l][:, col:col + 512], [], [wk + "b"], eng="sync")
                for s in range(2):
                    ps, pk = pp.get()
                    for k in range(8):
                        C.mm(ps[:], rep[:, s, k, :], w[:, k, :], k == 0, k == 7, ["rep", wk], [pk])
                    C.tt(m[:, s, half * 512:(half + 1) * 512], ps[:], bb[:], ALU.add, [pk, wk + "b"], ["mod%d" % blk])
        C.flush()
    return out


def phase_A(C, pp, xs, hT, mods, gpre, ident, es):
    M1 = C.sb("M1", [128, 2, 1024], F32, es)
    for s in range(2):
        C.stt(M1[:, s, :], mods[1][:, s, :], 1.0, gpre[:], ALU.add, ALU.mult, ["mod1", "gpre"], ["M1"])
    SH = mods[0]
    ss = C.sb("ssA", [128, 17], F32, es)
    rs = C.sb("rsA", [128, 17], F32, es)
    junk = C.sb("junkA", [128, 1024], F32, es)
    C.memset(ss[:], 1.0, ["ssA"])
    for t in range(17):
        rows = 128 if t < 16 else 64
        C.act(junk[:rows, :], xs[:rows, t, :], AF.Square, ["xs"], ["junkA", "ssA"], accum_out=ss[:rows, t:t + 1])
    C.ts(rs[:], ss[:], 1.0 / D, ALU.mult, ["ssA"], ["rsA"], s2=EPS, op1=ALU.add)
    C.act(rs[:], rs[:], AF.Sqrt, ["rsA"], ["rsA"])
    C.recip(rs[:], rs[:], ["rsA"], ["rsA"])
    tmp = [C.sb("tmpA", [128, 1024], F32, es) for _ in range(2)]
    hb = [C.sb("hbA", [128, 1024], BF16, es) for _ in range(2)]
    for t in range(17):
        rows = 128 if t < 16 else 64
        s = 0 if t < 16 else 1
        i = t % 2
        C.stt(tmp[i][:rows, :], xs[:rows, t, :], rs[:rows, t:t + 1], M1[:rows, s, :], ALU.mult, ALU.mult,
              ["xs", "rsA", "M1"], ["tmpA%d" % i])
        C.tt(hb[i][:rows, :], tmp[i][:rows, :], SH[:rows, s, :], ALU.add, ["tmpA%d" % i, "mod0"], ["hbA%d" % i],
             eng="gpsimd")
        ps, pk = pp.get()
        psb = ps[:].bitcast(BF16)
        for k in range(8):
            C.tr(psb[:, k * 128:k * 128 + rows], hb[i][:rows, k * 128:(k + 1) * 128], ident[:rows, :rows],
                 ["hbA%d" % i, "ident"], [pk])
        C.cp(hT[:, :, t * 128:t * 128 + rows],
             psb.rearrange("p (k t) -> p k t", k=8)[:, :, 0:rows], [pk], ["hT"], eng="scalar")


def phase_C1(C, pp, hT_d, dnT_d, naT_d, wg_d, wpa_d, wpb_d, yT_all, es0, colmap=None):
    with contextlib.ExitStack() as es:
        wg = C.sb("wg", [128, 8, 2048], BF16, es)
        wpa = C.sb("wpa", [128, 8, 1024], BF16, es)
        wpb = C.sb("wpb", [128, 4, 1024], BF16, es)
        for k in range(8):
            C.dma(wg[:, k, :], wg_d[k * 128:(k + 1) * 128, :], [], ["wg"], eng="gpsimd")
            C.dma(wpa[:, k, :], wpa_d[k * 128:(k + 1) * 128, :], [], ["wpa"], eng="gpsimd")
        for k in range(4):
            C.dma(wpb[:, k, :], wpb_d[k * 128:(k + 1) * 128, :], [], ["wpb"], eng="gpsimd")
        hTt = [C.sb("hTt", [128, 8, 512], BF16, es) for _ in range(2)]
        dnt = [C.sb("dnt", [128, 8, 512], BF16, es) for _ in range(2)]
        nat = [C.sb("nat", [128, 4, 512], BF16, es) for _ in range(2)]
        s1 = [C.sb("s1", [128, 512], F32, es) for _ in range(2)]
        s2 = [C.sb("s2", [128, 512], F32, es) for _ in range(2)]
        for tt in range(5):
            n = 512 if tt < 4 else 64
            t0 = tt * 512
            i = tt % 2
            C.dma(hTt[i][:, :, :n], hT_d.rearrange("(k p) t -> p k t", p=128)[:, :, t0:t0 + n], [], ["hTt%d" % i], eng="sync")
            if colmap is None:
                C.dma(dnt[i][:, :, :n], dnT_d.rearrange("h p t -> p h t")[:, :, t0:t0 + n], [], ["dnt%d" % i], eng="sync")
                C.dma(nat[i][:, :, :n], naT_d.rearrange("h p t -> p h t")[:, :, t0:t0 + n], [], ["nat%d" % i], eng="sync")
            else:
                m0 = colmap(tt)
                C.dma(dnt[i][:, :, :n], dnT_d.rearrange("h p t -> p h t")[:, :, m0:m0 + n], [], ["dnt%d" % i], eng="sync")
                nav_ = naT_d.rearrange("(j two) d t -> two d j t", two=2)
                for tw in range(2):
                    C.dma(nat[i][tw * 64:(tw + 1) * 64, :, :n], nav_[tw][:, :, m0:m0 + n], [], ["nat%d" % i], eng="sync")
            for oc in range(8):
                j = oc % 2
                p1, k1 = pp.get()
                for k in range(8):
                    C.mm(p1[:, :n], wg[:, k, oc * 128:(oc + 1) * 128], hTt[i][:, k, :n], k == 0, k == 7,
                         ["wg", "hTt%d" % i], [k1])
                p2, k2 = pp.get()
                for k in range(8):
                    C.mm(p2[:, :n], wg[:, k, 1024 + oc * 128:1024 + (oc + 1) * 128], hTt[i][:, k, :n], k == 0, k == 7,
                         ["wg", "hTt%d" % i], [k2])
                p3, k3 = pp.get()
                for k in range(8):
                    C.mm(p3[:, :n], wpa[:, k, oc * 128:(oc + 1) * 128], dnt[i][:, k, :n], k == 0, k == 7,
                         ["wpa", "dnt%d" % i], [k3])
                p4, k4 = pp.get()
                for k in range(4):
                    C.mm(p4[:, :n], wpb[:, k, oc * 128:(oc + 1) * 128], nat[i][:, k, :n], k == 0, k == 3,
                         ["wpb", "nat%d" % i], [k4])
                C.act(s1[j][:, :n], p1[:, :n], AF.Sigmoid, [k1], ["s1%d" % j])
                C.act(s2[j][:, :n], p2[:, :n], AF.Sigmoid, [k2], ["s2%d" % j])
                C.tt(s1[j][:, :n], s1[j][:, :n], p3[:, :n], ALU.mult, ["s1%d" % j, k3], ["s1%d" % j])
                C.tt(s2[j][:, :n], s2[j][:, :n], p4[:, :n], ALU.mult, ["s2%d" % j, k4], ["s2%d" % j])
                C.tt(yT_all[:, oc, t0:t0 + n], s1[j][:, :n], s2[j][:, :n], ALU.add, ["s1%d" % j, "s2%d" % j], ["yT"],
                     eng="gpsimd")
        C.flush()


def small_rstd(C, ssum, rs, rows, inv_n, kin, kout):
    C.ts(rs[:rows, :], ssum[:rows, :], inv_n, ALU.mult, [kin], [kout], s2=EPS, op1=ALU.add)
    C.act(rs[:rows, :], rs[:rows, :], AF.Sqrt, [kout], [kout])
    C.recip(rs[:rows, :], rs[:rows, :], [kout], [kout])


def phase_C2(C, pp, yT_all, wout_d, xs_in_d, xs_mid_d, mods, gpost, gpre2, h2T_all, identb, identf,
             router_d, gates, es0):
    with contextlib.ExitStack() as es:
        wout = C.sb("wout", [128, 8, 1024], BF16, es)
        for k in range(8):
            C.dma(wout[:, k, :], wout_d[k * 128:(k + 1) * 128, :], [], ["wout"], eng="gpsimd")
        G1P = C.sb("G1P", [128, 2, 1024], F32, es)
        M2 = C.sb("M2", [128, 2, 1024], F32, es)
        for s in range(2):
            C.tt(G1P[:, s, :], mods[2][:, s, :], gpost[:], ALU.mult, ["mod2", "gpost"], ["G1P"])
            C.stt(M2[:, s, :], mods[4][:, s, :], 1.0, gpre2[:], ALU.add, ALU.mult, ["mod4", "gpre2"], ["M2"])
        SH2 = mods[3]
        moe = router_d is not None
        if moe:
            rt = C.sb("router", [128, 8, 8], F32, es)
            C.dma(rt[:], router_d.rearrange("(k p) e -> p k e", p=128), [], ["router"], eng="sync")
            lg = C.sb("logits", [128, 17, 8], F32, es)
            C.memset(lg[:], 0.0, ["logits"])
            hf = [C.sb("h2f", [128, 8, 128], F32, es) for _ in range(2)]
            for i_ in range(2):
                C.memset(hf[i_][:], 0.0, ["h2f%d" % i_])
        xt = [C.sb("xt", [128, 1024], F32, es) for _ in range(2)]
        tmp = [C.sb("tmpC", [128, 1024], F32, es) for _ in range(2)]
        hb = [C.sb("hbC", [128, 1024], BF16, es) for _ in range(2)]
        junk = C.sb("junkC", [128, 1024], F32, es)
        ssy = [C.sb("ssy", [128, 4], F32, es) for _ in range(2)]
        for t in range(17):
            rows = 128 if t < 16 else 64
            s = 0 if t < 16 else 1
            i = t % 2
            ki = "%d" % i
            C.dma(xt[i][:rows, :], xs_in_d[t * 128:t * 128 + rows, :], [], ["xt" + ki], eng="sync")
            ph = []
            for half in range(2):
                p, pk = pp.get()
                for oc in range(8):
                    C.mm(p[:rows, :], yT_all[:, oc, t * 128:t * 128 + rows], wout[:, oc, half * 512:(half + 1) * 512],
                         oc == 0, oc == 7, ["yT", "wout"], [pk])
                C.act(junk[:rows, half * 512:(half + 1) * 512], p[:rows, :], AF.Square, [pk], ["junkC", "ssy" + ki],
                      accum_out=ssy[i][:rows, half:half + 1])
                ph.append((p, pk))
            C.tt(ssy[i][:rows, 2:3], ssy[i][:rows, 0:1], ssy[i][:rows, 1:2], ALU.add, ["ssy" + ki], ["ssy" + ki])
            small_rstd(C, ssy[i][:, 2:3], ssy[i][:, 3:4], rows, 1.0 / D, "ssy" + ki, "ssy" + ki)
            for half in range(2):
                p, pk = ph[half]
                sl = slice(half * 512, (half + 1) * 512)
                C.stt(tmp[i][:rows, sl], p[:rows, :], ssy[i][:rows, 3:4], G1P[:rows, s, sl], ALU.mult, ALU.mult,
                      [pk, "ssy" + ki, "G1P"], ["tmpC" + ki])
            C.tt(xt[i][:rows, :], xt[i][:rows, :], tmp[i][:rows, :], ALU.add, ["xt" + ki, "tmpC" + ki], ["xt" + ki],
                 eng="gpsimd")
            C.dma(xs_mid_d[t * 128:t * 128 + rows, :], xt[i][:rows, :], ["xt" + ki], ["xs_mid"], eng="sync")
            C.act(junk[:rows, :], xt[i][:rows, :], AF.Square, ["xt" + ki], ["junkC", "ssy" + ki],
                  accum_out=ssy[i][:rows, 0:1])
            small_rstd(C, ssy[i][:, 0:1], ssy[i][:, 1:2], rows, 1.0 / D, "ssy" + ki, "ssy" + ki)
            C.stt(tmp[i][:rows, :], xt[i][:rows, :], ssy[i][:rows, 1:2], M2[:rows, s, :], ALU.mult, ALU.mult,
                  ["xt" + ki, "ssy" + ki, "M2"], ["tmpC" + ki])
            if not moe:
                C.tt(hb[i][:rows, :], tmp[i][:rows, :], SH2[:rows, s, :], ALU.add, ["tmpC" + ki, "mod3"], ["hbC" + ki],
                     eng="gpsimd")
                p, pk = pp.get()
                pb = p[:].bitcast(BF16)
                for k in range(8):
                    C.tr(pb[:, k * 128:k * 128 + rows], hb[i][:rows, k * 128:(k + 1) * 128], identb[:rows, :rows],
                         ["hbC" + ki, "identb"], [pk])
                C.cp(h2T_all[:, :, t * 128:t * 128 + rows], pb.rearrange("p (k t) -> p k t", k=8)[:, :, 0:rows],
                     [pk], ["h2T"], eng="scalar")
            else:
                C.tt(tmp[i][:rows, :], tmp[i][:rows, :], SH2[:rows, s, :], ALU.add, ["tmpC" + ki, "mod3"], ["tmpC" + ki],
                     eng="gpsimd")
                dbx = os.environ.get("DBGX", "")
                pa, pka = pp.get()
                pb_, pkb = pp.get()
                if "t" not in dbx:
                    for k in range(8):
                        p, pk = (pa, pka) if k < 4 else (pb_, pkb)
                        kk = k % 4
                        C.tr(p[:, kk * 128:kk * 128 + rows], tmp[i][:rows, k * 128:(k + 1) * 128], identf[:rows, :rows],
                             ["tmpC" + ki, "identf"], [pk])
                if "e" not in dbx:
                    for hh, (p, pk) in enumerate(((pa, pka), (pb_, pkb))):
                        src = p[:].rearrange("p (k t) -> p k t", k=4)[:, :, 0:rows]
                        C.cp(h2T_all[:, hh * 4:(hh + 1) * 4, t * 128:t * 128 + rows], src, [pk], ["h2T"], eng="scalar")
                        C.cp(hf[i][:, hh * 4:(hh + 1) * 4, 0:rows], src, [pk], ["h2f" + ki], eng="vector")
                if "r" not in dbx:
                    p, pk = pp.get()
                    for k in range(8):
                        C.mm(p[:, 0:8], hf[i][:, k, :], rt[:, k, :], k == 0, k == 7, ["h2f" + ki, "router"], [pk])
                    C.cp(lg[:rows, t, :], p[:rows, 0:8], [pk], ["logits"], eng="vector")
        if moe and os.environ.get("DBG", "") != "2":
            srt = C.sb("srt", [128, 8], F32, es)
            nm1 = C.sb("nm1", [128, 1], F32, es)
            msk = C.sb("msk", [128, 8], F32, es)
            ex = C.sb("ex", [128, 8], F32, es)
            den = C.sb("den", [128, 1], F32, es)
            for t in range(17):
                rows = 128 if t < 16 else 64
                C.P.op("vector", lambda e, t=t, rows=rows: e.max(out=srt[:rows, :], in_=lg[:rows, t, :]), ["logits"], ["srt"])
                C.ts(nm1[:rows, :], srt[:rows, 0:1], -1.0, ALU.mult, ["srt"], ["nm1"])
                C.ts(msk[:rows, :], lg[:rows, t, :], srt[:rows, 1:2], ALU.is_ge, ["logits", "srt"], ["msk"])
                C.act(ex[:rows, :], lg[:rows, t, :], AF.Exp, ["logits", "nm1"], ["ex"], bias=nm1[:rows, :])
                C.tt(ex[:rows, :], ex[:rows, :], msk[:rows, :], ALU.mult, ["ex", "msk"], ["ex"])
                C.red(den[:rows, :], ex[:rows, :], ALU.add, ["ex"], ["den"])
                C.recip(den[:rows, :], den[:rows, :], ["den"], ["den"])
                C.ts(gates[:rows, t, :], ex[:rows, :], den[:rows, 0:1], ALU.mult, ["ex", "den"], ["gates"])
        C.flush()


def phase_C3(C, pp, h2T_all, w1_d, w3_d, w2_d, n_exp, dff, gates, acc, es0):
    nchunks = dff // 128
    groups = []
    c0 = 0
    while c0 < nchunks:
        nch = min(4, nchunks - c0)
        groups.append((c0, nch))
        c0 += nch
    with contextlib.ExitStack() as es:
        w1g = [C.sb("w1g", [128, 8, 512], BF16, es) for _ in range(2)]
        w3g = [C.sb("w3g", [128, 8, 512], BF16, es) for _ in range(2)]
        w2g = [C.sb("w2g", [128, 4, 1024], BF16, es) for _ in range(2)]
        actT = [C.sb("actT", [128, 4, 512], BF16, es) for _ in range(2)]
        sa = [C.sb("sa", [128, 512], F32, es) for _ in range(2)]
        C.memset(acc[:], 0.0, ["acc"])
        it = 0
        for e in range(n_exp):
            for (c0, nch) in groups:
                wi = it % 2
                it += 1
                kw = "wffn%d" % wi
                cs = slice(c0 * 128, (c0 + nch) * 128)
                w1v = w1_d[e].rearrange("(k p) n -> p k n", p=128)
                w3v = w3_d[e].rearrange("(k p) n -> p k n", p=128)
                for k in range(8):
                    C.dma(w1g[wi][:, k, 0:nch * 128], w1v[:, k, cs], [], [kw + "a"], eng="gpsimd")
                    C.dma(w3g[wi][:, k, 0:nch * 128], w3v[:, k, cs], [], [kw + "b"], eng="gpsimd")
                for j in range(nch):
                    C.dma(w2g[wi][:, j, :], w2_d[e][(c0 + j) * 128:(c0 + j + 1) * 128, :], [], [kw + "c"], eng="gpsimd")
                for tt in range(5):
                    n = 512 if tt < 4 else 64
                    t0 = tt * 512
                    ai = tt % 2
                    ka = "actT%d" % ai
                    for j in range(nch):
                        pa, pka = pp.get()
                        for k in range(8):
                            C.mm(pa[:, :n], w1g[wi][:, k, j * 128:(j + 1) * 128], h2T_all[:, k, t0:t0 + n], k == 0, k == 7,
                                 [kw + "a", "h2T"], [pka])
                        pb, pkb = pp.get()
                        for k in range(8):
                            C.mm(pb[:, :n], w3g[wi][:, k, j * 128:(j + 1) * 128], h2T_all[:, k, t0:t0 + n], k == 0, k == 7,
                                 [kw + "b", "h2T"], [pkb])
                        sj = j % 2
                        C.act(sa[sj][:, :n], pa[:, :n], AF.Silu, [pka], ["sa%d" % sj])
                        C.tt(actT[ai][:, j, :n], sa[sj][:, :n], pb[:, :n], ALU.mult, ["sa%d" % sj, pkb], [ka])
                    nsub = 4 if tt < 4 else 1
                    for sub in range(nsub):
                        t = tt * 4 + sub
                        rows = 128 if tt < 4 else 64
                        for half in range(2):
                            p, pk = pp.get()
                            for j in range(nch):
                                C.mm(p[:rows, :], actT[ai][:, j, sub * 128:sub * 128 + rows],
                                     w2g[wi][:, j, half * 512:(half + 1) * 512], j == 0, j == nch - 1, [ka, kw + "c"], [pk])
                            sl = slice(half * 512, (half + 1) * 512)
                            if gates is None:
                                C.tt(acc[:rows, t, sl], acc[:rows, t, sl], p[:rows, :], ALU.add, ["acc", pk], ["acc"])
                            else:
                                C.stt(acc[:rows, t, sl], p[:rows, :], gates[:rows, t, e:e + 1], acc[:rows, t, sl],
                                      ALU.mult, ALU.add, [pk, "gates", "acc"], ["acc"])
        C.flush()


def phase_C4(C, pp, acc, xs_mid_d, xs_out_d, mods, gpost2, n_tiles, es0):
    with contextlib.ExitStack() as es:
        G2P = C.sb("G2P", [128, 2, 1024], F32, es)
        for s in range(2):
            C.tt(G2P[:, s, :], mods[5][:, s, :], gpost2[:], ALU.mult, ["mod5", "gpost2"], ["G2P"])
        xt = [C.sb("xt4", [128, 1024], F32, es) for _ in range(2)]
        junk = C.sb("junk4", [128, 1024], F32, es)
        ss = [C.sb("ss4", [128, 2], F32, es) for _ in range(2)]
        for t in range(n_tiles):
            rows = 128 if t < 16 else 64
            s = 0 if t < 16 else 1
            i = t % 2
            ki = "%d" % i
            C.dma(xt[i][:rows, :], xs_mid_d[t * 128:t * 128 + rows, :], ["xs_mid"], ["xt4" + ki], eng="sync")
            C.act(junk[:rows, :], acc[:rows, t, :], AF.Square, ["acc"], ["junk4", "ss4" + ki], accum_out=ss[i][:rows, 0:1])
            small_rstd(C, ss[i][:, 0:1], ss[i][:, 1:2], rows, 1.0 / D, "ss4" + ki, "ss4" + ki)
            C.stt(junk[:rows, :], acc[:rows, t, :], ss[i][:rows, 1:2], G2P[:rows, s, :], ALU.mult, ALU.mult,
                  ["acc", "ss4" + ki, "G2P"], ["junk4"])
            C.tt(xt[i][:rows, :], xt[i][:rows, :], junk[:rows, :], ALU.add, ["xt4" + ki, "junk4"], ["xt4" + ki], eng="gpsimd")
            C.dma(xs_out_d[t * 128:t * 128 + rows, :], xt[i][:rows, :], ["xt4" + ki], ["xs_out"], eng="sync")
        C.flush()


def _finish(C):
    C.P.finish()
    C.P.build()
    C.es.close()
    C.P.es.close()


def load_consts(C, identb_d, identf_d=None):
    identb = C.sb("identb", [128, 128], BF16)
    C.dma(identb[:], identb_d, [], ["identb"], eng="sync")
    identf = None
    if identf_d is not None:
        identf = C.sb("identf", [128, 128], F32)
        C.dma(identf[:], identf_d, [], ["identf"], eng="sync")
    ones_f = C.sb("ones_f", [128, 128], F32)
    C.memset(ones_f[:], 1.0, ["ones_f"])
    return identb, identf, ones_f


def run_A(C, pp, xs_d, cvT_d, ada_w_d, ada_b_d, gpre_d, hT_d, identb, ones_f):
    with contextlib.ExitStack() as es:
        xs = C.sb("xs", [128, 17, D], F32, es)
        hT = C.sb("hT", [128, 8, OWN], BF16, es)
        gpre = C.sb("gpre", [128, D], F32, es)
        C.dma(xs[:, 0:16, :], xs_d[0:2048, :].rearrange("(t p) d -> p t d", p=128), ["xs_out"], ["xs"], eng="sync")
        C.dma(xs[0:64, 16, :], xs_d[2048:2112, :], ["xs_out"], ["xs"], eng="sync")
        C.dma(gpre[:], gpre_d, [], ["gpre"], eng="sync")
        mods = compute_mods(C, pp, 0, [0, 1], cvT_d, ada_w_d, ada_b_d, ones_f, es)
        phase_A(C, pp, xs, hT, mods, gpre, identb, es)
        C.dma(hT_d.rearrange("(k p) t -> p k t", p=128), hT[:], ["hT"], ["hT_d"], eng="sync")
        C.flush()


def build_A():
    nc = bass.Bass("TRN2", target_bir_lowering=False)
    C = Ctx(nc)
    xs_d = C.dram("xs", [OWN, D], F32, "ExternalInput")
    cvT_d = C.dram("cvT", [128, 2, 8], F32, "ExternalInput")
    ada_w_d = C.dram("ada_w", [1, D, 6 * D], F32, "ExternalInput")
    ada_b_d = C.dram("ada_b", [1, 128, 6 * D], F32, "ExternalInput")
    gpre_d = C.dram("gpre", [128, D], F32, "ExternalInput")
    identb_d = C.dram("identb", [128, 128], BF16, "ExternalInput")
    hT_d = C.dram("hT", [D, OWN], BF16, "ExternalOutput")
    pp = PsumPool(C)
    identb, _, ones_f = load_consts(C, identb_d)
    run_A(C, pp, xs_d, cvT_d, ada_w_d, ada_b_d, gpre_d, hT_d, identb, ones_f)
    _finish(C)
    return nc


def build_C(moe, with_next_A, n_exp_dbg=None):
    nc = bass.Bass("TRN2", target_bir_lowering=False)
    C = Ctx(nc)
    xs_d = C.dram("xs", [OWN, D], F32, "ExternalInput")
    hT_d = C.dram("hT", [D, OWN], BF16, "ExternalInput")
    dnT_d = C.dram("dnT", [8, 128, OWN], BF16, "ExternalInput")
    naT_d = C.dram("naT", [4, 128, OWN], BF16, "ExternalInput")
    cvT_d = C.dram("cvT", [128, 2, 8], F32, "ExternalInput")
    ada_w_d = C.dram("ada_w", [1, D, 6 * D], F32, "ExternalInput")
    ada_b_d = C.dram("ada_b", [1, 128, 6 * D], F32, "ExternalInput")
    gpost_d = C.dram("gpost", [128, D], F32, "ExternalInput")
    gpre2_d = C.dram("gpre2", [128, D], F32, "ExternalInput")
    gpost2_d = C.dram("gpost2", [128, D], F32, "ExternalInput")
    wg_d = C.dram("wg", [D, 2048], F32, "ExternalInput")
    wpa_d = C.dram("wpa", [D, D], F32, "ExternalInput")
    wpb_d = C.dram("wpb", [512, D], F32, "ExternalInput")
    wout_d = C.dram("wout", [D, D], F32, "ExternalInput")
    identb_d = C.dram("identb", [128, 128], BF16, "ExternalInput")
    identf_d = C.dram("identf", [128, 128], F32, "ExternalInput")
    if moe:
        n_exp, dff = (n_exp_dbg or NE), DFE
        router_d = C.dram("router", [D, NE], F32, "ExternalInput")
    else:
        n_exp, dff = 1, DFF
        router_d = None
    w1_d = C.dram("w1", [n_exp, D, dff], F32, "ExternalInput")
    w3_d = C.dram("w3", [n_exp, D, dff], F32, "ExternalInput")
    w2_d = C.dram("w2", [n_exp, dff, D], F32, "ExternalInput")
    xs_mid_d = C.dram("xs_mid", [OWN, D], F32, "Internal")
    xs_out_d = C.dram("xs_out", [OWN, D], F32, "ExternalOutput")
    if with_next_A:
        ada_w2_d = C.dram("ada_w_n", [1, D, 6 * D], F32, "ExternalInput")
        ada_b2_d = C.dram("ada_b_n", [1, 128, 6 * D], F32, "ExternalInput")
        gpre_n_d = C.dram("gpre_n", [128, D], F32, "ExternalInput")
        hTn_d = C.dram("hT_n", [D, OWN], BF16, "ExternalOutput")
    pp = PsumPool(C)
    identb, identf, ones_f = load_consts(C, identb_d, identf_d)
    with contextlib.ExitStack() as es1:
        h2T_all = C.sb("h2T", [128, 8, OWN], BF16, es1)
        gates = C.sb("gates", [128, 17, 8], F32, es1) if moe else None
        gpost2 = C.sb("gpost2", [128, D], F32, es1)
        C.dma(gpost2[:], gpost2_d, [], ["gpost2"], eng="sync")
        with contextlib.ExitStack() as es2:
            mods = compute_mods(C, pp, 0, [2, 3, 4], cvT_d, ada_w_d, ada_b_d, ones_f, es2)
            gpost = C.sb("gpost", [128, D], F32, es2)
            gpre2 = C.sb("gpre2", [128, D], F32, es2)
            C.dma(gpost[:], gpost_d, [], ["gpost"], eng="sync")
            C.dma(gpre2[:], gpre2_d, [], ["gpre2"], eng="sync")
            yT_all = C.sb("yT", [128, 8, OWN], BF16, es2)
            phase_C1(C, pp, hT_d, dnT_d, naT_d, wg_d, wpa_d, wpb_d, yT_all, es2)
            phase_C2(C, pp, yT_all, wout_d, xs_d, xs_mid_d, mods, gpost, gpre2, h2T_all, identb, identf,
                     router_d, gates, es2)
            C.flush()
        with contextlib.ExitStack() as es3:
            acc = C.sb("acc", [128, 17, D], F32, es3)
            if os.environ.get("DBG", "") not in ("2", "3"):
                phase_C3(C, pp, h2T_all, w1_d, w3_d, w2_d, n_exp, dff, gates, acc, es3)
            else:
                C.memset(acc[:], 0.0, ["acc"])
            mods5 = compute_mods(C, pp, 0, [5], cvT_d, ada_w_d, ada_b_d, ones_f, es3)
            phase_C4(C, pp, acc, xs_mid_d, xs_out_d, mods5, gpost2, 17, es3)
            C.flush()
        C.flush()
    if with_next_A:
        run_A(C, pp, xs_out_d, cvT_d, ada_w2_d, ada_b2_d, gpre_n_d, hTn_d, identb, ones_f)
    _finish(C)
    return nc


TT_B = [(i * 512, 512) for i in range(16)] + [(8192, 256)]


def na_pattern(p):
    r = 2 * p
    ws = min(max(r - 4, 0), 118)
    pat = {0: 0, 2: 1, 124: 3, 126: 4}.get(r, 2)
    return ws, pat


def phase_NA(C, pp, b, hT_full_d, wnaq_d, wnak_d, wnav_d, nabias_d, namask_d, naT_d, identb):
    with contextlib.ExitStack() as es:
        wq = C.sb("wnq", [128, 8, 64], BF16, es)
        wk = C.sb("wnk", [128, 8, 64], BF16, es)
        wv = C.sb("wnv", [128, 8, 64], BF16, es)
        for w, d, kk in ((wq, wnaq_d, "wnq"), (wk, wnak_d, "wnk"), (wv, wnav_d, "wnv")):
            C.dma(w[:], d.rearrange("(k p) n -> p k n", p=128), [], [kk], eng="gpsimd")
        bias = C.sb("nabias", [128, 5, 640], F32, es)
        mask = C.sb("namask", [128, 5, 640], F32, es)
        C.dma(bias[:], nabias_d.rearrange("a p n -> p a n"), [], ["nabias"], eng="sync")
        C.dma(mask[:], namask_d.rearrange("a p n -> p a n"), [], ["namask"], eng="sync")
        C.tt(bias[:], bias[:], mask[:], ALU.add, ["nabias", "namask"], ["nabias"])
        qT = C.sb("naqT", [64, TB], BF16, es)
        kT = C.sb("nakT", [64, TB], BF16, es)
        vt = C.sb("nav", [128, NT_B, 64], BF16, es)
        oT = C.sb("naoT", [64, TB], BF16, es)
        hTt = [C.sb("hTtn", [128, 8, 512], BF16, es) for _ in range(2)]
        hv = hT_full_d[b].rearrange("(k p) t -> p k t", p=128)
        for ti, (t0, n) in enumerate(TT_B):
            i = ti % 2
            kh = "hTtn%d" % i
            C.dma(hTt[i][:, :, :n], hv[:, :, t0:t0 + n], [], [kh], eng="sync")
            p, pk = pp.get()
            for k in range(8):
                C.mm(p[0:64, :n], wq[:, k, :], hTt[i][:, k, :n], k == 0, k == 7, ["wnq", kh], [pk])
            C.act(qT[:, t0:t0 + n], p[0:64, :n], AF.Copy, [pk], ["naqT"], scale=0.125)
            p, pk = pp.get()
            for k in range(8):
                C.mm(p[0:64, :n], wk[:, k, :], hTt[i][:, k, :n], k == 0, k == 7, ["wnk", kh], [pk])
            C.cp(kT[:, t0:t0 + n], p[0:64, :n], [pk], ["nakT"], eng="vector")
            p, pk = pp.get()
            for sub in range(n // 128):
                for k in range(8):
                    C.mm(p[:, sub * 64:(sub + 1) * 64], hTt[i][:, k, sub * 128:(sub + 1) * 128], wv[:, k, :], k == 0, k == 7,
                         ["wnv", kh], [pk])
            C.cp(vt[:, t0 // 128:t0 // 128 + n // 128, :], p[:, 0:(n // 128) * 64].rearrange("p (s d) -> p s d", d=64),
                 [pk], ["nav"], eng="scalar")
        NS = 4
        Sb = [C.sb("naS", [128, 896], F32, es) for _ in range(NS)]
        Pb = [C.sb("naP", [128, 896], BF16, es) for _ in range(NS)]
        PT = [C.sb("naPT", [128, 7, 128], BF16, es) for _ in range(NS)]
        st = [C.sb("nast", [128, 4], F32, es) for _ in range(NS)]
        ob = [C.sb("naob", [128, 64], BF16, es) for _ in range(NS)]

        def na_tile(qi, i):
            ki = "%d" % i
            bX, kX = pp.t[2 * i], "psum%d" % (2 * i)
            bY, kY = pp.t[2 * i + 1], "psum%d" % (2 * i + 1)
            bXb = bX[:].bitcast(BF16)
            bYb = bY[:].bitcast(BF16)
            if qi < 2:
                q0 = qi * 128
                nk, nblk, vtiles = 256, 2, [0, 1]
                C.mm(bY[:, 0:256], qT[:, q0:q0 + 128], kT[:, 0:256], True, True, ["naqT", "nakT"], [kY])
                yield
                C.cp(Sb[i][:, 0:256], bY[:, 0:256], [kY], ["naS" + ki], eng="scalar")
                yield
            else:
                p_ = qi - 2
                ws, pat = na_pattern(p_)
                q0 = CTX + 128 * p_
                k0 = CTX + ws * 64
                nk, nblk = 896, 7
                vtiles = [2 + ws // 2 + j for j in range(5)] + [0, 1]
                C.mm(bX[:, 0:512], qT[:, q0:q0 + 128], kT[:, k0:k0 + 512], True, True, ["naqT", "nakT"], [kX])
                C.mm(bY[:, 0:128], qT[:, q0:q0 + 128], kT[:, k0 + 512:k0 + 640], True, True, ["naqT", "nakT"], [kY])
                C.mm(bY[:, 128:384], qT[:, q0:q0 + 128], kT[:, 0:256], True, True, ["naqT", "nakT"], [kY])
                yield
                C.tt(Sb[i][:, 0:512], bX[:, 0:512], bias[:, pat, 0:512], ALU.add, [kX, "nabias"], ["naS" + ki])
                C.cp(Sb[i][:, 640:896], bY[:, 128:384], [kY], ["naS" + ki], eng="scalar")
                yield
                C.tt(Sb[i][:, 512:640], bY[:, 0:128], bias[:, pat, 512:640], ALU.add, [kY, "nabias"], ["naS" + ki])
                yield
            C.red(st[i][:, 0:1], Sb[i][:, 0:nk], ALU.max, ["naS" + ki], ["nast" + ki])
            yield
            C.ts(st[i][:, 1:2], st[i][:, 0:1], -1.0, ALU.mult, ["nast" + ki], ["nast" + ki])
            yield
            C.act(Pb[i][:, 0:nk], Sb[i][:, 0:nk], AF.Exp, ["naS" + ki, "nast" + ki], ["naP" + ki, "nast" + ki],
                  bias=st[i][:, 1:2], accum_out=st[i][:, 2:3])
            yield
            C.recip(st[i][:, 3:4], st[i][:, 2:3], ["nast" + ki], ["nast" + ki])
            for j in range(nblk):
                C.tr(bXb[:, j * 128:(j + 1) * 128], Pb[i][:, j * 128:(j + 1) * 128], identb[:], ["naP" + ki, "identb"], [kX])
            yield
            C.cp(PT[i][:, 0:nblk, :], bXb[:, 0:nblk * 128].rearrange("p (j t) -> p j t", t=128), [kX], ["naPT" + ki],
                 eng="scalar")
            yield
            for j in range(nblk):
                C.mm(bY[:, 384:448], PT[i][:, j, :], vt[:, vtiles[j], :], j == 0, j == nblk - 1, ["naPT" + ki, "nav"], [kY])
            yield
            C.ts(ob[i][:], bY[:, 384:448], st[i][:, 3:4], ALU.mult, [kY, "nast" + ki], ["naob" + ki])
            yield
            C.tr(bYb[0:64, 896:1024], ob[i][:], identb[:], ["naob" + ki, "identb"], [kY])
            yield
            C.cp(oT[:, q0:q0 + 128], bYb[0:64, 896:1024], [kY], ["naoT"], eng="scalar")
            yield

        def na_interleave(gens):
            gens = list(gens)
            while gens:
                for g_ in list(gens):
                    try:
                        next(g_)
                    except StopIteration:
                        gens.remove(g_)

        tiles_all = list(range(2 + 64))
        for g0 in range(0, len(tiles_all), NS):
            na_interleave([na_tile(qi, qi % NS) for qi in tiles_all[g0:g0 + NS]])
        C.dma(naT_d[b], oT[:], ["naoT"], ["naT_d"], eng="sync")
        C.flush()


def phase_DN(C, pp, b, hT_full_d, wq_d, wk_d, wv_d, wzab_d, conv_d, dnpar_d, normw_d, cos_d, sin_d,
             cm_d, cmb_d, dnT_d, identb, identf, ones_f):
    with contextlib.ExitStack() as es:
        cm = C.sb("cm", [128, 5, 128], F32, es)
        cmb = C.sb("cmb", [128, 6, 128], BF16, es)
        C.dma(cm[:], cm_d.rearrange("a p n -> p a n"), [], ["cm"], eng="sync")
        C.dma(cmb[:], cmb_d.rearrange("a p n -> p a n"), [], ["cmb"], eng="sync")
        PM = [cmb[:, 0, :], cmb[:, 2, :]]
        NMt = [cmb[:, 1, :], cmb[:, 3, :]]
        rotT = cmb[:, 4, :]
        ones_b = cmb[:, 5, :]
        wqkv = C.sb("wqkv", [128, 3, 8, 128], BF16, es)
        for x, d in enumerate((wq_d, wk_d, wv_d)):
            C.dma(wqkv[:, x, :, :], d.rearrange("(k p) n -> p k n", p=128), [], ["wqkv"], eng="gpsimd")
        wzab = C.sb("wzab", [128, 8, 132], BF16, es)
        if isinstance(wzab_d, tuple):
            C.dma(wzab[:, :, 0:128], wzab_d[0].rearrange("(k p) n -> p k n", p=128), [], ["wzab"], eng="gpsimd")
            abblk = C.sb("abblk", [128, 8, 32], BF16, es)
            C.dma(abblk[:], wzab_d[1].rearrange("(k p) n -> p k n", p=128), [], ["abblk"], eng="gpsimd")
            C.cp(wzab[:, :, 128:132], abblk[:, :, wzab_d[2]:32:8], ["abblk"], ["wzab"], eng="vector")
        else:
            C.dma(wzab[:], wzab_d.rearrange("(k p) n -> p k n", p=128), [], ["wzab"], eng="gpsimd")
        convw = C.sb("convw", [128, 3, 5], F32, es)
        C.dma(convw[:], conv_d.rearrange("x p k -> p x k"), [], ["convw"], eng="sync")
        par = C.sb("dnpar", [128, 4], F32, es)
        C.dma(par[:], dnpar_d, [], ["dnpar"], eng="sync")
        normw = C.sb("normw", [128, 128], F32, es)
        C.dma(normw[:], normw_d, [], ["normw"], eng="sync")
        qT = C.sb("dqT", [128, TB], BF16, es)
        kT = C.sb("dkT", [128, TB], BF16, es)
        ktok = C.sb("dktok", [128, NT_B, 128], BF16, es)
        vtok = C.sb("dvtok", [128, NT_B, 128], BF16, es)
        sz = C.sb("dsz", [128, NT_B, 128], BF16, es)
        abt = C.sb("dab", [128, NT_B, 4], F32, es)
        RW = 2 + CTX + 2 + 2 + SEQ + 2
        OFFC, OFFL = 2, 2 + CTX + 2 + 2
        with contextlib.ExitStack() as es1:
            raw = C.sb("draw", [128, 3, RW], BF16, es1)
            for x in range(3):
                C.memset(raw[:, x, 0:2], 0.0, ["draw"], eng="gpsimd")
                C.memset(raw[:, x, OFFC + CTX:OFFL], 0.0, ["draw"], eng="gpsimd")
                C.memset(raw[:, x, OFFL + SEQ:RW], 0.0, ["draw"], eng="gpsimd")
            hTt = [C.sb("hTtd", [128, 8, 512], BF16, es1) for _ in range(2)]
            ztmp = [C.sb("ztmp", [128, 128], F32, es1) for _ in range(2)]
            hv = hT_full_d[b].rearrange("(k p) t -> p k t", p=128)
            tiles = [(0, 256)] + [(CTX + i * 512, 512) for i in range(16)]
            for ti, (t0, n) in enumerate(tiles):
                i = ti % 2
                kh = "hTtd%d" % i
                C.dma(hTt[i][:, :, :n], hv[:, :, t0:t0 + n], [], [kh], eng="sync")
                ro = OFFC + t0 if t0 < CTX else OFFL + (t0 - CTX)
                for x in range(3):
                    p, pk = pp.get()
                    for k in range(8):
                        C.mm(p[:, :n], wqkv[:, x, k, :], hTt[i][:, k, :n], k == 0, k == 7, ["wqkv", kh], [pk])
                    C.cp(raw[:, x, ro:ro + n], p[:, :n], [pk], ["draw"], eng=("scalar" if x != 1 else "vector"))
                for sub in range(n // 128):
                    tl = t0 // 128 + sub
                    p, pk = pp.get()
                    for k in range(8):
                        C.mm(p[:, 0:132], hTt[i][:, k, sub * 128:(sub + 1) * 128], wzab[:, k, :], k == 0, k == 7,
                             ["wzab", kh], [pk])
                    zi = tl % 2
                    C.act(ztmp[zi][:], p[:, 0:128], AF.Silu, [pk], ["ztmp%d" % zi])
                    C.tt(sz[:, tl, :], ztmp[zi][:], normw[:], ALU.mult, ["ztmp%d" % zi, "normw"], ["dsz"], eng="gpsimd")
                    C.cp(abt[:, tl, :], p[:, 128:132], [pk], ["dab"], eng="vector")
            acc = [C.sb("cacc", [128, 512], F32, es1) for _ in range(2)]
            sq = [C.sb("csq", [128, 512], BF16, es1) for _ in range(2)]
            rn = [C.sb("crn", [128, 512], F32, es1) for _ in range(2)]
            t1 = [C.sb("ct1", [128, 512], F32, es1) for _ in range(2)]
            t2 = [C.sb("ct2", [128, 512], F32, es1) for _ in range(2)]
            cs = [C.sb("ccs", [128, 2, 512], F32, es1) for _ in range(2)]
            yb3 = [[C.sb("cyb3", [128, 512], BF16, es1) for _ in range(3)] for _ in range(2)]
            for ti, (t0, n) in enumerate(tiles):
                lat = t0 >= CTX
                ro = OFFC + t0 if not lat else OFFL + (t0 - CTX)
                ci = ti % 2
                if lat:
                    C.dma(cs[ci][:, 0, :n], cos_d[:, t0 - CTX:t0 - CTX + n], [], ["ccs%d" % ci], eng="sync")
                    C.dma(cs[ci][:, 1, :n], sin_d[:, t0 - CTX:t0 - CTX + n], [], ["ccs%d" % ci], eng="sync")
                for x in range(3):
                    i = x % 2
                    ki = "%d" % i
                    ybx = yb3[ci][x]
                    ky = "cyb3_%d_%d" % (ci, x)
                    C.ts(acc[i][:, :n], raw[:, x, ro - 2:ro - 2 + n], convw[:, x, 0:1], ALU.mult, ["draw", "convw"], ["cacc" + ki])
                    for tap in range(1, 5):
                        C.stt(acc[i][:, :n], raw[:, x, ro - 2 + tap:ro - 2 + tap + n], convw[:, x, tap:tap + 1], acc[i][:, :n],
                              ALU.mult, ALU.add, ["draw", "convw", "cacc" + ki], ["cacc" + ki])
                    C.act(ybx[:, :n], acc[i][:, :n], AF.Silu, ["cacc" + ki], [ky])
                for x in (2, 0, 1):
                    i = x % 2
                    ki = "%d" % i
                    ybx = yb3[ci][x]
                    ky = "cyb3_%d_%d" % (ci, x)
                    if x == 2:
                        for sub in range(n // 128):
                            tl = t0 // 128 + sub
                            p, pk = pp.get()
                            pb = p[:].bitcast(BF16)
                            C.tr(pb[:, 0:128], ybx[:, sub * 128:(sub + 1) * 128], identb[:], [ky, "identb"], [pk])
                            C.cp(vtok[:, tl, :], pb[:, 0:128], [pk], ["dvtok"], eng="scalar")
                        continue
                    dst = qT if x == 0 else kT
                    dk = "dqT" if x == 0 else "dkT"
                    scale = 128.0 ** -0.5 if x == 0 else 1.0
                    C.tt(sq[i][:, :n], ybx[:, :n], ybx[:, :n], ALU.mult, [ky], ["csq" + ki], eng="gpsimd")
                    p, pk = pp.get()
                    C.mm(p[:, :n], ones_b, sq[i][:, :n], True, True, ["cmb", "csq" + ki], [pk])
                    C.act(rn[i][:, :n], p[:, :n], AF.Ln, [pk], ["crn" + ki], bias=EPS)
                    C.act(rn[i][:, :n], rn[i][:, :n], AF.Exp, ["crn" + ki], ["crn" + ki], scale=-0.5)
                    if lat:
                        p2, pk2 = pp.get()
                        C.mm(p2[:, :n], rotT, ybx[:, :n], True, True, ["cmb", ky], [pk2])
                        C.tt(t1[i][:, :n], ybx[:, :n], cs[ci][:, 0, :n], ALU.mult, [ky, "ccs%d" % ci], ["ct1" + ki],
                             eng="gpsimd")
                        C.tt(t2[i][:, :n], p2[:, :n], cs[ci][:, 1, :n], ALU.mult, [pk2, "ccs%d" % ci], ["ct2" + ki])
                        C.tt(t1[i][:, :n], t1[i][:, :n], t2[i][:, :n], ALU.add, ["ct1" + ki, "ct2" + ki], ["ct1" + ki], eng="gpsimd")
                        C.stt(dst[:, t0:t0 + n], t1[i][:, :n], scale, rn[i][:, :n], ALU.mult, ALU.mult,
                              ["ct1" + ki, "crn" + ki], [dk])
                    else:
                        C.stt(dst[:, t0:t0 + n], ybx[:, :n], scale, rn[i][:, :n], ALU.mult, ALU.mult,
                              [ky, "crn" + ki], [dk])
                    if x == 1:
                        for sub in range(n // 128):
                            tl = t0 // 128 + sub
                            p, pk = pp.get()
                            pb = p[:].bitcast(BF16)
                            C.tr(pb[:, 0:128], kT[:, t0 + sub * 128:t0 + (sub + 1) * 128], identb[:], ["dkT", "identb"], [pk])
                            C.cp(ktok[:, tl, :], pb[:, 0:128], [pk], ["dktok"], eng="scalar")
            C.flush()
        NT = NT_B
        G = C.sb("dG", [128, 2, NT], F32, es)
        BETA = C.sb("dBETA", [128, 2, NT], F32, es)
        GC = C.sb("dGC", [128, 2, NT], F32, es)
        NGC = C.sb("dNGC", [128, 2, NT], F32, es)
        NBT = C.sb("dNB", [128, 2, NT], F32, es)
        BEG = C.sb("dBEG", [128, 2, NT], F32, es)
        EDK = C.sb("dEDK", [128, 2, NT], F32, es)
        EGL = C.sb("dEGL", [128, 2, 2, NT], F32, es)
        nea = C.sb("dnea", [128, 2], F32, es)
        C.act(nea[:], par[:, 0:2], AF.Exp, ["dnpar"], ["dnea"])
        C.ts(nea[:], nea[:], -1.0, ALU.mult, ["dnea"], ["dnea"])
        for d in range(2):
            C.act(G[:, d, :], abt[:, :, d], AF.Exp, ["dab", "dnpar"], ["dG"], bias=par[:, 2 + d:3 + d])
            C.act(G[:, d, :], G[:, d, :], AF.Ln, ["dG"], ["dG"], bias=1.0)
            C.ts(G[:, d, :], G[:, d, :], nea[:, d:d + 1], ALU.mult, ["dG", "dnea"], ["dG"])
            C.act(BETA[:, d, :], abt[:, :, 2 + d], AF.Sigmoid, ["dab"], ["dBETA"])
            p, pk = pp.get()
            C.mm(p[:, 0:NT], cm[:, d, :], G[:, d, :], True, True, ["cm", "dG"], [pk])
            C.cp(GC[:, d, :], p[:, 0:NT], [pk], ["dGC"], eng="vector")
            p, pk = pp.get()
            C.mm(p[:, 0:NT], cm[:, 2, :], G[:, d, :], True, True, ["cm", "dG"], [pk])
            C.tt(EDK[:, d, :], p[:, 0:NT], GC[:, d, :], ALU.subtract, [pk, "dGC"], ["dEDK"])
            C.act(EDK[:, d, :], EDK[:, d, :], AF.Exp, ["dEDK"], ["dEDK"])
            for hf in range(2):
                p, pk = pp.get()
                C.mm(p[:, 0:NT], cm[:, 3 + hf, :], G[:, d, :], True, True, ["cm", "dG"], [pk])
                C.act(EGL[:, d, hf, :], p[:, 0:NT], AF.Exp, [pk], ["dEGL"])
        C.ts(NGC[:], GC[:], -1.0, ALU.mult, ["dGC"], ["dNGC"])
        C.ts(NBT[:], BETA[:], -1.0, ALU.mult, ["dBETA"], ["dNB"])
        C.act(BEG[:], GC[:], AF.Exp, ["dGC"], ["dBEG"])
        C.tt(BEG[:], BEG[:], BETA[:], ALU.mult, ["dBEG", "dBETA"], ["dBEG"])
        obuf = C.sb("dobuf", [128, NT, 128], F32, es)
        C.memset(obuf[:], 0.0, ["ob%d" % t for t in range(NT)], eng="gpsimd")
        S32 = [C.sb("dS32", [128, 128], F32, es) for _ in range(2)]
        S16 = [C.sb("dS16", [128, 128], BF16, es) for _ in range(2)]
        for d in range(2):
            C.memset(S32[d][:], 0.0, ["S32_%d" % d])
            C.memset(S16[d][:], 0.0, ["S16_%d" % d])
        NPAR = 3
        NSLOT = 2 * NPAR
        ring = []
        for d in range(2):
            ring.append([dict(u=C.sb("pu", [128, 128], F32, es), wT=C.sb("pw", [128, 128], BF16, es),
                              at=C.sb("pat", [128, 128], BF16, es), qg=C.sb("pqg", [128, 128], BF16, es),
                              kd=C.sb("pkd", [128, 128], BF16, es), vn=C.sb("pvn", [128, 128], BF16, es))
                         for _ in range(NSLOT)])
        tmpf = {n_: [C.sb("pt" + n_, [128, 128], F32, es) for _ in range(2 * NPAR)] for n_ in ("dg", "E", "ET", "EG")}
        tmpb = {n_: [C.sb("pb" + n_, [128, 128], BF16, es) for _ in range(4 * NPAR)] for n_ in ("N", "X", "PT")}
        tmpc = {n_: [C.sb("pc" + n_, [128, 128], BF16, es) for _ in range(2 * NPAR)] for n_ in ("kbg", "vb", "dgh", "dgl")}
        pmf = C.sb("pmf", [128, 4, 128], F32, es)
        C.cp(pmf[:], cmb[:, 0:4, :], ["cmb"], ["pmf"], eng="vector")
        PMf = [pmf[:, 0, :], pmf[:, 2, :]]
        NMf = [pmf[:, 1, :], pmf[:, 3, :]]
        GCHb = C.sb("dGCHb", [128, 2, NT], BF16, es)
        GCH = C.sb("dGCH", [128, 2, NT], F32, es)
        GCL = C.sb("dGCL", [128, 2, NT], F32, es)
        C.cp(GCHb[:], GC[:], ["dGC"], ["dGCHb"], eng="vector")
        C.cp(GCH[:], GCHb[:], ["dGCHb"], ["dGCH"], eng="vector")
        C.tt(GCL[:], GC[:], GCH[:], ALU.subtract, ["dGC", "dGCH"], ["dGCL"])

        def bankq(bi, qi):
            b_ = pp.t[bi]
            return b_[:, qi * 128:(qi + 1) * 128], b_[:].bitcast(BF16)[:, qi * 256:qi * 256 + 128], "psum%d" % bi

        def prep(t, d, slot, sk, par):
            R = ring[d][slot]
            c0 = t * 128
            q_ = NPAR * d + par
            kd = "%d" % q_
            Q0, _, kB = bankq(q_, 0)
            Q1, _, _ = bankq(q_, 1)
            Q2, _, _ = bankq(q_, 2)
            Q3, Q3b, _ = bankq(q_, 3)
            C.mm(Q0, kT[:, c0:c0 + 128], kT[:, c0:c0 + 128], True, True, ["dkT"], [kB])
            C.mm(Q1, kT[:, c0:c0 + 128], qT[:, c0:c0 + 128], True, True, ["dkT", "dqT"], [kB])
            dgh, dgl = tmpc["dgh"][q_], tmpc["dgl"][q_]
            C.act(dgh[:], identf[:], AF.Copy, ["identf", "dGCH"], ["dgh" + kd], scale=GCH[:, d, t:t + 1])
            C.act(dgl[:], identf[:], AF.Copy, ["identf", "dGCL"], ["dgl" + kd], scale=GCL[:, d, t:t + 1])
            kbg, vb = tmpc["kbg"][q_], tmpc["vb"][q_]
            C.act(kbg[:], ktok[:, t, :], AF.Copy, ["dktok", "dBEG"], ["kbg" + kd], scale=BEG[:, d, t:t + 1])
            C.ts(vb[:], vtok[:, t, :], BETA[:, d, t:t + 1], ALU.mult, ["dvtok", "dBETA"], ["vb" + kd], eng="vector")
            C.act(R["kd"][:], ktok[:, t, :], AF.Copy, ["dktok", "dEDK"], [sk + "kd"], scale=EDK[:, d, t:t + 1])
            yield
            C.mm(Q2, ones_b, dgh[:], True, False, ["cmb", "dgh" + kd], [kB])
            C.mm(Q2, ones_b, dgl[:], False, True, ["cmb", "dgl" + kd], [kB])
            yield
            E, ET, EG = tmpf["E"][q_], tmpf["ET"][q_], tmpf["EG"][q_]
            bs = tmpf["dg"][q_]
            C.cp(bs[:], Q2, [kB], ["bs" + kd], eng="scalar")
            yield
            C.tt(E[:], bs[:], PMf[d], ALU.add, ["bs" + kd, "pmf"], ["E" + kd], eng="gpsimd")
            C.tt(ET[:], bs[:], NMf[d], ALU.add, ["bs" + kd, "pmf"], ["ET" + kd], eng="gpsimd")
            yield
            C.act(E[:], E[:], AF.Exp, ["E" + kd, "dGC"], ["E" + kd], bias=GC[:, d, t:t + 1], scale=-1.0)
            C.act(ET[:], ET[:], AF.Exp, ["ET" + kd, "dNGC"], ["ET" + kd], bias=NGC[:, d, t:t + 1], scale=1.0)
            C.act(EG[:], bs[:], AF.Exp, ["bs" + kd], ["EG" + kd])
            yield
            N = tmpb["N"]
            X = tmpb["X"]
            PTb = tmpb["PT"]
            o = 2 * q_
            C.stt(N[o][:], Q0, NBT[:, d, t:t + 1], E[:], ALU.mult, ALU.mult, [kB, "dNB", "E" + kd], ["N%d" % o])
            C.tt(R["at"][:], Q1, ET[:], ALU.mult, [kB, "ET" + kd], [sk + "at"])
            yield
            C.tr(Q3b, N[o][:], identb[:], ["N%d" % o, "identb"], [kB])
            yield
            C.cp(X[o][:], Q3b, [kB], ["X%d" % o], eng="scalar")
            C.tt(PTb[o][:], Q3b, identb[:], ALU.add, [kB, "identb"], ["PT%d" % o])
            yield
            C.tt(R["qg"][:], qT[:, c0:c0 + 128], EG[:], ALU.mult, ["dqT", "EG" + kd], [sk + "qg"], eng="gpsimd")
            cur = 0
            for lev in range(1, 6):
                a, bb = o + cur, o + 1 - cur
                C.mm(Q0, X[a][:], N[a][:], True, True, ["X%d" % a, "N%d" % a], [kB])
                if lev < 5:
                    C.mm(Q1, N[a][:], X[a][:], True, True, ["X%d" % a, "N%d" % a], [kB])
                yield
                C.cp(N[bb][:], Q0, [kB], ["N%d" % bb], eng="scalar")
                if lev < 5:
                    C.cp(X[bb][:], Q1, [kB], ["X%d" % bb], eng="vector")
                yield
                C.mm(Q2, N[bb][:], PTb[a][:], True, True, ["N%d" % bb, "PT%d" % a], [kB])
                yield
                C.tt(PTb[bb][:], PTb[a][:], Q2, ALU.add, ["PT%d" % a, kB], ["PT%d" % bb])
                yield
                cur = 1 - cur
            fin = o + cur
            C.mm(Q0, PTb[fin][:], vb[:], True, True, ["PT%d" % fin, "vb" + kd], [kB])
            C.mm(Q1, kbg[:], PTb[fin][:], True, True, ["PT%d" % fin, "kbg" + kd], [kB])
            yield
            C.cp(R["u"][:], Q0, [kB], [sk + "u"], eng="scalar")
            C.cp(R["wT"][:], Q1, [kB], [sk + "wT"], eng="vector")
            yield

        def step(t, hf, d, slot, sk):
            R = ring[d][slot]
            sl = slice(64 * hf, 64 * hf + 64)
            kd = "%d" % d
            bS = pp.t[2 * NPAR + d]
            kS = "psum%d" % (2 * NPAR + d)
            pS, pO, pD = bS[:, 0:128], bS[:, 128:256], bS[:, 256:384]
            C.mm(pS[sl, :], R["wT"][:, sl], S16[d][:], True, True, [sk + "wT", "S16_" + kd], [kS])
            yield
            C.tt(R["vn"][sl, :], R["u"][sl, :], pS[sl, :], ALU.subtract, [sk + "u", kS], [sk + "vn"])
            yield
            C.mm(pO[sl, :], R["qg"][:, sl], S16[d][:], True, False, [sk + "qg", "S16_" + kd], [kS])
            C.mm(pO[sl, :], R["at"][sl, sl], R["vn"][sl, :], False, True, [sk + "at", sk + "vn"], [kS])
            C.mm(pD, R["kd"][sl, :], R["vn"][sl, :], True, True, [sk + "kd", sk + "vn"], [kS])
            yield
            C.stt(S32[d][:], S32[d][:], EGL[:, d, hf, t:t + 1], pD, ALU.mult, ALU.add,
                  ["S32_" + kd, "dEGL", kS], ["S32_" + kd])
            yield
            C.cp(S16[d][:], S32[d][:], ["S32_" + kd], ["S16_" + kd], eng="scalar")
            C.tt(obuf[sl, t, :], obuf[sl, t, :], pO[sl, :], ALU.add, ["ob%d" % t, kS], ["ob%d" % t])
            yield

        def steps(d, idxs, order):
            for i_ in idxs:
                t = order[i_]
                slot = i_ % NSLOT
                sk = "r%d_%d_" % (d, slot)
                for hf in ((0, 1) if d == 0 else (1, 0)):
                    yield from step(t, hf, d, slot, sk)

        def interleave(gens):
            gens = list(gens)
            while gens:
                for g_ in list(gens):
                    try:
                        next(g_)
                    except StopIteration:
                        gens.remove(g_)

        order_f = list(range(NT))
        order_b = [1, 0] + list(range(NT - 1, 1, -1))
        orders = (order_f, order_b)

        def preps_for(j):
            gl_ = []
            for i_ in range(NPAR * j, NPAR * j + NPAR):
                for d in range(2):
                    slot = i_ % NSLOT
                    gl_.append(prep(orders[d][i_], d, slot, "r%d_%d_" % (d, slot), i_ % NPAR))
            return gl_

        assert NT % NPAR == 0
        NP = NT // NPAR
        interleave(preps_for(0))
        for j in range(NP):
            gl_ = preps_for(j + 1) if j + 1 < NP else []
            gl_.append(steps(0, range(NPAR * j, NPAR * j + NPAR), order_f))
            gl_.append(steps(1, range(NPAR * j, NPAR * j + NPAR), order_b))
            interleave(gl_)
        with contextlib.ExitStack() as es2:
            sqb = vtok
            ssq = C.sb("dssq", [128, NT], F32, es2)
            oT = qT
            on = [C.sb("don", [128, 128], BF16, es2) for _ in range(2)]
            allob = ["ob%d" % t for t in range(NT)]
            C.tt(sqb[:], obuf[:], obuf[:], ALU.mult, allob, ["dvtok"])
            C.red(ssq[:], sqb[:], ALU.add, ["dvtok"], ["dssq"])
            C.ts(ssq[:], ssq[:], 1.0 / 128, ALU.mult, ["dssq"], ["dssq"], s2=EPS, op1=ALU.add)
            C.act(ssq[:], ssq[:], AF.Sqrt, ["dssq"], ["dssq"])
            C.recip(ssq[:], ssq[:], ["dssq"], ["dssq"])
            for t in range(NT):
                i = t % 2
                C.stt(on[i][:], obuf[:, t, :], ssq[:, t:t + 1], sz[:, t, :], ALU.mult, ALU.mult,
                      ["ob%d" % t, "dssq", "dsz"], ["don%d" % i])
                p, pk = pp.get()
                pb = p[:].bitcast(BF16)
                C.tr(pb[:, 0:128], on[i][:], identb[:], ["don%d" % i, "identb"], [pk])
                C.cp(oT[:, t * 128:(t + 1) * 128], pb[:, 0:128], [pk], ["dqT"], eng="scalar")
            C.dma(dnT_d[b], oT[:], ["dqT"], ["dnT_d"], eng="sync")
            C.flush()
        C.flush()


OFF_Q, OFF_K, OFF_V, OFF_Z, OFF_AB, OFF_NQ, OFF_NK, OFF_NV, OFF_GD, OFF_GN = (
    0, 1024, 2048, 3072, 4096, 4128, 4640, 5152, 5664, 6688)

_CONST_CACHE = {}


def b_consts():
    if "b" in _CONST_CACHE:
        return _CONST_CACHE["b"]
    idx = np.arange(128)
    same = (idx[:, None] // 64) == (idx[None, :] // 64)
    a, bq = idx[:, None], idx[None, :]
    cm = np.stack([
        same & (a <= bq),
        same & (a >= bq),
        same,
        np.broadcast_to(a < 64, (128, 128)),
        np.broadcast_to(a >= 64, (128, 128)),
    ]).astype(np.float32)
    big = 30000.0
    cmb = np.stack([
        np.where(same & (a > bq), 0.0, big),
        np.where(same & (bq >= a), 0.0, -big),
        np.where(same & (a < bq), 0.0, big),
        np.where(same & (bq <= a), 0.0, -big),
        np.where(a == bq + 64, -1.0, 0.0) + np.where(a == bq - 64, 1.0, 0.0),
        np.ones((128, 128)),
    ]).astype(NPBF)
    t = np.arange(SEQ)
    row = (t // 64).astype(np.float32)
    col = (t % 64).astype(np.float32)
    inv = (np.float32(10000.0) ** (-np.arange(32, dtype=np.float32) / np.float32(32))).astype(np.float32)
    ang = np.concatenate([row[None, :] * inv[:, None], col[None, :] * inv[:, None]], 0)
    ang = np.concatenate([ang, ang], 0).astype(np.float32)
    cosT = np.cos(ang).astype(np.float32)
    sinT = np.sin(ang).astype(np.float32)
    reps = [0, 2, 10, 124, 126]
    ri = np.zeros((5, 128, 640), np.int64)
    ci = np.zeros((5, 128, 640), np.int64)
    mk = np.zeros((5, 128, 640), np.float32)
    qq = np.arange(128)
    kk = np.arange(640)
    dr, cq = qq // 64, qq % 64
    kr, ck = kk // 64, kk % 64
    for pi, r in enumerate(reps):
        ws = min(max(r - 4, 0), 118)
        R_ = r + dr
        r0 = np.clip(R_ - 4, 0, 120)
        krow = ws + kr
        okr = (krow[None, :] >= r0[:, None]) & (krow[None, :] < r0[:, None] + 8)
        c0 = np.clip(cq - 8, 0, 48)
        okc = (ck[None, :] >= c0[:, None]) & (ck[None, :] < c0[:, None] + 16)
        ok = okr & okc
        ri[pi] = np.clip(krow[None, :] - R_[:, None] + 7, 0, 14)
        ci[pi] = np.clip(ck[None, :] - cq[:, None] + 15, 0, 30)
        mk[pi] = np.where(ok, 0.0, NEG)
    out = dict(cm=cm, cmb=cmb, cosT=cosT, sinT=sinT, ri=ri, ci=ci, mk=mk,
               identb=np.eye(128).astype(NPBF), identf=np.eye(128, dtype=np.float32))
    _CONST_CACHE["b"] = out
    return out


def host_inputs_B(inp, l, hd, hT_full):
    k = b_consts()
    w_in = inp["w_in"][l]
    sl = lambda o, n: np.ascontiguousarray(w_in[:, o + hd * n:o + (hd + 1) * n])
    ab_cols = [OFF_AB + hd, OFF_AB + 8 + hd, OFF_AB + 16 + hd, OFF_AB + 24 + hd]
    rpb = inp["na_rpb"][l, hd]
    conv = inp["dn_conv"][l]
    im = dict(
        hT_full=hT_full,
        wnaq=sl(OFF_NQ, 64), wnak=sl(OFF_NK, 64), wnav=sl(OFF_NV, 64),
        nabias=np.ascontiguousarray(rpb[k["ri"], k["ci"]]).astype(np.float32), namask=k["mk"],
        wq=sl(OFF_Q, 128), wk=sl(OFF_K, 128), wv=sl(OFF_V, 128),
        wzab=np.ascontiguousarray(np.concatenate([w_in[:, OFF_Z + hd * 128:OFF_Z + (hd + 1) * 128], w_in[:, ab_cols]], 1)),
        conv=np.ascontiguousarray(np.stack([conv[x * 1024 + hd * 128:x * 1024 + (hd + 1) * 128] for x in range(3)])),
        dnpar=np.ascontiguousarray(np.broadcast_to(np.array([inp["dn_a_log"][l, 0, hd], inp["dn_a_log"][l, 1, hd],
                                                             inp["dn_dt_bias"][l, 0, hd], inp["dn_dt_bias"][l, 1, hd]],
                                                            np.float32)[None], (128, 4))),
        normw=np.ascontiguousarray(np.broadcast_to(inp["dn_norm"][l][None], (128, 128))).astype(np.float32),
        cosT=k["cosT"], sinT=k["sinT"], cm=k["cm"], cmb=k["cmb"], identb=k["identb"], identf=k["identf"],
    )
    return im, None


def build_B(do_na=True, do_dn=True, nbatch=NB):
    nc = bass.Bass("TRN2", target_bir_lowering=False)
    C = Ctx(nc)
    hT_full_d = C.dram("hT_full", [NB, D, TB], BF16, "ExternalInput")
    wnaq_d = C.dram("wnaq", [D, 64], F32, "ExternalInput")
    wnak_d = C.dram("wnak", [D, 64], F32, "ExternalInput")
    wnav_d = C.dram("wnav", [D, 64], F32, "ExternalInput")
    nabias_d = C.dram("nabias", [5, 128, 640], F32, "ExternalInput")
    namask_d = C.dram("namask", [5, 128, 640], F32, "ExternalInput")
    wq_d = C.dram("wq", [D, 128], F32, "ExternalInput")
    wk_d = C.dram("wk", [D, 128], F32, "ExternalInput")
    wv_d = C.dram("wv", [D, 128], F32, "ExternalInput")
    wzab_d = C.dram("wzab", [D, 132], F32, "ExternalInput")
    conv_d = C.dram("conv", [3, 128, 5], F32, "ExternalInput")
    dnpar_d = C.dram("dnpar", [128, 4], F32, "ExternalInput")
    normw_d = C.dram("normw", [128, 128], F32, "ExternalInput")
    cos_d = C.dram("cosT", [128, SEQ], F32, "ExternalInput")
    sin_d = C.dram("sinT", [128, SEQ], F32, "ExternalInput")
    cm_d = C.dram("cm", [5, 128, 128], F32, "ExternalInput")
    cmb_d = C.dram("cmb", [6, 128, 128], BF16, "ExternalInput")
    identb_d = C.dram("identb", [128, 128], BF16, "ExternalInput")
    identf_d = C.dram("identf", [128, 128], F32, "ExternalInput")
    naT_d = C.dram("naT", [NB, 64, TB], BF16, "ExternalOutput")
    dnT_d = C.dram("dnT", [NB, 128, TB], BF16, "ExternalOutput")
    pp = PsumPool(C)
    identb, identf, ones_f = load_consts(C, identb_d, identf_d)
    for b in range(nbatch):
        if do_na:
            phase_NA(C, pp, b, hT_full_d, wnaq_d, wnak_d, wnav_d, nabias_d, namask_d, naT_d, identb)
        if do_dn:
            phase_DN(C, pp, b, hT_full_d, wq_d, wk_d, wv_d, wzab_d, conv_d, dnpar_d, normw_d, cos_d, sin_d,
                     cm_d, cmb_d, dnT_d, identb, identf, ones_f)
    _finish(C)
    return nc


def _rep(v, n=128):
    return np.ascontiguousarray(np.broadcast_to(np.asarray(v, np.float32)[None], (n,) + tuple(v.shape)))


def _run(nc, in_maps):
    res = run_bass_kernel_spmd(nc, in_maps, core_ids=list(range(len(in_maps))))
    return res.results


def _assemble_hT(hT_sh):
    out = np.empty((NB, D, TB), NPBF)
    for c in range(8):
        b, q = c // 4, c % 4
        out[b, :, CTX + 2048 * q:CTX + 2048 * (q + 1)] = hT_sh[c][:, :2048]
        out[b, :, 64 * q:64 * (q + 1)] = hT_sh[c][:, 2048:]
    return out


def _reshard_mixer(dnT, naT):
    dn_all, na_all = [], []
    for c in range(8):
        b, q = c // 4, c % 4
        cols = np.r_[CTX + 2048 * q:CTX + 2048 * (q + 1), 64 * q:64 * (q + 1)]
        dn_all.append(np.ascontiguousarray(np.stack([dnT[hd][b][:, cols] for hd in range(8)])))
        na = np.stack([naT[hd][b][:, cols] for hd in range(8)])
        na_all.append(np.ascontiguousarray(na.reshape(4, 128, OWN)))
    return dn_all, na_all


def kernel_unfused(**inp):
    inp = {k: np.asarray(v) for k, v in inp.items()}
    K = b_consts()
    x, ctx = inp["x"], inp["ctx"]
    xs, cvT = [], []
    for c in range(8):
        b, q = c // 4, c % 4
        xs.append(np.ascontiguousarray(np.concatenate([x[b, 2048 * q:2048 * (q + 1)], ctx[b, 64 * q:64 * (q + 1)]], 0)))
        cvT.append(np.ascontiguousarray(np.stack([inp["c"][b].reshape(8, 128).T, inp["c_ctx"].reshape(8, 128).T], 1)
                                        .astype(np.float32)))
    ada_w = [np.ascontiguousarray(inp["ada_w"][l:l + 1]) for l in range(2)]
    ada_b = [_rep(inp["ada_b"][l])[None] for l in range(2)]

    ncA = build_A()
    res = _run(ncA, [dict(xs=xs[c], cvT=cvT[c], ada_w=ada_w[0], ada_b=ada_b[0], gpre=_rep(inp["norm_mix_pre"][0]),
                          identb=K["identb"]) for c in range(8)])
    hT_sh = [r["hT"] for r in res]
    ncB = build_B()
    for l in range(2):
        hT_full = _assemble_hT(hT_sh)
        res = _run(ncB, [host_inputs_B(inp, l, hd, hT_full)[0] for hd in range(8)])
        dn_all, na_all = _reshard_mixer([r["dnT"] for r in res], [r["naT"] for r in res])
        moe = (l % 2 == 1)
        last = (l == 1)
        ncC = build_C(moe, not last)
        w_in = inp["w_in"][l]
        common = dict(ada_w=ada_w[l], ada_b=ada_b[l], gpost=_rep(inp["norm_mix_post"][l]), gpre2=_rep(inp["norm_ffn_pre"][l]),
                      gpost2=_rep(inp["norm_ffn_post"][l]), wg=np.ascontiguousarray(w_in[:, OFF_GD:OFF_GD + 2048]),
                      wpa=inp["w_branch_dn"][l], wpb=inp["w_branch_na"][l], wout=inp["w_out"][l],
                      identb=K["identb"], identf=K["identf"])
        if moe:
            common.update(router=inp["moe_router"][l // 2], w1=inp["moe_w1"][l // 2], w3=inp["moe_w3"][l // 2],
                          w2=inp["moe_w2"][l // 2])
        else:
            common.update(w1=inp["ffn_w1"][l // 2:l // 2 + 1], w3=inp["ffn_w3"][l // 2:l // 2 + 1],
                          w2=inp["ffn_w2"][l // 2:l // 2 + 1])
        if not last:
            common.update(ada_w_n=ada_w[l + 1], ada_b_n=ada_b[l + 1], gpre_n=_rep(inp["norm_mix_pre"][l + 1]))
        res = _run(ncC, [dict(common, xs=xs[c], hT=hT_sh[c], dnT=dn_all[c], naT=na_all[c], cvT=cvT[c]) for c in range(8)])
        xs = [r["xs_out"] for r in res]
        if not last:
            hT_sh = [r["hT_n"] for r in res]
    out = np.empty((NB, SEQ, D), np.float32)
    for c in range(8):
        b, q = c // 4, c % 4
        out[b, 2048 * q:2048 * (q + 1)] = xs[c][:2048]
    return out


def mods_to_dram(C, pp, l, cvT_d, ada_w_d, ada_b_d, ones_f, mods_d):
    for grp in ([0, 1, 2], [3, 4, 5]):
        with contextlib.ExitStack() as es:
            m = compute_mods(C, pp, l, grp, cvT_d, ada_w_d, ada_b_d, ones_f, es)
            for blk in grp:
                C.dma(mods_d[l, blk], m[blk][:], ["mod%d" % blk], ["modsd"], eng="sync")
            C.flush()


def load_mods(C, mods_dl, blocks, es):
    out = {}
    for blk in blocks:
        t = C.sb("mod%d" % blk, [128, 2, 1024], F32, es)
        C.dma(t[:], mods_dl[blk], [], ["mod%d" % blk], eng="sync")
        out[blk] = t
    return out


def run_A2(C, pp, xs_d, mods_dl, gpre_dl, outs, identb):
    with contextlib.ExitStack() as es:
        xs = C.sb("xs", [128, 17, D], F32, es)
        hT = C.sb("hT", [128, 8, OWN], BF16, es)
        gpre = C.sb("gpre", [128, D], F32, es)
        C.dma(xs[:, 0:8, :], xs_d[0:1024, :].rearrange("(t p) d -> p t d", p=128), [], ["xs"], eng="sync")
        C.dma(xs[:, 8:16, :], xs_d[1024:2048, :].rearrange("(t p) d -> p t d", p=128), [], ["xs"], eng="gpsimd")
        C.dma(xs[0:64, 16, :], xs_d[2048:2112, :], [], ["xs"], eng="gpsimd")
        C.dma(gpre[:], gpre_dl, [], ["gpre"], eng="sync")
        mods = load_mods(C, mods_dl, [0, 1], es)
        phase_A(C, pp, xs, hT, mods, gpre, identb, es)
        for (dst, c0, c1) in outs:
            C.dma(dst, hT[:, :, c0:c1], ["hT"], ["hT_out"], eng="sync")
        C.flush()


def stage_C(C, pp, moe, xs_in_d, xs_mid_d, xs_out_d, hT_d, dnT_d, naT_d, colmap, mods_dl, G, W, identb, identf):
    with contextlib.ExitStack() as es1:
        h2T_all = C.sb("h2T", [128, 8, OWN], BF16, es1)
        gates = C.sb("gates", [128, 17, 8], F32, es1) if moe else None
        gpost2 = C.sb("gpost2", [128, D], F32, es1)
        C.dma(gpost2[:], G["gpost2"], [], ["gpost2"], eng="sync")
        with contextlib.ExitStack() as es2:
            mods = load_mods(C, mods_dl, [2, 3, 4], es2)
            gpost = C.sb("gpost", [128, D], F32, es2)
            gpre2 = C.sb("gpre2", [128, D], F32, es2)
            C.dma(gpost[:], G["gpost"], [], ["gpost"], eng="sync")
            C.dma(gpre2[:], G["gpre2"], [], ["gpre2"], eng="sync")
            yT_all = C.sb("yT", [128, 8, OWN], BF16, es2)
            phase_C1(C, pp, hT_d, dnT_d, naT_d, W["wg"], W["wpa"], W["wpb"], yT_all, es2, colmap=colmap)
            phase_C2(C, pp, yT_all, W["wout"], xs_in_d, xs_mid_d, mods, gpost, gpre2, h2T_all, identb, identf,
                     W.get("router"), gates, es2)
            C.flush()
        with contextlib.ExitStack() as es3:
            acc = C.sb("acc", [128, 17, D], F32, es3)
            phase_C3(C, pp, h2T_all, W["w1"], W["w3"], W["w2"], (NE if moe else 1), (DFE if moe else DFF), gates, acc, es3)
            mods5 = load_mods(C, mods_dl, [5], es3)
            phase_C4(C, pp, acc, xs_mid_d, xs_out_d, mods5, gpost2, 17, es3)
            C.flush()
        C.flush()


def select4(C, sel, jobs, width, dt):
    with contextlib.ExitStack() as es:
        src = [[C.sb("selsrc", [128, width], dt, es) for _ in range(4)] for _ in range(2)]
        acc = [C.sb("selacc", [128, width], dt, es) for _ in range(2)]
        for ji, (dst, rows, slots) in enumerate(jobs):
            i = ji % 2
            for s in range(4):
                for (ap, c0, c1, r0, r1) in slots[s]:
                    C.dma(src[i][s][r0:r1, c0:c1], ap, [], ["selsrc%d_%d" % (i, s)], eng="sync")
            C.ts(acc[i][:rows, :], src[i][0][:rows, :], sel[:rows, 0:1], ALU.mult, ["selsrc%d_0" % i, "sel"], ["selacc%d" % i])
            for s in range(1, 4):
                C.stt(acc[i][:rows, :], src[i][s][:rows, :], sel[:rows, s:s + 1], acc[i][:rows, :], ALU.mult, ALU.add,
                      ["selsrc%d_%d" % (i, s), "sel", "selacc%d" % i], ["selacc%d" % i])
            C.dma(dst, acc[i][:rows, :], ["selacc%d" % i], ["seldst"], eng="sync")
        C.flush()


def build_fused(n_heads=8):
    nc = bass.Bass("TRN2", target_bir_lowering=False)
    C = Ctx(nc)
    I = "ExternalInput"
    xb_d = C.dram("xb", [SEQ, D], F32, I)
    ctxb_d = C.dram("ctxb", [CTX, D], F32, I)
    cvT_d = C.dram("cvT", [128, 2, 8], F32, I)
    sel_d = C.dram("sel", [128, 4], F32, I)
    ada_w_d = C.dram("ada_w", [2, D, 6 * D], F32, I)
    ada_b_d = C.dram("ada_b", [2, 128, 6 * D], F32, I)
    gains_d = C.dram("gains", [2, 4, 128, D], F32, I)
    w_in_d = C.dram("w_in", [2, D, D_IN], F32, I)
    conv_d = C.dram("dn_conv", [2, 3072, 5], F32, I)
    dnpar_d = C.dram("dnpar", [2, 8, 128, 4], F32, I)
    normw_d = C.dram("normw", [2, 128, 128], F32, I)
    nabias_d = C.dram("nabias", [2, 8, 5, 128, 640], F32, I)
    namask_d = C.dram("namask", [5, 128, 640], F32, I)
    cos_d = C.dram("cosT", [128, SEQ], F32, I)
    sin_d = C.dram("sinT", [128, SEQ], F32, I)
    cm_d = C.dram("cm", [5, 128, 128], F32, I)
    cmb_d = C.dram("cmb", [6, 128, 128], BF16, I)
    identb_d = C.dram("identb", [128, 128], BF16, I)
    identf_d = C.dram("identf", [128, 128], F32, I)
    wbd_d = C.dram("w_branch_dn", [2, D, D], F32, I)
    wbn_d = C.dram("w_branch_na", [2, 512, D], F32, I)
    wout_d = C.dram("w_out", [2, D, D], F32, I)
    f1_d = C.dram("ffn_w1", [1, D, DFF], F32, I)
    f3_d = C.dram("ffn_w3", [1, D, DFF], F32, I)
    f2_d = C.dram("ffn_w2", [1, DFF, D], F32, I)
    rt_d = C.dram("moe_router", [1, D, NE], F32, I)
    m1_d = C.dram("moe_w1", [1, NE, D, DFE], F32, I)
    m3_d = C.dram("moe_w3", [1, NE, D, DFE], F32, I)
    m2_d = C.dram("moe_w2", [1, NE, DFE, D], F32, I)
    out_d = C.dram("xs_out", [OWN, D], F32, "ExternalOutput")
    N = "Internal"
    xwork = C.dram("xwork", [4, OWN, D], F32, N)
    xmid = C.dram("xmid", [OWN, D], F32, N)
    hT_full = C.dram("hT_full", [1, D, TB], BF16, N)
    hT_q = C.dram("hT_q", [4, D, OWN], BF16, N)
    dnT_all = C.dram("dnT_all", [8, 128, TB], BF16, N)
    naT_all = C.dram("naT_all", [8, 64, TB], BF16, N)
    mods_d = C.dram("mods", [2, 6, 128, 2, 1024], F32, N)
    xs_sel = C.dram("xs_sel", [OWN, D], F32, N)
    hT_sel = C.dram("hT_sel", [D, OWN], BF16, N)
    dn_sel = C.dram("dn_sel", [8, 128, OWN], BF16, N)
    na_sel = C.dram("na_sel", [4, 128, OWN], BF16, N)

    pp = PsumPool(C)
    identb, identf, ones_f = load_consts(C, identb_d, identf_d)
    sel = C.sb("sel", [128, 4], F32)
    C.dma(sel[:], sel_d, [], ["sel"], eng="sync")
    for s in range(4):
        C.dma(xwork[s][0:2048, :], xb_d[2048 * s:2048 * (s + 1), :], [], ["xw%d" % s], eng="sync")
        C.dma(xwork[s][2048:2112, :], ctxb_d[64 * s:64 * (s + 1), :], [], ["xw%d" % s], eng="sync")
    C.flush()
    hfv = hT_full[0].rearrange("(k p) t -> p k t", p=128)
    for l in range(2):
        moe = (l % 2 == 1)
        mods_to_dram(C, pp, l, cvT_d, ada_w_d, ada_b_d, ones_f, mods_d)
        for s in range(4):
            outs = [(hT_q[s].rearrange("(k p) t -> p k t", p=128), 0, OWN),
                    (hfv[:, :, CTX + 2048 * s:CTX + 2048 * (s + 1)], 0, 2048),
                    (hfv[:, :, 64 * s:64 * (s + 1)], 2048, OWN)]
            run_A2(C, pp, xwork[s], mods_d[l], gains_d[l, 0], outs, identb)
        wl = w_in_d[l]
        for hd in range(n_heads):
            cs = lambda o, n: wl[:, o + hd * n:o + (hd + 1) * n]
            phase_NA(C, pp, 0, hT_full, cs(OFF_NQ, 64), cs(OFF_NK, 64), cs(OFF_NV, 64), nabias_d[l, hd], namask_d,
                     naT_all[hd:hd + 1], identb)
            convv = conv_d[l].rearrange("(x c) k -> x c k", x=3)[:, hd * 128:(hd + 1) * 128, :]
            phase_DN(C, pp, 0, hT_full, cs(OFF_Q, 128), cs(OFF_K, 128), cs(OFF_V, 128), (cs(OFF_Z, 128), wl[:, OFF_AB:OFF_AB + 32], hd), convv,
                     dnpar_d[l, hd], normw_d[l], cos_d, sin_d, cm_d, cmb_d, dnT_all[hd:hd + 1], identb, identf, ones_f)
        G = dict(gpost=gains_d[l, 1], gpre2=gains_d[l, 2], gpost2=gains_d[l, 3])
        W = dict(wg=wl[:, OFF_GD:OFF_GD + 2048], wpa=wbd_d[l], wpb=wbn_d[l], wout=wout_d[l])
        if not moe:
            W.update(w1=f1_d, w3=f3_d, w2=f2_d)
            for s in range(4):
                colmap = (lambda tt, s=s: (CTX + 2048 * s + tt * 512) if tt < 4 else 64 * s)
                stage_C(C, pp, False, xwork[s], xmid, xwork[s], hT_q[s], dnT_all, naT_all, colmap, mods_d[l], G, W,
                        identb, identf)
        else:
            W.update(w1=m1_d[0], w3=m3_d[0], w2=m2_d[0], router=rt_d[0])
            jobs = []
            for t in range(17):
                rows = 128 if t < 16 else 64
                jobs.append((xs_sel[t * 128:t * 128 + rows, :], rows,
                             [[(xwork[s][t * 128:t * 128 + rows, :], 0, D, 0, rows)] for s in range(4)]))
            select4(C, sel, jobs, D, F32)
            jobs = []
            for k in range(8):
                jobs.append((hT_sel[k * 128:(k + 1) * 128, :], 128,
                             [[(hT_q[s][k * 128:(k + 1) * 128, :], 0, OWN, 0, 128)] for s in range(4)]))
            for h in range(8):
                jobs.append((dn_sel[h], 128,
                             [[(dnT_all[h][:, CTX + 2048 * s:CTX + 2048 * (s + 1)], 0, 2048, 0, 128),
                               (dnT_all[h][:, 64 * s:64 * (s + 1)], 2048, OWN, 0, 128)] for s in range(4)]))
            for j in range(4):
                slots = []
                for s in range(4):
                    sl_ = []
                    for tw in range(2):
                        src_h = naT_all[2 * j + tw]
                        sl_.append((src_h[:, CTX + 2048 * s:CTX + 2048 * (s + 1)], 0, 2048, tw * 64, tw * 64 + 64))
                        sl_.append((src_h[:, 64 * s:64 * (s + 1)], 2048, OWN, tw * 64, tw * 64 + 64))
                    slots.append(sl_)
                jobs.append((na_sel[j], 128, slots))
            select4(C, sel, jobs, OWN, BF16)
            stage_C(C, pp, True, xs_sel, xmid, out_d, hT_sel, dn_sel, na_sel, None, mods_d[l], G, W, identb, identf)
    _finish(C)
    return nc


def fused_inputs(inp):
    K = b_consts()
    ri, ci = K["ri"], K["ci"]
    nabias = np.ascontiguousarray(np.stack([np.stack([inp["na_rpb"][l, hd][ri, ci] for hd in range(8)]) for l in range(2)])
                                  ).astype(np.float32)
    dnpar = np.empty((2, 8, 128, 4), np.float32)
    for l in range(2):
        for hd in range(8):
            dnpar[l, hd] = np.array([inp["dn_a_log"][l, 0, hd], inp["dn_a_log"][l, 1, hd],
                                     inp["dn_dt_bias"][l, 0, hd], inp["dn_dt_bias"][l, 1, hd]], np.float32)[None]
    gains = np.stack([np.stack([_rep(inp[k][l]) for k in ("norm_mix_pre", "norm_mix_post", "norm_ffn_pre", "norm_ffn_post")])
                      for l in range(2)])
    shared = dict(
        ada_w=np.ascontiguousarray(inp["ada_w"]), ada_b=np.stack([_rep(inp["ada_b"][l]) for l in range(2)]),
        gains=np.ascontiguousarray(gains), w_in=np.ascontiguousarray(inp["w_in"]), dn_conv=np.ascontiguousarray(inp["dn_conv"]),
        dnpar=dnpar, normw=np.stack([_rep(inp["dn_norm"][l]) for l in range(2)]), nabias=nabias, namask=K["mk"],
        cosT=K["cosT"], sinT=K["sinT"], cm=K["cm"], cmb=K["cmb"], identb=K["identb"], identf=K["identf"],
        w_branch_dn=inp["w_branch_dn"], w_branch_na=inp["w_branch_na"], w_out=inp["w_out"],
        ffn_w1=inp["ffn_w1"], ffn_w3=inp["ffn_w3"], ffn_w2=inp["ffn_w2"], moe_router=inp["moe_router"],
        moe_w1=inp["moe_w1"], moe_w3=inp["moe_w3"], moe_w2=inp["moe_w2"])
    shared = {k: np.ascontiguousarray(v) for k, v in shared.items()}
    maps = []
    for c in range(8):
        b, q = c // 4, c % 4
        selv = np.zeros((128, 4), np.float32)
        selv[:, q] = 1.0
        m = dict(shared)
        m.update(xb=np.ascontiguousarray(inp["x"][b]), ctxb=np.ascontiguousarray(inp["ctx"][b]),
                 cvT=np.ascontiguousarray(np.stack([inp["c"][b].reshape(8, 128).T, inp["c_ctx"].reshape(8, 128).T], 1)
                                          .astype(np.float32)), sel=selv)
        maps.append(m)
    return maps


def kernel_fused(**inp):
    inp = {k: np.asarray(v) for k, v in inp.items()}
    nc = build_fused()
    res = _run(nc, fused_inputs(inp))
    out = np.empty((NB, SEQ, D), np.float32)
    for c in range(8):
        b, q = c // 4, c % 4
        out[b, 2048 * q:2048 * (q + 1)] = res[c]["xs_out"][:2048]
    return out


def kernel(**inp):
    return kernel_fused(**inp)
```

```python
import contextlib
import os
import numpy as np
import ml_dtypes
import concourse.bass as bass
import concourse.mybir as mybir
from concourse.bass_utils import run_bass_kernel_spmd

F32 = mybir.dt.float32
BF16 = mybir.dt.bfloat16
AF = mybir.ActivationFunctionType
ALU = mybir.AluOpType
AX = mybir.AxisListType
NPBF = ml_dtypes.bfloat16

D = 1024
NB = 2
SEQ = 8192
CTX = 256
TB = CTX + SEQ
NT_B = TB // 128
OWN = 2048 + 64
EPS = 1e-6
DFF = 2816
NE = 8
DFE = 3584
D_IN = 7712
NEG = -30000.0

ENGINES = ("sync", "tensor", "vector", "scalar", "gpsimd")
EPOCH = 30000
DMA_K = 8


class Prog:
    def __init__(self, nc, n_sems=140):
        self.nc = nc
        self.es = contextlib.ExitStack()
        self.sem_next = 0
        self.streams = {e: [] for e in ENGINES}
        self.cnt = {e: 0 for e in ENGINES}
        self.sem = {e: self._new_sem() for e in ENGINES}
        self.known = {e: {} for e in ENGINES}
        self.last_write = {}
        self.readers = {}
        self.dma_sems = {}
        self.dma_cnt = {}
        self.ninst = 0

    def _new_sem(self):
        s = self.es.enter_context(self.nc.semaphore("s%d" % self.sem_next))
        self.sem_next += 1
        return s

    def _wait(self, eng, tok):
        s, v = tok
        k = self.known[eng]
        if k.get(id(s), 0) >= v:
            return
        k[id(s)] = v
        self.streams[eng].append(lambda e, s=s, v=v: e.wait_ge(s, v))

    def _deps(self, eng, reads, writes):
        toks = []
        for r in reads:
            t = self.last_write.get(r)
            if t is not None:
                toks.append(t)
        for w in writes:
            t = self.last_write.get(w)
            if t is not None:
                toks.append(t)
            toks.extend(self.readers.get(w, {}).values())
        own = self.sem[eng]
        for t in toks:
            if eng == "tensor" and t[0] is own:
                continue
            self._wait(eng, t)

    def _record(self, tok, reads, writes):
        for w in writes:
            self.last_write[w] = tok
            self.readers[w] = {}
        for r in reads:
            if r in writes:
                continue
            self.readers.setdefault(r, {})[id(tok[0])] = tok

    def op(self, eng, fn, reads=(), writes=()):
        reads = tuple(reads)
        writes = tuple(writes) + tuple(r for r in reads if r.startswith("psum"))
        self._deps(eng, reads, writes)
        if self.cnt[eng] >= EPOCH:
            self.sem[eng] = self._new_sem()
            self.cnt[eng] = 0
        self.cnt[eng] += 1
        s, v = self.sem[eng], self.cnt[eng]
        self.streams[eng].append(lambda e, s=s: fn(e).then_inc(s, 1))
        self._record((s, v), reads, writes)
        self.ninst += 1
        return (s, v)

    def dma(self, eng, fn, reads=(), writes=()):
        reads = tuple(reads)
        writes = tuple(writes)
        self._deps(eng, reads, writes)
        if eng not in self.dma_sems:
            self.dma_sems[eng] = [self._new_sem() for _ in range(DMA_K)]
            self.dma_cnt[eng] = 0
        i = self.dma_cnt[eng]
        self.dma_cnt[eng] += 1
        s = self.dma_sems[eng][i % DMA_K]
        tgt = 16 * (i // DMA_K + 1)
        if tgt > 16:
            self._wait(eng, (s, tgt - 16))
        self.streams[eng].append(lambda e, s=s: fn(e).then_inc(s, 16))
        self._record((s, tgt), reads, writes)
        self.ninst += 1
        return (s, tgt)

    def finish(self, eng="sync"):
        for t in list(self.last_write.values()):
            self._wait(eng, t)

    def build(self):
        nc = self.nc
        with nc.Block() as block:
            for e in ENGINES:
                stream = self.streams[e]
                if not stream:
                    continue

                def body(eng, stream=stream):
                    for f in stream:
                        f(eng)
                getattr(block, e)(body)
        self.streams = {e: [] for e in ENGINES}


class Ctx:
    def __init__(self, nc):
        self.nc = nc
        self.P = Prog(nc)
        self.es = contextlib.ExitStack()
        self.nps = 0
        self.dmaq = 0
        self.uid = 0

    def sb(self, name, shape, dt, es=None):
        self.uid += 1
        return (es or self.es).enter_context(self.nc.sbuf_tensor("%s_%d" % (name, self.uid), list(shape), dt))

    def psum(self, name, shape, dt, es=None):
        self.uid += 1
        return (es or self.es).enter_context(self.nc.psum_tensor("%s_%d" % (name, self.uid), list(shape), dt))

    def flush(self):
        if self.P.ninst == getattr(self, "_flushed_at", -1):
            return
        self.P.finish()
        self.P.build()
        self._flushed_at = self.P.ninst

    def dram(self, name, shape, dt, kind):
        return self.nc.dram_tensor(name, list(shape), dt, kind=kind).ap()

    def mm(self, out, lhsT, rhs, start, stop, r, w):
        return self.P.op("tensor", lambda e: e.matmul(out, lhsT=lhsT, rhs=rhs, start=start, stop=stop), r, w)

    def tr(self, out, in_, ident, r, w):
        return self.P.op("tensor", lambda e: e.transpose(out, in_, ident), r, w)

    def act(self, out, in_, func, r, w, bias=None, scale=None, accum_out=None, eng="scalar"):
        kw = {}
        if bias is not None:
            kw["bias"] = bias
        if scale is not None:
            kw["scale"] = scale
        if accum_out is not None:
            kw["accum_out"] = accum_out
        return self.P.op("scalar", lambda e: e.activation(out=out, in_=in_, func=func, **kw), r, w)

    def tt(self, out, in0, in1, op, r, w, eng="vector"):
        return self.P.op(eng, lambda e: e.tensor_tensor(out=out, in0=in0, in1=in1, op=op), r, w)

    def ts(self, out, in0, s1, op0, r, w, s2=None, op1=None, eng="vector", accum_out=None):
        kw = {}
        if op1 is not None:
            kw["op1"] = op1
        if accum_out is not None:
            kw["accum_out"] = accum_out
        return self.P.op(eng, lambda e: e.tensor_scalar(out=out, in0=in0, scalar1=s1, scalar2=s2, op0=op0, **kw), r, w)

    def stt(self, out, in0, scalar, in1, op0, op1, r, w):
        return self.P.op("vector", lambda e: e.scalar_tensor_tensor(out=out, in0=in0, scalar=scalar, in1=in1,
                                                                   op0=op0, op1=op1), r, w)

    def cp(self, out, in_, r, w, eng="vector"):
        if eng == "scalar":
            return self.P.op("scalar", lambda e: e.copy(out=out, in_=in_), r, w)
        return self.P.op(eng, lambda e: e.tensor_copy(out=out, in_=in_), r, w)

    def memset(self, ap, val, w, eng="vector"):
        return self.P.op(eng, lambda e: e.memset(ap, val), (), w)

    def recip(self, out, in_, r, w):
        return self.P.op("vector", lambda e: e.reciprocal(out=out, in_=in_), r, w)

    def red(self, out, in_, op, r, w, axis=AX.X):
        return self.P.op("vector", lambda e: e.tensor_reduce(out=out, in_=in_, axis=axis, op=op), r, w)

    def dma(self, out, in_, r, w, eng=None):
        if eng is None:
            eng = ("sync", "gpsimd")[self.dmaq % 2]
            self.dmaq += 1
        return self.P.dma(eng, lambda e: e.dma_start(out=out, in_=in_), r, w)


class PsumPool:
    def __init__(self, C, n=8):
        self.C = C
        self.t = [C.psum("psb", [128, 512], F32) for _ in range(n)]
        self.i = 0
        self.n = n

    def get(self):
        i = self.i
        self.i = (self.i + 1) % self.n
        return self.t[i], "psum%d" % i


def rstd_from_ss(C, ss, rs, n, key, inv_n):
    C.ts(rs, ss, inv_n, ALU.mult, [key], [key + "_r"], s2=EPS, op1=ALU.add)
    C.act(rs, rs, AF.Sqrt, [key + "_r"], [key + "_r"])
    C.recip(rs, rs, [key + "_r"], [key + "_r"])


def compute_mods(C, pp, l, blocks, cvT_d, ada_w_d, ada_b_d, ones_f, es):
    out = {}
    for blk in blocks:
        out[blk] = C.sb("mod%d" % blk, [128, 2, 1024], F32, es)
    with contextlib.ExitStack() as tes:
        cv = C.sb("cv", [128, 2, 8], F32, tes)
        C.dma(cv[:], cvT_d, [], ["cv"], eng="sync")
        C.act(cv[:], cv[:], AF.Silu, ["cv"], ["cv"])
        rep = C.sb("rep", [128, 2, 8, 128], F32, tes)
        for s in range(2):
            for k in range(8):
                C.ts(rep[:, s, k, :], ones_f[:], cv[:, s, k:k + 1], ALU.mult, ["cv", "ones_f"], ["rep"])
        wt = [C.sb("adaw", [128, 8, 512], F32, tes) for _ in range(2)]
        bt = [C.sb("adab", [128, 512], F32, tes) for _ in range(2)]
        it = 0
        for blk in blocks:
            m = out[blk]
            for half in range(2):
                col = blk * 1024 + half * 512
                w = wt[it % 2]
                bb = bt[it % 2]
                wk = "adaw%d" % (it % 2)
                it += 1
                C.dma(w[:], ada_w_d[l].rearrange("(k p) n -> p k n", p=128)[:, :, col:col + 512], [], [wk], eng="sync")
                C.dma(bb[:], ada_b_d[l][:, col:col + 512], [], [wk + "b"], eng="sync")
                for s in range(2):
                    ps, pk = pp.get()
                    for k in range(8):
                        C.mm(ps[:], rep[:, s, k, :], w[:, k, :], k == 0, k == 7, ["rep", wk], [pk])
                    C.tt(m[:, s, half * 512:(half + 1) * 512], ps[:], bb[:], ALU.add, [pk, wk + "b"], ["mod%d" % blk])
        C.flush()
    return out


def phase_A(C, pp, xs, hT, mods, gpre, ident, es):
    M1 = C.sb("M1", [128, 2, 1024], F32, es)
    for s in range(2):
        C.stt(M1[:, s, :], mods[1][:, s, :], 1.0, gpre[:], ALU.add, ALU.mult, ["mod1", "gpre"], ["M1"])
    SH = mods[0]
    ss = C.sb("ssA", [128, 17], F32, es)
    rs = C.sb("rsA", [128, 17], F32, es)
    junk = C.sb("junkA", [128, 1024], F32, es)
    C.memset(ss[:], 1.0, ["ssA"])
    for t in range(17):
        rows = 128 if t < 16 else 64
        C.act(junk[:rows, :], xs[:rows, t, :], AF.Square, ["xs"], ["junkA", "ssA"], accum_out=ss[:rows, t:t + 1])
    C.ts(rs[:], ss[:], 1.0 / D, ALU.mult, ["ssA"], ["rsA"], s2=EPS, op1=ALU.add)
    C.act(rs[:], rs[:], AF.Sqrt, ["rsA"], ["rsA"])
    C.recip(rs[:], rs[:], ["rsA"], ["rsA"])
    tmp = [C.sb("tmpA", [128, 1024], F32, es) for _ in range(2)]
    hb = [C.sb("hbA", [128, 1024], BF16, es) for _ in range(2)]
    for t in range(17):
        rows = 128 if t < 16 else 64
        s = 0 if t < 16 else 1
        i = t % 2
        C.stt(tmp[i][:rows, :], xs[:rows, t, :], rs[:rows, t:t + 1], M1[:rows, s, :], ALU.mult, ALU.mult,
              ["xs", "rsA", "M1"], ["tmpA%d" % i])
        C.tt(hb[i][:rows, :], tmp[i][:rows, :], SH[:rows, s, :], ALU.add, ["tmpA%d" % i, "mod0"], ["hbA%d" % i],
             eng="gpsimd")
        ps, pk = pp.get()
        psb = ps[:].bitcast(BF16)
        for k in range(8):
            C.tr(psb[:, k * 128:k * 128 + rows], hb[i][:rows, k * 128:(k + 1) * 128], ident[:rows, :rows],
                 ["hbA%d" % i, "ident"], [pk])
        C.cp(hT[:, :, t * 128:t * 128 + rows],
             psb.rearrange("p (k t) -> p k t", k=8)[:, :, 0:rows], [pk], ["hT"], eng="scalar")


def phase_C1(C, pp, hT_d, dnT_d, naT_d, wg_d, wpa_d, wpb_d, yT_all, es0, colmap=None):
    with contextlib.ExitStack() as es:
        wg = C.sb("wg", [128, 8, 2048], BF16, es)
        wpa = C.sb("wpa", [128, 8, 1024], BF16, es)
        wpb = C.sb("wpb", [128, 4, 1024], BF16, es)
        for k in range(8):
            C.dma(wg[:, k, :], wg_d[k * 128:(k + 1) * 128, :], [], ["wg"], eng="gpsimd")
            C.dma(wpa[:, k, :], wpa_d[k * 128:(k + 1) * 128, :], [], ["wpa"], eng="gpsimd")
        for k in range(4):
            C.dma(wpb[:, k, :], wpb_d[k * 128:(k + 1) * 128, :], [], ["wpb"], eng="gpsimd")
        hTt = [C.sb("hTt", [128, 8, 512], BF16, es) for _ in range(2)]
        dnt = [C.sb("dnt", [128, 8, 512], BF16, es) for _ in range(2)]
        nat = [C.sb("nat", [128, 4, 512], BF16, es) for _ in range(2)]
        s1 = [C.sb("s1", [128, 512], F32, es) for _ in range(2)]
        s2 = [C.sb("s2", [128, 512], F32, es) for _ in range(2)]
        for tt in range(5):
            n = 512 if tt < 4 else 64
            t0 = tt * 512
            i = tt % 2
            C.dma(hTt[i][:, :, :n], hT_d.rearrange("(k p) t -> p k t", p=128)[:, :, t0:t0 + n], [], ["hTt%d" % i], eng="sync")
            if colmap is None:
                C.dma(dnt[i][:, :, :n], dnT_d.rearrange("h p t -> p h t")[:, :, t0:t0 + n], [], ["dnt%d" % i], eng="sync")
                C.dma(nat[i][:, :, :n], naT_d.rearrange("h p t -> p h t")[:, :, t0:t0 + n], [], ["nat%d" % i], eng="sync")
            else:
                m0 = colmap(tt)
                C.dma(dnt[i][:, :, :n], dnT_d.rearrange("h p t -> p h t")[:, :, m0:m0 + n], [], ["dnt%d" % i], eng="sync")
                nav_ = naT_d.rearrange("(j two) d t -> two d j t", two=2)
                for tw in range(2):
                    C.dma(nat[i][tw * 64:(tw + 1) * 64, :, :n], nav_[tw][:, :, m0:m0 + n], [], ["nat%d" % i], eng="sync")
            for oc in range(8):
                j = oc % 2
                p1, k1 = pp.get()
                for k in range(8):
                    C.mm(p1[:, :n], wg[:, k, oc * 128:(oc + 1) * 128], hTt[i][:, k, :n], k == 0, k == 7,
                         ["wg", "hTt%d" % i], [k1])
                p2, k2 = pp.get()
                for k in range(8):
                    C.mm(p2[:, :n], wg[:, k, 1024 + oc * 128:1024 + (oc + 1) * 128], hTt[i][:, k, :n], k == 0, k == 7,
                         ["wg", "hTt%d" % i], [k2])
                p3, k3 = pp.get()
                for k in range(8):
                    C.mm(p3[:, :n], wpa[:, k, oc * 128:(oc + 1) * 128], dnt[i][:, k, :n], k == 0, k == 7,
                         ["wpa", "dnt%d" % i], [k3])
                p4, k4 = pp.get()
                for k in range(4):
                    C.mm(p4[:, :n], wpb[:, k, oc * 128:(oc + 1) * 128], nat[i][:, k, :n], k == 0, k == 3,
                         ["wpb", "nat%d" % i], [k4])
                C.act(s1[j][:, :n], p1[:, :n], AF.Sigmoid, [k1], ["s1%d" % j])
                C.act(s2[j][:, :n], p2[:, :n], AF.Sigmoid, [k2], ["s2%d" % j])
                C.tt(s1[j][:, :n], s1[j][:, :n], p3[:, :n], ALU.mult, ["s1%d" % j, k3], ["s1%d" % j])
                C.tt(s2[j][:, :n], s2[j][:, :n], p4[:, :n], ALU.mult, ["s2%d" % j, k4], ["s2%d" % j])
                C.tt(yT_all[:, oc, t0:t0 + n], s1[j][:, :n], s2[j][:, :n], ALU.add, ["s1%d" % j, "s2%d" % j], ["yT"],
                     eng="gpsimd")
        C.flush()


def small_rstd(C, ssum, rs, rows, inv_n, kin, kout):
    C.ts(rs[:rows, :], ssum[:rows, :], inv_n, ALU.mult, [kin], [kout], s2=EPS, op1=ALU.add)
    C.act(rs[:rows, :], rs[:rows, :], AF.Sqrt, [kout], [kout])
    C.recip(rs[:rows, :], rs[:rows, :], [kout], [kout])


def phase_C2(C, pp, yT_all, wout_d, xs_in_d, xs_mid_d, mods, gpost, gpre2, h2T_all, identb, identf,
             router_d, gates, es0):
    with contextlib.ExitStack() as es:
        wout = C.sb("wout", [128, 8, 1024], BF16, es)
        for k in range(8):
            C.dma(wout[:, k, :], wout_d[k * 128:(k + 1) * 128, :], [], ["wout"], eng="gpsimd")
        G1P = C.sb("G1P", [128, 2, 1024], F32, es)
        M2 = C.sb("M2", [128, 2, 1024], F32, es)
        for s in range(2):
            C.tt(G1P[:, s, :], mods[2][:, s, :], gpost[:], ALU.mult, ["mod2", "gpost"], ["G1P"])
            C.stt(M2[:, s, :], mods[4][:, s, :], 1.0, gpre2[:], ALU.add, ALU.mult, ["mod4", "gpre2"], ["M2"])
        SH2 = mods[3]
        moe = router_d is not None
        if moe:
            rt = C.sb("router", [128, 8, 8], F32, es)
            C.dma(rt[:], router_d.rearrange("(k p) e -> p k e", p=128), [], ["router"], eng="sync")
            lg = C.sb("logits", [128, 17, 8], F32, es)
            C.memset(lg[:], 0.0, ["logits"])
            hf = [C.sb("h2f", [128, 8, 128], F32, es) for _ in range(2)]
            for i_ in range(2):
                C.memset(hf[i_][:], 0.0, ["h2f%d" % i_])
        xt = [C.sb("xt", [128, 1024], F32, es) for _ in range(2)]
        tmp = [C.sb("tmpC", [128, 1024], F32, es) for _ in range(2)]
        hb = [C.sb("hbC", [128, 1024], BF16, es) for _ in range(2)]
        junk = C.sb("junkC", [128, 1024], F32, es)
        ssy = [C.sb("ssy", [128, 4], F32, es) for _ in range(2)]
        for t in range(17):
            rows = 128 if t < 16 else 64
            s = 0 if t < 16 else 1
            i = t % 2
            ki = "%d" % i
            C.dma(xt[i][:rows, :], xs_in_d[t * 128:t * 128 + rows, :], [], ["xt" + ki], eng="sync")
            ph = []
            for half in range(2):
                p, pk = pp.get()
                for oc in range(8):
                    C.mm(p[:rows, :], yT_all[:, oc, t * 128:t * 128 + rows], wout[:, oc, half * 512:(half + 1) * 512],
                         oc == 0, oc == 7, ["yT", "wout"], [pk])
                C.act(junk[:rows, half * 512:(half + 1) * 512], p[:rows, :], AF.Square, [pk], ["junkC", "ssy" + ki],
                      accum_out=ssy[i][:rows, half:half + 1])
                ph.append((p, pk))
            C.tt(ssy[i][:rows, 2:3], ssy[i][:rows, 0:1], ssy[i][:rows, 1:2], ALU.add, ["ssy" + ki], ["ssy" + ki])
            small_rstd(C, ssy[i][:, 2:3], ssy[i][:, 3:4], rows, 1.0 / D, "ssy" + ki, "ssy" + ki)
            for half in range(2):
                p, pk = ph[half]
                sl = slice(half * 512, (half + 1) * 512)
                C.stt(tmp[i][:rows, sl], p[:rows, :], ssy[i][:rows, 3:4], G1P[:rows, s, sl], ALU.mult, ALU.mult,
                      [pk, "ssy" + ki, "G1P"], ["tmpC" + ki])
            C.tt(xt[i][:rows, :], xt[i][:rows, :], tmp[i][:rows, :], ALU.add, ["xt" + ki, "tmpC" + ki], ["xt" + ki],
                 eng="gpsimd")
            C.dma(xs_mid_d[t * 128:t * 128 + rows, :], xt[i][:rows, :], ["xt" + ki], ["xs_mid"], eng="sync")
            C.act(junk[:rows, :], xt[i][:rows, :], AF.Square, ["xt" + ki], ["junkC", "ssy" + ki],
                  accum_out=ssy[i][:rows, 0:1])
            small_rstd(C, ssy[i][:, 0:1], ssy[i][:, 1:2], rows, 1.0 / D, "ssy" + ki, "ssy" + ki)
            C.stt(tmp[i][:rows, :], xt[i][:rows, :], ssy[i][:rows, 1:2], M2[:rows, s, :], ALU.mult, ALU.mult,
                  ["xt" + ki, "ssy" + ki, "M2"], ["tmpC" + ki])
            if not moe:
                C.tt(hb[i][:rows, :], tmp[i][:rows, :], SH2[:rows, s, :], ALU.add, ["tmpC" + ki, "mod3"], ["hbC" + ki],
                     eng="gpsimd")
                p, pk = pp.get()
                pb = p[:].bitcast(BF16)
                for k in range(8):
                    C.tr(pb[:, k * 128:k * 128 + rows], hb[i][:rows, k * 128:(k + 1) * 128], identb[:rows, :rows],
                         ["hbC" + ki, "identb"], [pk])
                C.cp(h2T_all[:, :, t * 128:t * 128 + rows], pb.rearrange("p (k t) -> p k t", k=8)[:, :, 0:rows],
                     [pk], ["h2T"], eng="scalar")
            else:
                C.tt(tmp[i][:rows, :], tmp[i][:rows, :], SH2[:rows, s, :], ALU.add, ["tmpC" + ki, "mod3"], ["tmpC" + ki],
                     eng="gpsimd")
                dbx = os.environ.get("DBGX", "")
                pa, pka = pp.get()
                pb_, pkb = pp.get()
                if "t" not in dbx:
                    for k in range(8):
                        p, pk = (pa, pka) if k < 4 else (pb_, pkb)
                        kk = k % 4
                        C.tr(p[:, kk * 128:kk * 128 + rows], tmp[i][:rows, k * 128:(k + 1) * 128], identf[:rows, :rows],
                             ["tmpC" + ki, "identf"], [pk])
                if "e" not in dbx:
                    for hh, (p, pk) in enumerate(((pa, pka), (pb_, pkb))):
                        src = p[:].rearrange("p (k t) -> p k t", k=4)[:, :, 0:rows]
                        C.cp(h2T_all[:, hh * 4:(hh + 1) * 4, t * 128:t * 128 + rows], src, [pk], ["h2T"], eng="scalar")
                        C.cp(hf[i][:, hh * 4:(hh + 1) * 4, 0:rows], src, [pk], ["h2f" + ki], eng="vector")
                if "r" not in dbx:
                    p, pk = pp.get()
                    for k in range(8):
                        C.mm(p[:, 0:8], hf[i][:, k, :], rt[:, k, :], k == 0, k == 7, ["h2f" + ki, "router"], [pk])
                    C.cp(lg[:rows, t, :], p[:rows, 0:8], [pk], ["logits"], eng="vector")
        if moe and os.environ.get("DBG", "") != "2":
            srt = C.sb("srt", [128, 8], F32, es)
            nm1 = C.sb("nm1", [128, 1], F32, es)
            msk = C.sb("msk", [128, 8], F32, es)
            ex = C.sb("ex", [128, 8], F32, es)
            den = C.sb("den", [128, 1], F32, es)
            for t in range(17):
                rows = 128 if t < 16 else 64
                C.P.op("vector", lambda e, t=t, rows=rows: e.max(out=srt[:rows, :], in_=lg[:rows, t, :]), ["logits"], ["srt"])
                C.ts(nm1[:rows, :], srt[:rows, 0:1], -1.0, ALU.mult, ["srt"], ["nm1"])
                C.ts(msk[:rows, :], lg[:rows, t, :], srt[:rows, 1:2], ALU.is_ge, ["logits", "srt"], ["msk"])
                C.act(ex[:rows, :], lg[:rows, t, :], AF.Exp, ["logits", "nm1"], ["ex"], bias=nm1[:rows, :])
                C.tt(ex[:rows, :], ex[:rows, :], msk[:rows, :], ALU.mult, ["ex", "msk"], ["ex"])
                C.red(den[:rows, :], ex[:rows, :], ALU.add, ["ex"], ["den"])
                C.recip(den[:rows, :], den[:rows, :], ["den"], ["den"])
                C.ts(gates[:rows, t, :], ex[:rows, :], den[:rows, 0:1], ALU.mult, ["ex", "den"], ["gates"])
        C.flush()


def phase_C3(C, pp, h2T_all, w1_d, w3_d, w2_d, n_exp, dff, gates, acc, es0):
    nchunks = dff // 128
    groups = []
    c0 = 0
    while c0 < nchunks:
        nch = min(4, nchunks - c0)
        groups.append((c0, nch))
        c0 += nch
    with contextlib.ExitStack() as es:
        NWB = 3 if gates is not None else 2
        w1g = [C.sb("w1g", [128, 8, 512], BF16, es) for _ in range(NWB)]
        w3g = [C.sb("w3g", [128, 8, 512], BF16, es) for _ in range(NWB)]
        w2g = [C.sb("w2g", [128, 4, 1024], BF16, es) for _ in range(NWB)]
        actT = [C.sb("actT", [128, 4, 512], BF16, es) for _ in range(2)]
        sa = [C.sb("sa", [128, 512], F32, es) for _ in range(2)]
        C.memset(acc[:], 0.0, ["acc"])
        it = 0
        for e in range(n_exp):
            for (c0, nch) in groups:
                wi = it % NWB
                it += 1
                kw = "wffn%d" % wi
                cs = slice(c0 * 128, (c0 + nch) * 128)
                w1v = w1_d[e].rearrange("(k p) n -> p k n", p=128)
                w3v = w3_d[e].rearrange("(k p) n -> p k n", p=128)
                for k in range(8):
                    C.dma(w1g[wi][:, k, 0:nch * 128], w1v[:, k, cs], [], [kw + "a"], eng="gpsimd")
                    C.dma(w3g[wi][:, k, 0:nch * 128], w3v[:, k, cs], [], [kw + "b"], eng="gpsimd")
                for j in range(nch):
                    C.dma(w2g[wi][:, j, :], w2_d[e][(c0 + j) * 128:(c0 + j + 1) * 128, :], [], [kw + "c"], eng="gpsimd")
                for tt in range(5):
                    n = 512 if tt < 4 else 64
                    t0 = tt * 512
                    ai = tt % 2
                    ka = "actT%d" % ai
                    for j in range(nch):
                        pa, pka = pp.get()
                        for k in range(8):
                            C.mm(pa[:, :n], w1g[wi][:, k, j * 128:(j + 1) * 128], h2T_all[:, k, t0:t0 + n], k == 0, k == 7,
                                 [kw + "a", "h2T"], [pka])
                        pb, pkb = pp.get()
                        for k in range(8):
                            C.mm(pb[:, :n], w3g[wi][:, k, j * 128:(j + 1) * 128], h2T_all[:, k, t0:t0 + n], k == 0, k == 7,
                                 [kw + "b", "h2T"], [pkb])
                        sj = j % 2
                        C.act(sa[sj][:, :n], pa[:, :n], AF.Silu, [pka], ["sa%d" % sj])
                        C.tt(actT[ai][:, j, :n], sa[sj][:, :n], pb[:, :n], ALU.mult, ["sa%d" % sj, pkb], [ka])
                    nsub = 4 if tt < 4 else 1
                    for sub in range(nsub):
                        t = tt * 4 + sub
                        rows = 128 if tt < 4 else 64
                        for half in range(2):
                            p, pk = pp.get()
                            for j in range(nch):
                                C.mm(p[:rows, :], actT[ai][:, j, sub * 128:sub * 128 + rows],
                                     w2g[wi][:, j, half * 512:(half + 1) * 512], j == 0, j == nch - 1, [ka, kw + "c"], [pk])
                            sl = slice(half * 512, (half + 1) * 512)
                            if gates is None:
                                C.tt(acc[:rows, t, sl], acc[:rows, t, sl], p[:rows, :], ALU.add, ["acc", pk], ["acc"])
                            else:
                                C.stt(acc[:rows, t, sl], p[:rows, :], gates[:rows, t, e:e + 1], acc[:rows, t, sl],
                                      ALU.mult, ALU.add, [pk, "gates", "acc"], ["acc"])
        C.flush()


def phase_C4(C, pp, acc, xs_mid_d, xs_out_d, mods, gpost2, n_tiles, es0):
    with contextlib.ExitStack() as es:
        G2P = C.sb("G2P", [128, 2, 1024], F32, es)
        for s in range(2):
            C.tt(G2P[:, s, :], mods[5][:, s, :], gpost2[:], ALU.mult, ["mod5", "gpost2"], ["G2P"])
        xt = [C.sb("xt4", [128, 1024], F32, es) for _ in range(2)]
        junk = C.sb("junk4", [128, 1024], F32, es)
        ss = [C.sb("ss4", [128, 2], F32, es) for _ in range(2)]
        for t in range(n_tiles):
            rows = 128 if t < 16 else 64
            s = 0 if t < 16 else 1
            i = t % 2
            ki = "%d" % i
            C.dma(xt[i][:rows, :], xs_mid_d[t * 128:t * 128 + rows, :], ["xs_mid"], ["xt4" + ki], eng="sync")
            C.act(junk[:rows, :], acc[:rows, t, :], AF.Square, ["acc"], ["junk4", "ss4" + ki], accum_out=ss[i][:rows, 0:1])
            small_rstd(C, ss[i][:, 0:1], ss[i][:, 1:2], rows, 1.0 / D, "ss4" + ki, "ss4" + ki)
            C.stt(junk[:rows, :], acc[:rows, t, :], ss[i][:rows, 1:2], G2P[:rows, s, :], ALU.mult, ALU.mult,
                  ["acc", "ss4" + ki, "G2P"], ["junk4"])
            C.tt(xt[i][:rows, :], xt[i][:rows, :], junk[:rows, :], ALU.add, ["xt4" + ki, "junk4"], ["xt4" + ki], eng="gpsimd")
            C.dma(xs_out_d[t * 128:t * 128 + rows, :], xt[i][:rows, :], ["xt4" + ki], ["xs_out"], eng="sync")
        C.flush()


def _finish(C):
    C.P.finish()
    C.P.build()
    C.es.close()
    C.P.es.close()


def load_consts(C, identb_d, identf_d=None):
    identb = C.sb("identb", [128, 128], BF16)
    C.dma(identb[:], identb_d, [], ["identb"], eng="sync")
    identf = None
    if identf_d is not None:
        identf = C.sb("identf", [128, 128], F32)
        C.dma(identf[:], identf_d, [], ["identf"], eng="sync")
    ones_f = C.sb("ones_f", [128, 128], F32)
    C.memset(ones_f[:], 1.0, ["ones_f"])
    return identb, identf, ones_f


def run_A(C, pp, xs_d, cvT_d, ada_w_d, ada_b_d, gpre_d, hT_d, identb, ones_f):
    with contextlib.ExitStack() as es:
        xs = C.sb("xs", [128, 17, D], F32, es)
        hT = C.sb("hT", [128, 8, OWN], BF16, es)
        gpre = C.sb("gpre", [128, D], F32, es)
        C.dma(xs[:, 0:16, :], xs_d[0:2048, :].rearrange("(t p) d -> p t d", p=128), ["xs_out"], ["xs"], eng="sync")
        C.dma(xs[0:64, 16, :], xs_d[2048:2112, :], ["xs_out"], ["xs"], eng="sync")
        C.dma(gpre[:], gpre_d, [], ["gpre"], eng="sync")
        mods = compute_mods(C, pp, 0, [0, 1], cvT_d, ada_w_d, ada_b_d, ones_f, es)
        phase_A(C, pp, xs, hT, mods, gpre, identb, es)
        C.dma(hT_d.rearrange("(k p) t -> p k t", p=128), hT[:], ["hT"], ["hT_d"], eng="sync")
        C.flush()


def build_A():
    nc = bass.Bass("TRN2", target_bir_lowering=False)
    C = Ctx(nc)
    xs_d = C.dram("xs", [OWN, D], F32, "ExternalInput")
    cvT_d = C.dram("cvT", [128, 2, 8], F32, "ExternalInput")
    ada_w_d = C.dram("ada_w", [1, D, 6 * D], F32, "ExternalInput")
    ada_b_d = C.dram("ada_b", [1, 128, 6 * D], F32, "ExternalInput")
    gpre_d = C.dram("gpre", [128, D], F32, "ExternalInput")
    identb_d = C.dram("identb", [128, 128], BF16, "ExternalInput")
    hT_d = C.dram("hT", [D, OWN], BF16, "ExternalOutput")
    pp = PsumPool(C)
    identb, _, ones_f = load_consts(C, identb_d)
    run_A(C, pp, xs_d, cvT_d, ada_w_d, ada_b_d, gpre_d, hT_d, identb, ones_f)
    _finish(C)
    return nc


def build_C(moe, with_next_A, n_exp_dbg=None):
    nc = bass.Bass("TRN2", target_bir_lowering=False)
    C = Ctx(nc)
    xs_d = C.dram("xs", [OWN, D], F32, "ExternalInput")
    hT_d = C.dram("hT", [D, OWN], BF16, "ExternalInput")
    dnT_d = C.dram("dnT", [8, 128, OWN], BF16, "ExternalInput")
    naT_d = C.dram("naT", [4, 128, OWN], BF16, "ExternalInput")
    cvT_d = C.dram("cvT", [128, 2, 8], F32, "ExternalInput")
    ada_w_d = C.dram("ada_w", [1, D, 6 * D], F32, "ExternalInput")
    ada_b_d = C.dram("ada_b", [1, 128, 6 * D], F32, "ExternalInput")
    gpost_d = C.dram("gpost", [128, D], F32, "ExternalInput")
    gpre2_d = C.dram("gpre2", [128, D], F32, "ExternalInput")
    gpost2_d = C.dram("gpost2", [128, D], F32, "ExternalInput")
    wg_d = C.dram("wg", [D, 2048], F32, "ExternalInput")
    wpa_d = C.dram("wpa", [D, D], F32, "ExternalInput")
    wpb_d = C.dram("wpb", [512, D], F32, "ExternalInput")
    wout_d = C.dram("wout", [D, D], F32, "ExternalInput")
    identb_d = C.dram("identb", [128, 128], BF16, "ExternalInput")
    identf_d = C.dram("identf", [128, 128], F32, "ExternalInput")
    if moe:
        n_exp, dff = (n_exp_dbg or NE), DFE
        router_d = C.dram("router", [D, NE], F32, "ExternalInput")
    else:
        n_exp, dff = 1, DFF
        router_d = None
    w1_d = C.dram("w1", [n_exp, D, dff], F32, "ExternalInput")
    w3_d = C.dram("w3", [n_exp, D, dff], F32, "ExternalInput")
    w2_d = C.dram("w2", [n_exp, dff, D], F32, "ExternalInput")
    xs_mid_d = C.dram("xs_mid", [OWN, D], F32, "Internal")
    xs_out_d = C.dram("xs_out", [OWN, D], F32, "ExternalOutput")
    if with_next_A:
        ada_w2_d = C.dram("ada_w_n", [1, D, 6 * D], F32, "ExternalInput")
        ada_b2_d = C.dram("ada_b_n", [1, 128, 6 * D], F32, "ExternalInput")
        gpre_n_d = C.dram("gpre_n", [128, D], F32, "ExternalInput")
        hTn_d = C.dram("hT_n", [D, OWN], BF16, "ExternalOutput")
    pp = PsumPool(C)
    identb, identf, ones_f = load_consts(C, identb_d, identf_d)
    with contextlib.ExitStack() as es1:
        h2T_all = C.sb("h2T", [128, 8, OWN], BF16, es1)
        gates = C.sb("gates", [128, 17, 8], F32, es1) if moe else None
        gpost2 = C.sb("gpost2", [128, D], F32, es1)
        C.dma(gpost2[:], gpost2_d, [], ["gpost2"], eng="sync")
        with contextlib.ExitStack() as es2:
            mods = compute_mods(C, pp, 0, [2, 3, 4], cvT_d, ada_w_d, ada_b_d, ones_f, es2)
            gpost = C.sb("gpost", [128, D], F32, es2)
            gpre2 = C.sb("gpre2", [128, D], F32, es2)
            C.dma(gpost[:], gpost_d, [], ["gpost"], eng="sync")
            C.dma(gpre2[:], gpre2_d, [], ["gpre2"], eng="sync")
            yT_all = C.sb("yT", [128, 8, OWN], BF16, es2)
            phase_C1(C, pp, hT_d, dnT_d, naT_d, wg_d, wpa_d, wpb_d, yT_all, es2)
            phase_C2(C, pp, yT_all, wout_d, xs_d, xs_mid_d, mods, gpost, gpre2, h2T_all, identb, identf,
                     router_d, gates, es2)
            C.flush()
        with contextlib.ExitStack() as es3:
            acc = C.sb("acc", [128, 17, D], F32, es3)
            if os.environ.get("DBG", "") not in ("2", "3"):
                phase_C3(C, pp, h2T_all, w1_d, w3_d, w2_d, n_exp, dff, gates, acc, es3)
            else:
                C.memset(acc[:], 0.0, ["acc"])
            mods5 = compute_mods(C, pp, 0, [5], cvT_d, ada_w_d, ada_b_d, ones_f, es3)
            phase_C4(C, pp, acc, xs_mid_d, xs_out_d, mods5, gpost2, 17, es3)
            C.flush()
        C.flush()
    if with_next_A:
        run_A(C, pp, xs_out_d, cvT_d, ada_w2_d, ada_b2_d, gpre_n_d, hTn_d, identb, ones_f)
    _finish(C)
    return nc


TT_B = [(i * 512, 512) for i in range(16)] + [(8192, 256)]


def na_pattern(p):
    r = 2 * p
    ws = min(max(r - 4, 0), 118)
    pat = {0: 0, 2: 1, 124: 3, 126: 4}.get(r, 2)
    return ws, pat


def phase_NA(C, pp, b, hT_full_d, wnaq_d, wnak_d, wnav_d, nabias_d, namask_d, naT_d, identb):
    with contextlib.ExitStack() as es:
        wq = C.sb("wnq", [128, 8, 64], BF16, es)
        wk = C.sb("wnk", [128, 8, 64], BF16, es)
        wv = C.sb("wnv", [128, 8, 64], BF16, es)
        for w, d, kk in ((wq, wnaq_d, "wnq"), (wk, wnak_d, "wnk"), (wv, wnav_d, "wnv")):
            C.dma(w[:], d.rearrange("(k p) n -> p k n", p=128), [], [kk], eng="gpsimd")
        bias = C.sb("nabias", [128, 5, 640], F32, es)
        mask = C.sb("namask", [128, 5, 640], F32, es)
        C.dma(bias[:], nabias_d.rearrange("a p n -> p a n"), [], ["nabias"], eng="sync")
        C.dma(mask[:], namask_d.rearrange("a p n -> p a n"), [], ["namask"], eng="sync")
        C.tt(bias[:], bias[:], mask[:], ALU.add, ["nabias", "namask"], ["nabias"])
        qT = C.sb("naqT", [64, TB], BF16, es)
        kT = C.sb("nakT", [64, TB], BF16, es)
        vt = C.sb("nav", [128, NT_B, 64], BF16, es)
        oT = C.sb("naoT", [64, TB], BF16, es)
        hTt = [C.sb("hTtn", [128, 8, 512], BF16, es) for _ in range(2)]
        hv = hT_full_d[b].rearrange("(k p) t -> p k t", p=128)
        for ti, (t0, n) in enumerate(TT_B):
            i = ti % 2
            kh = "hTtn%d" % i
            C.dma(hTt[i][:, :, :n], hv[:, :, t0:t0 + n], [], [kh], eng="sync")
            p, pk = pp.get()
            for k in range(8):
                C.mm(p[0:64, :n], wq[:, k, :], hTt[i][:, k, :n], k == 0, k == 7, ["wnq", kh], [pk])
            C.act(qT[:, t0:t0 + n], p[0:64, :n], AF.Copy, [pk], ["naqT"], scale=0.125)
            p, pk = pp.get()
            for k in range(8):
                C.mm(p[0:64, :n], wk[:, k, :], hTt[i][:, k, :n], k == 0, k == 7, ["wnk", kh], [pk])
            C.cp(kT[:, t0:t0 + n], p[0:64, :n], [pk], ["nakT"], eng="vector")
            p, pk = pp.get()
            for sub in range(n // 128):
                for k in range(8):
                    C.mm(p[:, sub * 64:(sub + 1) * 64], hTt[i][:, k, sub * 128:(sub + 1) * 128], wv[:, k, :], k == 0, k == 7,
                         ["wnv", kh], [pk])
            C.cp(vt[:, t0 // 128:t0 // 128 + n // 128, :], p[:, 0:(n // 128) * 64].rearrange("p (s d) -> p s d", d=64),
                 [pk], ["nav"], eng="scalar")
        NS = 4
        Sb = [C.sb("naS", [128, 896], F32, es) for _ in range(NS)]
        Pb = [C.sb("naP", [128, 896], BF16, es) for _ in range(NS)]
        PT = [C.sb("naPT", [128, 7, 128], BF16, es) for _ in range(NS)]
        st = [C.sb("nast", [128, 4], F32, es) for _ in range(NS)]
        ob = [C.sb("naob", [128, 64], BF16, es) for _ in range(NS)]

        def na_tile(qi, i):
            ki = "%d" % i
            bX, kX = pp.t[2 * i], "psum%d" % (2 * i)
            bY, kY = pp.t[2 * i + 1], "psum%d" % (2 * i + 1)
            bXb = bX[:].bitcast(BF16)
            bYb = bY[:].bitcast(BF16)
            if qi < 2:
                q0 = qi * 128
                nk, nblk, vtiles = 256, 2, [0, 1]
                C.mm(bY[:, 0:256], qT[:, q0:q0 + 128], kT[:, 0:256], True, True, ["naqT", "nakT"], [kY])
                yield
                C.cp(Sb[i][:, 0:256], bY[:, 0:256], [kY], ["naS" + ki], eng="scalar")
                yield
            else:
                p_ = qi - 2
                ws, pat = na_pattern(p_)
                q0 = CTX + 128 * p_
                k0 = CTX + ws * 64
                nk, nblk = 896, 7
                vtiles = [2 + ws // 2 + j for j in range(5)] + [0, 1]
                C.mm(bX[:, 0:512], qT[:, q0:q0 + 128], kT[:, k0:k0 + 512], True, True, ["naqT", "nakT"], [kX])
                C.mm(bY[:, 0:128], qT[:, q0:q0 + 128], kT[:, k0 + 512:k0 + 640], True, True, ["naqT", "nakT"], [kY])
                C.mm(bY[:, 128:384], qT[:, q0:q0 + 128], kT[:, 0:256], True, True, ["naqT", "nakT"], [kY])
                yield
                C.tt(Sb[i][:, 0:512], bX[:, 0:512], bias[:, pat, 0:512], ALU.add, [kX, "nabias"], ["naS" + ki])
                C.cp(Sb[i][:, 640:896], bY[:, 128:384], [kY], ["naS" + ki], eng="scalar")
                yield
                C.tt(Sb[i][:, 512:640], bY[:, 0:128], bias[:, pat, 512:640], ALU.add, [kY, "nabias"], ["naS" + ki])
                yield
            C.red(st[i][:, 0:1], Sb[i][:, 0:nk], ALU.max, ["naS" + ki], ["nast" + ki])
            yield
            C.ts(st[i][:, 1:2], st[i][:, 0:1], -1.0, ALU.mult, ["nast" + ki], ["nast" + ki])
            yield
            C.act(Pb[i][:, 0:nk], Sb[i][:, 0:nk], AF.Exp, ["naS" + ki, "nast" + ki], ["naP" + ki, "nast" + ki],
                  bias=st[i][:, 1:2], accum_out=st[i][:, 2:3])
            yield
            C.recip(st[i][:, 3:4], st[i][:, 2:3], ["nast" + ki], ["nast" + ki])
            for j in range(nblk):
                C.tr(bXb[:, j * 128:(j + 1) * 128], Pb[i][:, j * 128:(j + 1) * 128], identb[:], ["naP" + ki, "identb"], [kX])
            yield
            C.cp(PT[i][:, 0:nblk, :], bXb[:, 0:nblk * 128].rearrange("p (j t) -> p j t", t=128), [kX], ["naPT" + ki],
                 eng="scalar")
            yield
            for j in range(nblk):
                C.mm(bY[:, 384:448], PT[i][:, j, :], vt[:, vtiles[j], :], j == 0, j == nblk - 1, ["naPT" + ki, "nav"], [kY])
            yield
            C.ts(ob[i][:], bY[:, 384:448], st[i][:, 3:4], ALU.mult, [kY, "nast" + ki], ["naob" + ki])
            yield
            C.tr(bYb[0:64, 896:1024], ob[i][:], identb[:], ["naob" + ki, "identb"], [kY])
            yield
            C.cp(oT[:, q0:q0 + 128], bYb[0:64, 896:1024], [kY], ["naoT"], eng="scalar")
            yield

        def na_interleave(gens):
            gens = list(gens)
            while gens:
                for g_ in list(gens):
                    try:
                        next(g_)
                    except StopIteration:
                        gens.remove(g_)

        tiles_all = list(range(2 + 64))
        for g0 in range(0, len(tiles_all), NS):
            na_interleave([na_tile(qi, qi % NS) for qi in tiles_all[g0:g0 + NS]])
        C.dma(naT_d[b], oT[:], ["naoT"], ["naT_d"], eng="sync")
        C.flush()


def phase_DN(C, pp, b, hT_full_d, wq_d, wk_d, wv_d, wzab_d, conv_d, dnpar_d, normw_d, cos_d, sin_d,
             cm_d, cmb_d, dnT_d, identb, identf, ones_f):
    with contextlib.ExitStack() as es:
        cm = C.sb("cm", [128, 5, 128], F32, es)
        cmb = C.sb("cmb", [128, 6, 128], BF16, es)
        C.dma(cm[:], cm_d.rearrange("a p n -> p a n"), [], ["cm"], eng="sync")
        C.dma(cmb[:], cmb_d.rearrange("a p n -> p a n"), [], ["cmb"], eng="sync")
        PM = [cmb[:, 0, :], cmb[:, 2, :]]
        NMt = [cmb[:, 1, :], cmb[:, 3, :]]
        rotT = cmb[:, 4, :]
        ones_b = cmb[:, 5, :]
        wqkv = C.sb("wqkv", [128, 3, 8, 128], BF16, es)
        for x, d in enumerate((wq_d, wk_d, wv_d)):
            C.dma(wqkv[:, x, :, :], d.rearrange("(k p) n -> p k n", p=128), [], ["wqkv"], eng="gpsimd")
        wzab = C.sb("wzab", [128, 8, 132], BF16, es)
        if isinstance(wzab_d, tuple):
            C.dma(wzab[:, :, 0:128], wzab_d[0].rearrange("(k p) n -> p k n", p=128), [], ["wzab"], eng="gpsimd")
            abblk = C.sb("abblk", [128, 8, 32], BF16, es)
            C.dma(abblk[:], wzab_d[1].rearrange("(k p) n -> p k n", p=128), [], ["abblk"], eng="gpsimd")
            C.cp(wzab[:, :, 128:132], abblk[:, :, wzab_d[2]:32:8], ["abblk"], ["wzab"], eng="vector")
        else:
            C.dma(wzab[:], wzab_d.rearrange("(k p) n -> p k n", p=128), [], ["wzab"], eng="gpsimd")
        convw = C.sb("convw", [128, 3, 5], F32, es)
        C.dma(convw[:], conv_d.rearrange("x p k -> p x k"), [], ["convw"], eng="sync")
        par = C.sb("dnpar", [128, 4], F32, es)
        C.dma(par[:], dnpar_d, [], ["dnpar"], eng="sync")
        normw = C.sb("normw", [128, 128], F32, es)
        C.dma(normw[:], normw_d, [], ["normw"], eng="sync")
        qT = C.sb("dqT", [128, TB], BF16, es)
        kT = C.sb("dkT", [128, TB], BF16, es)
        ktok = C.sb("dktok", [128, NT_B, 128], BF16, es)
        vtok = C.sb("dvtok", [128, NT_B, 128], BF16, es)
        sz = C.sb("dsz", [128, NT_B, 128], BF16, es)
        abt = C.sb("dab", [128, NT_B, 4], F32, es)
        RW = 2 + CTX + 2 + 2 + SEQ + 2
        OFFC, OFFL = 2, 2 + CTX + 2 + 2
        with contextlib.ExitStack() as es1:
            raw = C.sb("draw", [128, 3, RW], BF16, es1)
            for x in range(3):
                C.memset(raw[:, x, 0:2], 0.0, ["draw"], eng="gpsimd")
                C.memset(raw[:, x, OFFC + CTX:OFFL], 0.0, ["draw"], eng="gpsimd")
                C.memset(raw[:, x, OFFL + SEQ:RW], 0.0, ["draw"], eng="gpsimd")
            hTt = [C.sb("hTtd", [128, 8, 512], BF16, es1) for _ in range(2)]
            ztmp = [C.sb("ztmp", [128, 128], F32, es1) for _ in range(2)]
            hv = hT_full_d[b].rearrange("(k p) t -> p k t", p=128)
            tiles = [(0, 256)] + [(CTX + i * 512, 512) for i in range(16)]
            for ti, (t0, n) in enumerate(tiles):
                i = ti % 2
                kh = "hTtd%d" % i
                C.dma(hTt[i][:, :, :n], hv[:, :, t0:t0 + n], [], [kh], eng="sync")
                ro = OFFC + t0 if t0 < CTX else OFFL + (t0 - CTX)
                for x in range(3):
                    p, pk = pp.get()
                    for k in range(8):
                        C.mm(p[:, :n], wqkv[:, x, k, :], hTt[i][:, k, :n], k == 0, k == 7, ["wqkv", kh], [pk])
                    C.cp(raw[:, x, ro:ro + n], p[:, :n], [pk], ["draw"], eng=("scalar" if x != 1 else "vector"))
                for sub in range(n // 128):
                    tl = t0 // 128 + sub
                    p, pk = pp.get()
                    for k in range(8):
                        C.mm(p[:, 0:132], hTt[i][:, k, sub * 128:(sub + 1) * 128], wzab[:, k, :], k == 0, k == 7,
                             ["wzab", kh], [pk])
                    zi = tl % 2
                    C.act(ztmp[zi][:], p[:, 0:128], AF.Silu, [pk], ["ztmp%d" % zi])
                    C.tt(sz[:, tl, :], ztmp[zi][:], normw[:], ALU.mult, ["ztmp%d" % zi, "normw"], ["dsz"], eng="gpsimd")
                    C.cp(abt[:, tl, :], p[:, 128:132], [pk], ["dab"], eng="vector")
            acc = [C.sb("cacc", [128, 512], F32, es1) for _ in range(2)]
            sq = [C.sb("csq", [128, 512], BF16, es1) for _ in range(2)]
            rn = [C.sb("crn", [128, 512], F32, es1) for _ in range(2)]
            t1 = [C.sb("ct1", [128, 512], F32, es1) for _ in range(2)]
            t2 = [C.sb("ct2", [128, 512], F32, es1) for _ in range(2)]
            cs = [C.sb("ccs", [128, 2, 512], F32, es1) for _ in range(2)]
            yb3 = [[C.sb("cyb3", [128, 512], BF16, es1) for _ in range(3)] for _ in range(2)]
            for ti, (t0, n) in enumerate(tiles):
                lat = t0 >= CTX
                ro = OFFC + t0 if not lat else OFFL + (t0 - CTX)
                ci = ti % 2
                if lat:
                    C.dma(cs[ci][:, 0, :n], cos_d[:, t0 - CTX:t0 - CTX + n], [], ["ccs%d" % ci], eng="sync")
                    C.dma(cs[ci][:, 1, :n], sin_d[:, t0 - CTX:t0 - CTX + n], [], ["ccs%d" % ci], eng="sync")
                for x in range(3):
                    i = x % 2
                    ki = "%d" % i
                    ybx = yb3[ci][x]
                    ky = "cyb3_%d_%d" % (ci, x)
                    C.ts(acc[i][:, :n], raw[:, x, ro - 2:ro - 2 + n], convw[:, x, 0:1], ALU.mult, ["draw", "convw"], ["cacc" + ki])
                    for tap in range(1, 5):
                        C.stt(acc[i][:, :n], raw[:, x, ro - 2 + tap:ro - 2 + tap + n], convw[:, x, tap:tap + 1], acc[i][:, :n],
                              ALU.mult, ALU.add, ["draw", "convw", "cacc" + ki], ["cacc" + ki])
                    C.act(ybx[:, :n], acc[i][:, :n], AF.Silu, ["cacc" + ki], [ky])
                for x in (2, 0, 1):
                    i = x % 2
                    ki = "%d" % i
                    ybx = yb3[ci][x]
                    ky = "cyb3_%d_%d" % (ci, x)
                    if x == 2:
                        for sub in range(n // 128):
                            tl = t0 // 128 + sub
                            p, pk = pp.get()
                            pb = p[:].bitcast(BF16)
                            C.tr(pb[:, 0:128], ybx[:, sub * 128:(sub + 1) * 128], identb[:], [ky, "identb"], [pk])
                            C.cp(vtok[:, tl, :], pb[:, 0:128], [pk], ["dvtok"], eng="scalar")
                        continue
                    dst = qT if x == 0 else kT
                    dk = "dqT" if x == 0 else "dkT"
                    scale = 128.0 ** -0.5 if x == 0 else 1.0
                    C.tt(sq[i][:, :n], ybx[:, :n], ybx[:, :n], ALU.mult, [ky], ["csq" + ki], eng="gpsimd")
                    p, pk = pp.get()
                    C.mm(p[:, :n], ones_b, sq[i][:, :n], True, True, ["cmb", "csq" + ki], [pk])
                    C.act(rn[i][:, :n], p[:, :n], AF.Ln, [pk], ["crn" + ki], bias=EPS)
                    C.act(rn[i][:, :n], rn[i][:, :n], AF.Exp, ["crn" + ki], ["crn" + ki], scale=-0.5)
                    if lat:
                        p2, pk2 = pp.get()
                        C.mm(p2[:, :n], rotT, ybx[:, :n], True, True, ["cmb", ky], [pk2])
                        C.tt(t1[i][:, :n], ybx[:, :n], cs[ci][:, 0, :n], ALU.mult, [ky, "ccs%d" % ci], ["ct1" + ki],
                             eng="gpsimd")
                        C.tt(t2[i][:, :n], p2[:, :n], cs[ci][:, 1, :n], ALU.mult, [pk2, "ccs%d" % ci], ["ct2" + ki])
                        C.tt(t1[i][:, :n], t1[i][:, :n], t2[i][:, :n], ALU.add, ["ct1" + ki, "ct2" + ki], ["ct1" + ki], eng="gpsimd")
                        C.stt(dst[:, t0:t0 + n], t1[i][:, :n], scale, rn[i][:, :n], ALU.mult, ALU.mult,
                              ["ct1" + ki, "crn" + ki], [dk])
                    else:
                        C.stt(dst[:, t0:t0 + n], ybx[:, :n], scale, rn[i][:, :n], ALU.mult, ALU.mult,
                              [ky, "crn" + ki], [dk])
                    if x == 1:
                        for sub in range(n // 128):
                            tl = t0 // 128 + sub
                            p, pk = pp.get()
                            pb = p[:].bitcast(BF16)
                            C.tr(pb[:, 0:128], kT[:, t0 + sub * 128:t0 + (sub + 1) * 128], identb[:], ["dkT", "identb"], [pk])
                            C.cp(ktok[:, tl, :], pb[:, 0:128], [pk], ["dktok"], eng="scalar")
            C.flush()
        NT = NT_B
        G = C.sb("dG", [128, 2, NT], F32, es)
        BETA = C.sb("dBETA", [128, 2, NT], F32, es)
        GC = C.sb("dGC", [128, 2, NT], F32, es)
        NGC = C.sb("dNGC", [128, 2, NT], F32, es)
        NBT = C.sb("dNB", [128, 2, NT], F32, es)
        BEG = C.sb("dBEG", [128, 2, NT], F32, es)
        EDK = C.sb("dEDK", [128, 2, NT], F32, es)
        EGL = C.sb("dEGL", [128, 2, 2, NT], F32, es)
        nea = C.sb("dnea", [128, 2], F32, es)
        C.act(nea[:], par[:, 0:2], AF.Exp, ["dnpar"], ["dnea"])
        C.ts(nea[:], nea[:], -1.0, ALU.mult, ["dnea"], ["dnea"])
        for d in range(2):
            C.act(G[:, d, :], abt[:, :, d], AF.Exp, ["dab", "dnpar"], ["dG"], bias=par[:, 2 + d:3 + d])
            C.act(G[:, d, :], G[:, d, :], AF.Ln, ["dG"], ["dG"], bias=1.0)
            C.ts(G[:, d, :], G[:, d, :], nea[:, d:d + 1], ALU.mult, ["dG", "dnea"], ["dG"])
            C.act(BETA[:, d, :], abt[:, :, 2 + d], AF.Sigmoid, ["dab"], ["dBETA"])
            p, pk = pp.get()
            C.mm(p[:, 0:NT], cm[:, d, :], G[:, d, :], True, True, ["cm", "dG"], [pk])
            C.cp(GC[:, d, :], p[:, 0:NT], [pk], ["dGC"], eng="vector")
            p, pk = pp.get()
            C.mm(p[:, 0:NT], cm[:, 2, :], G[:, d, :], True, True, ["cm", "dG"], [pk])
            C.tt(EDK[:, d, :], p[:, 0:NT], GC[:, d, :], ALU.subtract, [pk, "dGC"], ["dEDK"])
            C.act(EDK[:, d, :], EDK[:, d, :], AF.Exp, ["dEDK"], ["dEDK"])
            for hf in range(2):
                p, pk = pp.get()
                C.mm(p[:, 0:NT], cm[:, 3 + hf, :], G[:, d, :], True, True, ["cm", "dG"], [pk])
                C.act(EGL[:, d, hf, :], p[:, 0:NT], AF.Exp, [pk], ["dEGL"])
        C.ts(NGC[:], GC[:], -1.0, ALU.mult, ["dGC"], ["dNGC"])
        C.ts(NBT[:], BETA[:], -1.0, ALU.mult, ["dBETA"], ["dNB"])
        C.act(BEG[:], GC[:], AF.Exp, ["dGC"], ["dBEG"])
        C.tt(BEG[:], BEG[:], BETA[:], ALU.mult, ["dBEG", "dBETA"], ["dBEG"])
        obuf = C.sb("dobuf", [128, NT, 128], F32, es)
        C.memset(obuf[:], 0.0, ["ob%d" % t for t in range(NT)], eng="gpsimd")
        S32 = [C.sb("dS32", [128, 128], F32, es) for _ in range(2)]
        S16 = [C.sb("dS16", [128, 128], BF16, es) for _ in range(2)]
        for d in range(2):
            C.memset(S32[d][:], 0.0, ["S32_%d" % d])
            C.memset(S16[d][:], 0.0, ["S16_%d" % d])
        NPAR = 3
        NSLOT = 2 * NPAR
        ring = []
        for d in range(2):
            ring.append([dict(u=C.sb("pu", [128, 128], F32, es), wT=C.sb("pw", [128, 128], BF16, es),
                              at=C.sb("pat", [128, 128], BF16, es), qg=C.sb("pqg", [128, 128], BF16, es),
                              kd=C.sb("pkd", [128, 128], BF16, es), vn=C.sb("pvn", [128, 128], BF16, es))
                         for _ in range(NSLOT)])
        tmpf = {n_: [C.sb("pt" + n_, [128, 128], F32, es) for _ in range(2 * NPAR)] for n_ in ("dg", "E", "ET", "EG")}
        tmpb = {n_: [C.sb("pb" + n_, [128, 128], BF16, es) for _ in range(4 * NPAR)] for n_ in ("N", "X", "PT")}
        tmpc = {n_: [C.sb("pc" + n_, [128, 128], BF16, es) for _ in range(2 * NPAR)] for n_ in ("kbg", "vb", "dgh", "dgl")}
        pmf = C.sb("pmf", [128, 4, 128], F32, es)
        C.cp(pmf[:], cmb[:, 0:4, :], ["cmb"], ["pmf"], eng="vector")
        PMf = [pmf[:, 0, :], pmf[:, 2, :]]
        NMf = [pmf[:, 1, :], pmf[:, 3, :]]
        GCHb = C.sb("dGCHb", [128, 2, NT], BF16, es)
        GCH = C.sb("dGCH", [128, 2, NT], F32, es)
        GCL = C.sb("dGCL", [128, 2, NT], F32, es)
        C.cp(GCHb[:], GC[:], ["dGC"], ["dGCHb"], eng="vector")
        C.cp(GCH[:], GCHb[:], ["dGCHb"], ["dGCH"], eng="vector")
        C.tt(GCL[:], GC[:], GCH[:], ALU.subtract, ["dGC", "dGCH"], ["dGCL"])

        def bankq(bi, qi):
            b_ = pp.t[bi]
            return b_[:, qi * 128:(qi + 1) * 128], b_[:].bitcast(BF16)[:, qi * 256:qi * 256 + 128], "psum%d" % bi

        def prep(t, d, slot, sk, par):
            R = ring[d][slot]
            c0 = t * 128
            q_ = NPAR * d + par
            kd = "%d" % q_
            Q0, _, kB = bankq(q_, 0)
            Q1, _, _ = bankq(q_, 1)
            Q2, _, _ = bankq(q_, 2)
            Q3, Q3b, _ = bankq(q_, 3)
            C.mm(Q0, kT[:, c0:c0 + 128], kT[:, c0:c0 + 128], True, True, ["dkT"], [kB])
            C.mm(Q1, kT[:, c0:c0 + 128], qT[:, c0:c0 + 128], True, True, ["dkT", "dqT"], [kB])
            dgh, dgl = tmpc["dgh"][q_], tmpc["dgl"][q_]
            C.act(dgh[:], identf[:], AF.Copy, ["identf", "dGCH"], ["dgh" + kd], scale=GCH[:, d, t:t + 1])
            C.act(dgl[:], identf[:], AF.Copy, ["identf", "dGCL"], ["dgl" + kd], scale=GCL[:, d, t:t + 1])
            kbg, vb = tmpc["kbg"][q_], tmpc["vb"][q_]
            C.act(kbg[:], ktok[:, t, :], AF.Copy, ["dktok", "dBEG"], ["kbg" + kd], scale=BEG[:, d, t:t + 1])
            C.ts(vb[:], vtok[:, t, :], BETA[:, d, t:t + 1], ALU.mult, ["dvtok", "dBETA"], ["vb" + kd], eng="vector")
            C.act(R["kd"][:], ktok[:, t, :], AF.Copy, ["dktok", "dEDK"], [sk + "kd"], scale=EDK[:, d, t:t + 1])
            yield
            C.mm(Q2, ones_b, dgh[:], True, False, ["cmb", "dgh" + kd], [kB])
            C.mm(Q2, ones_b, dgl[:], False, True, ["cmb", "dgl" + kd], [kB])
            yield
            E, ET, EG = tmpf["E"][q_], tmpf["ET"][q_], tmpf["EG"][q_]
            bs = tmpf["dg"][q_]
            C.cp(bs[:], Q2, [kB], ["bs" + kd], eng="scalar")
            yield
            C.tt(E[:], bs[:], PMf[d], ALU.add, ["bs" + kd, "pmf"], ["E" + kd], eng="gpsimd")
            C.tt(ET[:], bs[:], NMf[d], ALU.add, ["bs" + kd, "pmf"], ["ET" + kd], eng="gpsimd")
            yield
            C.act(E[:], E[:], AF.Exp, ["E" + kd, "dGC"], ["E" + kd], bias=GC[:, d, t:t + 1], scale=-1.0)
            C.act(ET[:], ET[:], AF.Exp, ["ET" + kd, "dNGC"], ["ET" + kd], bias=NGC[:, d, t:t + 1], scale=1.0)
            C.act(EG[:], bs[:], AF.Exp, ["bs" + kd], ["EG" + kd])
            yield
            N = tmpb["N"]
            X = tmpb["X"]
            PTb = tmpb["PT"]
            o = 2 * q_
            C.stt(N[o][:], Q0, NBT[:, d, t:t + 1], E[:], ALU.mult, ALU.mult, [kB, "dNB", "E" + kd], ["N%d" % o])
            C.tt(R["at"][:], Q1, ET[:], ALU.mult, [kB, "ET" + kd], [sk + "at"])
            yield
            C.tr(Q3b, N[o][:], identb[:], ["N%d" % o, "identb"], [kB])
            yield
            C.cp(X[o][:], Q3b, [kB], ["X%d" % o], eng="scalar")
            C.tt(PTb[o][:], Q3b, identb[:], ALU.add, [kB, "identb"], ["PT%d" % o])
            yield
            C.tt(R["qg"][:], qT[:, c0:c0 + 128], EG[:], ALU.mult, ["dqT", "EG" + kd], [sk + "qg"], eng="gpsimd")
            cur = 0
            for lev in range(1, 6):
                a, bb = o + cur, o + 1 - cur
                C.mm(Q0, X[a][:], N[a][:], True, True, ["X%d" % a, "N%d" % a], [kB])
                if lev < 5:
                    C.mm(Q1, N[a][:], X[a][:], True, True, ["X%d" % a, "N%d" % a], [kB])
                yield
                C.cp(N[bb][:], Q0, [kB], ["N%d" % bb], eng="scalar")
                if lev < 5:
                    C.cp(X[bb][:], Q1, [kB], ["X%d" % bb], eng="vector")
                yield
                C.mm(Q2, N[bb][:], PTb[a][:], True, True, ["N%d" % bb, "PT%d" % a], [kB])
                yield
                C.tt(PTb[bb][:], PTb[a][:], Q2, ALU.add, ["PT%d" % a, kB], ["PT%d" % bb])
                yield
                cur = 1 - cur
            fin = o + cur
            C.mm(Q0, PTb[fin][:], vb[:], True, True, ["PT%d" % fin, "vb" + kd], [kB])
            C.mm(Q1, kbg[:], PTb[fin][:], True, True, ["PT%d" % fin, "kbg" + kd], [kB])
            yield
            C.cp(R["u"][:], Q0, [kB], [sk + "u"], eng="scalar")
            C.cp(R["wT"][:], Q1, [kB], [sk + "wT"], eng="vector")
            yield

        def step(t, hf, d, slot, sk):
            R = ring[d][slot]
            sl = slice(64 * hf, 64 * hf + 64)
            kd = "%d" % d
            bS = pp.t[2 * NPAR + d]
            kS = "psum%d" % (2 * NPAR + d)
            pS, pO, pD = bS[:, 0:128], bS[:, 128:256], bS[:, 256:384]
            C.mm(pS[sl, :], R["wT"][:, sl], S16[d][:], True, True, [sk + "wT", "S16_" + kd], [kS])
            yield
            C.tt(R["vn"][sl, :], R["u"][sl, :], pS[sl, :], ALU.subtract, [sk + "u", kS], [sk + "vn"])
            yield
            C.mm(pO[sl, :], R["qg"][:, sl], S16[d][:], True, False, [sk + "qg", "S16_" + kd], [kS])
            C.mm(pO[sl, :], R["at"][sl, sl], R["vn"][sl, :], False, True, [sk + "at", sk + "vn"], [kS])
            C.mm(pD, R["kd"][sl, :], R["vn"][sl, :], True, True, [sk + "kd", sk + "vn"], [kS])
            yield
            C.stt(S32[d][:], S32[d][:], EGL[:, d, hf, t:t + 1], pD, ALU.mult, ALU.add,
                  ["S32_" + kd, "dEGL", kS], ["S32_" + kd])
            yield
            C.cp(S16[d][:], S32[d][:], ["S32_" + kd], ["S16_" + kd], eng="scalar")
            C.tt(obuf[sl, t, :], obuf[sl, t, :], pO[sl, :], ALU.add, ["ob%d" % t, kS], ["ob%d" % t])
            yield

        def steps(d, idxs, order):
            for i_ in idxs:
                t = order[i_]
                slot = i_ % NSLOT
                sk = "r%d_%d_" % (d, slot)
                for hf in ((0, 1) if d == 0 else (1, 0)):
                    yield from step(t, hf, d, slot, sk)

        def interleave(gens):
            gens = list(gens)
            while gens:
                for g_ in list(gens):
                    try:
                        next(g_)
                    except StopIteration:
                        gens.remove(g_)

        order_f = list(range(NT))
        order_b = [1, 0] + list(range(NT - 1, 1, -1))
        orders = (order_f, order_b)

        def preps_for(j):
            gl_ = []
            for i_ in range(NPAR * j, NPAR * j + NPAR):
                for d in range(2):
                    slot = i_ % NSLOT
                    gl_.append(prep(orders[d][i_], d, slot, "r%d_%d_" % (d, slot), i_ % NPAR))
            return gl_

        assert NT % NPAR == 0
        NP = NT // NPAR
        interleave(preps_for(0))
        for j in range(NP):
            gl_ = preps_for(j + 1) if j + 1 < NP else []
            gl_.append(steps(0, range(NPAR * j, NPAR * j + NPAR), order_f))
            gl_.append(steps(1, range(NPAR * j, NPAR * j + NPAR), order_b))
            interleave(gl_)
        with contextlib.ExitStack() as es2:
            sqb = vtok
            ssq = C.sb("dssq", [128, NT], F32, es2)
            oT = qT
            on = [C.sb("don", [128, 128], BF16, es2) for _ in range(2)]
            allob = ["ob%d" % t for t in range(NT)]
            C.tt(sqb[:], obuf[:], obuf[:], ALU.mult, allob, ["dvtok"])
            C.red(ssq[:], sqb[:], ALU.add, ["dvtok"], ["dssq"])
            C.ts(ssq[:], ssq[:], 1.0 / 128, ALU.mult, ["dssq"], ["dssq"], s2=EPS, op1=ALU.add)
            C.act(ssq[:], ssq[:], AF.Sqrt, ["dssq"], ["dssq"])
            C.recip(ssq[:], ssq[:], ["dssq"], ["dssq"])
            for t in range(NT):
                i = t % 2
                C.stt(on[i][:], obuf[:, t, :], ssq[:, t:t + 1], sz[:, t, :], ALU.mult, ALU.mult,
                      ["ob%d" % t, "dssq", "dsz"], ["don%d" % i])
                p, pk = pp.get()
                pb = p[:].bitcast(BF16)
                C.tr(pb[:, 0:128], on[i][:], identb[:], ["don%d" % i, "identb"], [pk])
                C.cp(oT[:, t * 128:(t + 1) * 128], pb[:, 0:128], [pk], ["dqT"], eng="scalar")
            C.dma(dnT_d[b], oT[:], ["dqT"], ["dnT_d"], eng="sync")
            C.flush()
        C.flush()


OFF_Q, OFF_K, OFF_V, OFF_Z, OFF_AB, OFF_NQ, OFF_NK, OFF_NV, OFF_GD, OFF_GN = (
    0, 1024, 2048, 3072, 4096, 4128, 4640, 5152, 5664, 6688)

_CONST_CACHE = {}


def b_consts():
    if "b" in _CONST_CACHE:
        return _CONST_CACHE["b"]
    idx = np.arange(128)
    same = (idx[:, None] // 64) == (idx[None, :] // 64)
    a, bq = idx[:, None], idx[None, :]
    cm = np.stack([
        same & (a <= bq),
        same & (a >= bq),
        same,
        np.broadcast_to(a < 64, (128, 128)),
        np.broadcast_to(a >= 64, (128, 128)),
    ]).astype(np.float32)
    big = 30000.0
    cmb = np.stack([
        np.where(same & (a > bq), 0.0, big),
        np.where(same & (bq >= a), 0.0, -big),
        np.where(same & (a < bq), 0.0, big),
        np.where(same & (bq <= a), 0.0, -big),
        np.where(a == bq + 64, -1.0, 0.0) + np.where(a == bq - 64, 1.0, 0.0),
        np.ones((128, 128)),
    ]).astype(NPBF)
    t = np.arange(SEQ)
    row = (t // 64).astype(np.float32)
    col = (t % 64).astype(np.float32)
    inv = (np.float32(10000.0) ** (-np.arange(32, dtype=np.float32) / np.float32(32))).astype(np.float32)
    ang = np.concatenate([row[None, :] * inv[:, None], col[None, :] * inv[:, None]], 0)
    ang = np.concatenate([ang, ang], 0).astype(np.float32)
    cosT = np.cos(ang).astype(np.float32)
    sinT = np.sin(ang).astype(np.float32)
    reps = [0, 2, 10, 124, 126]
    ri = np.zeros((5, 128, 640), np.int64)
    ci = np.zeros((5, 128, 640), np.int64)
    mk = np.zeros((5, 128, 640), np.float32)
    qq = np.arange(128)
    kk = np.arange(640)
    dr, cq = qq // 64, qq % 64
    kr, ck = kk // 64, kk % 64
    for pi, r in enumerate(reps):
        ws = min(max(r - 4, 0), 118)
        R_ = r + dr
        r0 = np.clip(R_ - 4, 0, 120)
        krow = ws + kr
        okr = (krow[None, :] >= r0[:, None]) & (krow[None, :] < r0[:, None] + 8)
        c0 = np.clip(cq - 8, 0, 48)
        okc = (ck[None, :] >= c0[:, None]) & (ck[None, :] < c0[:, None] + 16)
        ok = okr & okc
        ri[pi] = np.clip(krow[None, :] - R_[:, None] + 7, 0, 14)
        ci[pi] = np.clip(ck[None, :] - cq[:, None] + 15, 0, 30)
        mk[pi] = np.where(ok, 0.0, NEG)
    out = dict(cm=cm, cmb=cmb, cosT=cosT, sinT=sinT, ri=ri, ci=ci, mk=mk,
               identb=np.eye(128).astype(NPBF), identf=np.eye(128, dtype=np.float32))
    _CONST_CACHE["b"] = out
    return out


def host_inputs_B(inp, l, hd, hT_full):
    k = b_consts()
    w_in = inp["w_in"][l]
    sl = lambda o, n: np.ascontiguousarray(w_in[:, o + hd * n:o + (hd + 1) * n])
    ab_cols = [OFF_AB + hd, OFF_AB + 8 + hd, OFF_AB + 16 + hd, OFF_AB + 24 + hd]
    rpb = inp["na_rpb"][l, hd]
    conv = inp["dn_conv"][l]
    im = dict(
        hT_full=hT_full,
        wnaq=sl(OFF_NQ, 64), wnak=sl(OFF_NK, 64), wnav=sl(OFF_NV, 64),
        nabias=np.ascontiguousarray(rpb[k["ri"], k["ci"]]).astype(np.float32), namask=k["mk"],
        wq=sl(OFF_Q, 128), wk=sl(OFF_K, 128), wv=sl(OFF_V, 128),
        wzab=np.ascontiguousarray(np.concatenate([w_in[:, OFF_Z + hd * 128:OFF_Z + (hd + 1) * 128], w_in[:, ab_cols]], 1)),
        conv=np.ascontiguousarray(np.stack([conv[x * 1024 + hd * 128:x * 1024 + (hd + 1) * 128] for x in range(3)])),
        dnpar=np.ascontiguousarray(np.broadcast_to(np.array([inp["dn_a_log"][l, 0, hd], inp["dn_a_log"][l, 1, hd],
                                                             inp["dn_dt_bias"][l, 0, hd], inp["dn_dt_bias"][l, 1, hd]],
                                                            np.float32)[None], (128, 4))),
        normw=np.ascontiguousarray(np.broadcast_to(inp["dn_norm"][l][None], (128, 128))).astype(np.float32),
        cosT=k["cosT"], sinT=k["sinT"], cm=k["cm"], cmb=k["cmb"], identb=k["identb"], identf=k["identf"],
    )
    return im, None


def build_B(do_na=True, do_dn=True, nbatch=NB):
    nc = bass.Bass("TRN2", target_bir_lowering=False)
    C = Ctx(nc)
    hT_full_d = C.dram("hT_full", [NB, D, TB], BF16, "ExternalInput")
    wnaq_d = C.dram("wnaq", [D, 64], F32, "ExternalInput")
    wnak_d = C.dram("wnak", [D, 64], F32, "ExternalInput")
    wnav_d = C.dram("wnav", [D, 64], F32, "ExternalInput")
    nabias_d = C.dram("nabias", [5, 128, 640], F32, "ExternalInput")
    namask_d = C.dram("namask", [5, 128, 640], F32, "ExternalInput")
    wq_d = C.dram("wq", [D, 128], F32, "ExternalInput")
    wk_d = C.dram("wk", [D, 128], F32, "ExternalInput")
    wv_d = C.dram("wv", [D, 128], F32, "ExternalInput")
    wzab_d = C.dram("wzab", [D, 132], F32, "ExternalInput")
    conv_d = C.dram("conv", [3, 128, 5], F32, "ExternalInput")
    dnpar_d = C.dram("dnpar", [128, 4], F32, "ExternalInput")
    normw_d = C.dram("normw", [128, 128], F32, "ExternalInput")
    cos_d = C.dram("cosT", [128, SEQ], F32, "ExternalInput")
    sin_d = C.dram("sinT", [128, SEQ], F32, "ExternalInput")
    cm_d = C.dram("cm", [5, 128, 128], F32, "ExternalInput")
    cmb_d = C.dram("cmb", [6, 128, 128], BF16, "ExternalInput")
    identb_d = C.dram("identb", [128, 128], BF16, "ExternalInput")
    identf_d = C.dram("identf", [128, 128], F32, "ExternalInput")
    naT_d = C.dram("naT", [NB, 64, TB], BF16, "ExternalOutput")
    dnT_d = C.dram("dnT", [NB, 128, TB], BF16, "ExternalOutput")
    pp = PsumPool(C)
    identb, identf, ones_f = load_consts(C, identb_d, identf_d)
    for b in range(nbatch):
        if do_na:
            phase_NA(C, pp, b, hT_full_d, wnaq_d, wnak_d, wnav_d, nabias_d, namask_d, naT_d, identb)
        if do_dn:
            phase_DN(C, pp, b, hT_full_d, wq_d, wk_d, wv_d, wzab_d, conv_d, dnpar_d, normw_d, cos_d, sin_d,
                     cm_d, cmb_d, dnT_d, identb, identf, ones_f)
    _finish(C)
    return nc


def _rep(v, n=128):
    return np.ascontiguousarray(np.broadcast_to(np.asarray(v, np.float32)[None], (n,) + tuple(v.shape)))


def _run(nc, in_maps):
    res = run_bass_kernel_spmd(nc, in_maps, core_ids=list(range(len(in_maps))))
    return res.results


def _assemble_hT(hT_sh):
    out = np.empty((NB, D, TB), NPBF)
    for c in range(8):
        b, q = c // 4, c % 4
        out[b, :, CTX + 2048 * q:CTX + 2048 * (q + 1)] = hT_sh[c][:, :2048]
        out[b, :, 64 * q:64 * (q + 1)] = hT_sh[c][:, 2048:]
    return out


def _reshard_mixer(dnT, naT):
    dn_all, na_all = [], []
    for c in range(8):
        b, q = c // 4, c % 4
        cols = np.r_[CTX + 2048 * q:CTX + 2048 * (q + 1), 64 * q:64 * (q + 1)]
        dn_all.append(np.ascontiguousarray(np.stack([dnT[hd][b][:, cols] for hd in range(8)])))
        na = np.stack([naT[hd][b][:, cols] for hd in range(8)])
        na_all.append(np.ascontiguousarray(na.reshape(4, 128, OWN)))
    return dn_all, na_all


def kernel_unfused(**inp):
    inp = {k: np.asarray(v) for k, v in inp.items()}
    K = b_consts()
    x, ctx = inp["x"], inp["ctx"]
    xs, cvT = [], []
    for c in range(8):
        b, q = c // 4, c % 4
        xs.append(np.ascontiguousarray(np.concatenate([x[b, 2048 * q:2048 * (q + 1)], ctx[b, 64 * q:64 * (q + 1)]], 0)))
        cvT.append(np.ascontiguousarray(np.stack([inp["c"][b].reshape(8, 128).T, inp["c_ctx"].reshape(8, 128).T], 1)
                                        .astype(np.float32)))
    ada_w = [np.ascontiguousarray(inp["ada_w"][l:l + 1]) for l in range(2)]
    ada_b = [_rep(inp["ada_b"][l])[None] for l in range(2)]

    ncA = build_A()
    res = _run(ncA, [dict(xs=xs[c], cvT=cvT[c], ada_w=ada_w[0], ada_b=ada_b[0], gpre=_rep(inp["norm_mix_pre"][0]),
                          identb=K["identb"]) for c in range(8)])
    hT_sh = [r["hT"] for r in res]
    ncB = build_B()
    for l in range(2):
        hT_full = _assemble_hT(hT_sh)
        res = _run(ncB, [host_inputs_B(inp, l, hd, hT_full)[0] for hd in range(8)])
        dn_all, na_all = _reshard_mixer([r["dnT"] for r in res], [r["naT"] for r in res])
        moe = (l % 2 == 1)
        last = (l == 1)
        ncC = build_C(moe, not last)
        w_in = inp["w_in"][l]
        common = dict(ada_w=ada_w[l], ada_b=ada_b[l], gpost=_rep(inp["norm_mix_post"][l]), gpre2=_rep(inp["norm_ffn_pre"][l]),
                      gpost2=_rep(inp["norm_ffn_post"][l]), wg=np.ascontiguousarray(w_in[:, OFF_GD:OFF_GD + 2048]),
                      wpa=inp["w_branch_dn"][l], wpb=inp["w_branch_na"][l], wout=inp["w_out"][l],
                      identb=K["identb"], identf=K["identf"])
        if moe:
            common.update(router=inp["moe_router"][l // 2], w1=inp["moe_w1"][l // 2], w3=inp["moe_w3"][l // 2],
                          w2=inp["moe_w2"][l // 2])
        else:
            common.update(w1=inp["ffn_w1"][l // 2:l // 2 + 1], w3=inp["ffn_w3"][l // 2:l // 2 + 1],
                          w2=inp["ffn_w2"][l // 2:l // 2 + 1])
        if not last:
            common.update(ada_w_n=ada_w[l + 1], ada_b_n=ada_b[l + 1], gpre_n=_rep(inp["norm_mix_pre"][l + 1]))
        res = _run(ncC, [dict(common, xs=xs[c], hT=hT_sh[c], dnT=dn_all[c], naT=na_all[c], cvT=cvT[c]) for c in range(8)])
        xs = [r["xs_out"] for r in res]
        if not last:
            hT_sh = [r["hT_n"] for r in res]
    out = np.empty((NB, SEQ, D), np.float32)
    for c in range(8):
        b, q = c // 4, c % 4
        out[b, 2048 * q:2048 * (q + 1)] = xs[c][:2048]
    return out


def mods_to_dram(C, pp, l, cvT_d, ada_w_d, ada_b_d, ones_f, mods_d):
    for grp in ([0, 1, 2], [3, 4, 5]):
        with contextlib.ExitStack() as es:
            m = compute_mods(C, pp, l, grp, cvT_d, ada_w_d, ada_b_d, ones_f, es)
            for blk in grp:
                C.dma(mods_d[l, blk], m[blk][:], ["mod%d" % blk], ["modsd"], eng="sync")
            C.flush()


def load_mods(C, mods_dl, blocks, es):
    out = {}
    for blk in blocks:
        t = C.sb("mod%d" % blk, [128, 2, 1024], F32, es)
        C.dma(t[:], mods_dl[blk], [], ["mod%d" % blk], eng="sync")
        out[blk] = t
    return out


def run_A2(C, pp, xs_d, mods_dl, gpre_dl, outs, identb):
    with contextlib.ExitStack() as es:
        xs = C.sb("xs", [128, 17, D], F32, es)
        hT = C.sb("hT", [128, 8, OWN], BF16, es)
        gpre = C.sb("gpre", [128, D], F32, es)
        C.dma(xs[:, 0:16, :], xs_d[0:2048, :].rearrange("(t p) d -> p t d", p=128), [], ["xs"], eng="sync")
        C.dma(xs[0:64, 16, :], xs_d[2048:2112, :], [], ["xs"], eng="sync")
        C.dma(gpre[:], gpre_dl, [], ["gpre"], eng="sync")
        mods = load_mods(C, mods_dl, [0, 1], es)
        phase_A(C, pp, xs, hT, mods, gpre, identb, es)
        for (dst, c0, c1) in outs:
            C.dma(dst, hT[:, :, c0:c1], ["hT"], ["hT_out"], eng="sync")
        C.flush()


def stage_C(C, pp, moe, xs_in_d, xs_mid_d, xs_out_d, hT_d, dnT_d, naT_d, colmap, mods_dl, G, W, identb, identf):
    with contextlib.ExitStack() as es1:
        h2T_all = C.sb("h2T", [128, 8, OWN], BF16, es1)
        gates = C.sb("gates", [128, 17, 8], F32, es1) if moe else None
        gpost2 = C.sb("gpost2", [128, D], F32, es1)
        C.dma(gpost2[:], G["gpost2"], [], ["gpost2"], eng="sync")
        with contextlib.ExitStack() as es2:
            mods = load_mods(C, mods_dl, [2, 3, 4], es2)
            gpost = C.sb("gpost", [128, D], F32, es2)
            gpre2 = C.sb("gpre2", [128, D], F32, es2)
            C.dma(gpost[:], G["gpost"], [], ["gpost"], eng="sync")
            C.dma(gpre2[:], G["gpre2"], [], ["gpre2"], eng="sync")
            yT_all = C.sb("yT", [128, 8, OWN], BF16, es2)
            phase_C1(C, pp, hT_d, dnT_d, naT_d, W["wg"], W["wpa"], W["wpb"], yT_all, es2, colmap=colmap)
            phase_C2(C, pp, yT_all, W["wout"], xs_in_d, xs_mid_d, mods, gpost, gpre2, h2T_all, identb, identf,
                     W.get("router"), gates, es2)
            C.flush()
        with contextlib.ExitStack() as es3:
            acc = C.sb("acc", [128, 17, D], F32, es3)
            phase_C3(C, pp, h2T_all, W["w1"], W["w3"], W["w2"], (NE if moe else 1), (DFE if moe else DFF), gates, acc, es3)
            mods5 = load_mods(C, mods_dl, [5], es3)
            phase_C4(C, pp, acc, xs_mid_d, xs_out_d, mods5, gpost2, 17, es3)
            C.flush()
        C.flush()


def select4(C, sel, jobs, width, dt):
    with contextlib.ExitStack() as es:
        src = [[C.sb("selsrc", [128, width], dt, es) for _ in range(4)] for _ in range(2)]
        acc = [C.sb("selacc", [128, width], dt, es) for _ in range(2)]
        for ji, (dst, rows, slots) in enumerate(jobs):
            i = ji % 2
            for s in range(4):
                for (ap, c0, c1, r0, r1) in slots[s]:
                    C.dma(src[i][s][r0:r1, c0:c1], ap, [], ["selsrc%d_%d" % (i, s)], eng="sync")
            C.ts(acc[i][:rows, :], src[i][0][:rows, :], sel[:rows, 0:1], ALU.mult, ["selsrc%d_0" % i, "sel"], ["selacc%d" % i])
            for s in range(1, 4):
                C.stt(acc[i][:rows, :], src[i][s][:rows, :], sel[:rows, s:s + 1], acc[i][:rows, :], ALU.mult, ALU.add,
                      ["selsrc%d_%d" % (i, s), "sel", "selacc%d" % i], ["selacc%d" % i])
            C.dma(dst, acc[i][:rows, :], ["selacc%d" % i], ["seldst"], eng="sync")
        C.flush()


def build_fused(n_heads=8):
    nc = bass.Bass("TRN2", target_bir_lowering=False)
    C = Ctx(nc)
    I = "ExternalInput"
    xb_d = C.dram("xb", [SEQ, D], F32, I)
    ctxb_d = C.dram("ctxb", [CTX, D], F32, I)
    cvT_d = C.dram("cvT", [128, 2, 8], F32, I)
    sel_d = C.dram("sel", [128, 4], F32, I)
    ada_w_d = C.dram("ada_w", [2, D, 6 * D], F32, I)
    ada_b_d = C.dram("ada_b", [2, 128, 6 * D], F32, I)
    gains_d = C.dram("gains", [2, 4, 128, D], F32, I)
    w_in_d = C.dram("w_in", [2, D, D_IN], F32, I)
    conv_d = C.dram("dn_conv", [2, 3072, 5], F32, I)
    dnpar_d = C.dram("dnpar", [2, 8, 128, 4], F32, I)
    normw_d = C.dram("normw", [2, 128, 128], F32, I)
    nabias_d = C.dram("nabias", [2, 8, 5, 128, 640], F32, I)
    namask_d = C.dram("namask", [5, 128, 640], F32, I)
    cos_d = C.dram("cosT", [128, SEQ], F32, I)
    sin_d = C.dram("sinT", [128, SEQ], F32, I)
    cm_d = C.dram("cm", [5, 128, 128], F32, I)
    cmb_d = C.dram("cmb", [6, 128, 128], BF16, I)
    identb_d = C.dram("identb", [128, 128], BF16, I)
    identf_d = C.dram("identf", [128, 128], F32, I)
    wbd_d = C.dram("w_branch_dn", [2, D, D], F32, I)
    wbn_d = C.dram("w_branch_na", [2, 512, D], F32, I)
    wout_d = C.dram("w_out", [2, D, D], F32, I)
    f1_d = C.dram("ffn_w1", [1, D, DFF], F32, I)
    f3_d = C.dram("ffn_w3", [1, D, DFF], F32, I)
    f2_d = C.dram("ffn_w2", [1, DFF, D], F32, I)
    rt_d = C.dram("moe_router", [1, D, NE], F32, I)
    m1_d = C.dram("moe_w1", [1, NE, D, DFE], F32, I)
    m3_d = C.dram("moe_w3", [1, NE, D, DFE], F32, I)
    m2_d = C.dram("moe_w2", [1, NE, DFE, D], F32, I)
    out_d = C.dram("xs_out", [OWN, D], F32, "ExternalOutput")
    N = "Internal"
    xwork = C.dram("xwork", [4, OWN, D], F32, N)
    xmid = C.dram("xmid", [OWN, D], F32, N)
    hT_full = C.dram("hT_full", [1, D, TB], BF16, N)
    hT_q = C.dram("hT_q", [4, D, OWN], BF16, N)
    dnT_all = C.dram("dnT_all", [8, 128, TB], BF16, N)
    naT_all = C.dram("naT_all", [8, 64, TB], BF16, N)
    mods_d = C.dram("mods", [2, 6, 128, 2, 1024], F32, N)
    xs_sel = C.dram("xs_sel", [OWN, D], F32, N)
    hT_sel = C.dram("hT_sel", [D, OWN], BF16, N)
    dn_sel = C.dram("dn_sel", [8, 128, OWN], BF16, N)
    na_sel = C.dram("na_sel", [4, 128, OWN], BF16, N)

    pp = PsumPool(C)
    identb, identf, ones_f = load_consts(C, identb_d, identf_d)
    sel = C.sb("sel", [128, 4], F32)
    C.dma(sel[:], sel_d, [], ["sel"], eng="sync")
    for s in range(4):
        C.dma(xwork[s][0:2048, :], xb_d[2048 * s:2048 * (s + 1), :], [], ["xw%d" % s], eng="sync")
        C.dma(xwork[s][2048:2112, :], ctxb_d[64 * s:64 * (s + 1), :], [], ["xw%d" % s], eng="sync")
    C.flush()
    hfv = hT_full[0].rearrange("(k p) t -> p k t", p=128)
    for l in range(2):
        moe = (l % 2 == 1)
        mods_to_dram(C, pp, l, cvT_d, ada_w_d, ada_b_d, ones_f, mods_d)
        for s in range(4):
            outs = [(hT_q[s].rearrange("(k p) t -> p k t", p=128), 0, OWN),
                    (hfv[:, :, CTX + 2048 * s:CTX + 2048 * (s + 1)], 0, 2048),
                    (hfv[:, :, 64 * s:64 * (s + 1)], 2048, OWN)]
            run_A2(C, pp, xwork[s], mods_d[l], gains_d[l, 0], outs, identb)
        wl = w_in_d[l]
        for hd in range(n_heads):
            cs = lambda o, n: wl[:, o + hd * n:o + (hd + 1) * n]
            phase_NA(C, pp, 0, hT_full, cs(OFF_NQ, 64), cs(OFF_NK, 64), cs(OFF_NV, 64), nabias_d[l, hd], namask_d,
                     naT_all[hd:hd + 1], identb)
            convv = conv_d[l].rearrange("(x c) k -> x c k", x=3)[:, hd * 128:(hd + 1) * 128, :]
            phase_DN(C, pp, 0, hT_full, cs(OFF_Q, 128), cs(OFF_K, 128), cs(OFF_V, 128), (cs(OFF_Z, 128), wl[:, OFF_AB:OFF_AB + 32], hd), convv,
                     dnpar_d[l, hd], normw_d[l], cos_d, sin_d, cm_d, cmb_d, dnT_all[hd:hd + 1], identb, identf, ones_f)
        G = dict(gpost=gains_d[l, 1], gpre2=gains_d[l, 2], gpost2=gains_d[l, 3])
        W = dict(wg=wl[:, OFF_GD:OFF_GD + 2048], wpa=wbd_d[l], wpb=wbn_d[l], wout=wout_d[l])
        if not moe:
            W.update(w1=f1_d, w3=f3_d, w2=f2_d)
            for s in range(4):
                colmap = (lambda tt, s=s: (CTX + 2048 * s + tt * 512) if tt < 4 else 64 * s)
                stage_C(C, pp, False, xwork[s], xmid, xwork[s], hT_q[s], dnT_all, naT_all, colmap, mods_d[l], G, W,
                        identb, identf)
        else:
            W.update(w1=m1_d[0], w3=m3_d[0], w2=m2_d[0], router=rt_d[0])
            jobs = []
            for t in range(17):
                rows = 128 if t < 16 else 64
                jobs.append((xs_sel[t * 128:t * 128 + rows, :], rows,
                             [[(xwork[s][t * 128:t * 128 + rows, :], 0, D, 0, rows)] for s in range(4)]))
            select4(C, sel, jobs, D, F32)
            jobs = []
            for k in range(8):
                jobs.append((hT_sel[k * 128:(k + 1) * 128, :], 128,
                             [[(hT_q[s][k * 128:(k + 1) * 128, :], 0, OWN, 0, 128)] for s in range(4)]))
            for h in range(8):
                jobs.append((dn_sel[h], 128,
                             [[(dnT_all[h][:, CTX + 2048 * s:CTX + 2048 * (s + 1)], 0, 2048, 0, 128),
                               (dnT_all[h][:, 64 * s:64 * (s + 1)], 2048, OWN, 0, 128)] for s in range(4)]))
            for j in range(4):
                slots = []
                for s in range(4):
                    sl_ = []
                    for tw in range(2):
                        src_h = naT_all[2 * j + tw]
                        sl_.append((src_h[:, CTX + 2048 * s:CTX + 2048 * (s + 1)], 0, 2048, tw * 64, tw * 64 + 64))
                        sl_.append((src_h[:, 64 * s:64 * (s + 1)], 2048, OWN, tw * 64, tw * 64 + 64))
                    slots.append(sl_)
                jobs.append((na_sel[j], 128, slots))
            select4(C, sel, jobs, OWN, BF16)
            stage_C(C, pp, True, xs_sel, xmid, out_d, hT_sel, dn_sel, na_sel, None, mods_d[l], G, W, identb, identf)
    _finish(C)
    return nc


def fused_inputs(inp):
    K = b_consts()
    ri, ci = K["ri"], K["ci"]
    nabias = np.ascontiguousarray(np.stack([np.stack([inp["na_rpb"][l, hd][ri, ci] for hd in range(8)]) for l in range(2)])
                                  ).astype(np.float32)
    dnpar = np.empty((2, 8, 128, 4), np.float32)
    for l in range(2):
        for hd in range(8):
            dnpar[l, hd] = np.array([inp["dn_a_log"][l, 0, hd], inp["dn_a_log"][l, 1, hd],
                                     inp["dn_dt_bias"][l, 0, hd], inp["dn_dt_bias"][l, 1, hd]], np.float32)[None]
    gains = np.stack([np.stack([_rep(inp[k][l]) for k in ("norm_mix_pre", "norm_mix_post", "norm_ffn_pre", "norm_ffn_post")])
                      for l in range(2)])
    shared = dict(
        ada_w=np.ascontiguousarray(inp["ada_w"]), ada_b=np.stack([_rep(inp["ada_b"][l]) for l in range(2)]),
        gains=np.ascontiguousarray(gains), w_in=np.ascontiguousarray(inp["w_in"]), dn_conv=np.ascontiguousarray(inp["dn_conv"]),
        dnpar=dnpar, normw=np.stack([_rep(inp["dn_norm"][l]) for l in range(2)]), nabias=nabias, namask=K["mk"],
        cosT=K["cosT"], sinT=K["sinT"], cm=K["cm"], cmb=K["cmb"], identb=K["identb"], identf=K["identf"],
        w_branch_dn=inp["w_branch_dn"], w_branch_na=inp["w_branch_na"], w_out=inp["w_out"],
        ffn_w1=inp["ffn_w1"], ffn_w3=inp["ffn_w3"], ffn_w2=inp["ffn_w2"], moe_router=inp["moe_router"],
        moe_w1=inp["moe_w1"], moe_w3=inp["moe_w3"], moe_w2=inp["moe_w2"])
    shared = {k: np.ascontiguousarray(v) for k, v in shared.items()}
    maps = []
    for c in range(8):
        b, q = c // 4, c % 4
        selv = np.zeros((128, 4), np.float32)
        selv[:, q] = 1.0
        m = dict(shared)
        m.update(xb=np.ascontiguousarray(inp["x"][b]), ctxb=np.ascontiguousarray(inp["ctx"][b]),
                 cvT=np.ascontiguousarray(np.stack([inp["c"][b].reshape(8, 128).T, inp["c_ctx"].reshape(8, 128).T], 1)
                                          .astype(np.float32)), sel=selv)
        maps.append(m)
    return maps


def kernel_fused(**inp):
    inp = {k: np.asarray(v) for k, v in inp.items()}
    nc = build_fused()
    res = _run(nc, fused_inputs(inp))
    out = np.empty((NB, SEQ, D), np.float32)
    for c in range(8):
        b, q = c // 4, c % 4
        out[b, 2048 * q:2048 * (q + 1)] = res[c]["xs_out"][:2048]
    return out


def kernel(**inp):
    return kernel_fused(**inp)
```
